# Optimizing a Trainium2 kernel written in Bass

```python
import jax, jax.numpy as jnp
from jax import lax
import numpy as np

D_MODEL = 1024
BATCH = 4
SEQ = 8192
DEPTH = 4
DEC_BATCH = 8
DEC_SEQ = 2048
PAST_LEN = 128

GRID_W = 64
NA_HEADS = 8
NA_HEAD_DIM = 64
NA_WIDTH = NA_HEADS * NA_HEAD_DIM
NA_KH_MAX = 8
NA_KW = 16
MLA_HEADS = 8
MLA_NOPE_DIM = 64
MLA_ROPE_DIM = 32
MLA_V_DIM = 64
MLA_Q_RANK = 256
MLA_KV_RANK = 128
MLA_WIDTH = MLA_HEADS * MLA_V_DIM
ROPE_THETA = 10000.0
Q_BLOCK = 128
N_GROUPS = 4
EXPERTS_PER_GROUP = 8
N_EXPERTS = N_GROUPS * EXPERTS_PER_GROUP
TOP_K = 2
D_EXPERT = 512
MOE_BLOCK = 128
EPS = 1e-6
IN_SPLITS = (NA_WIDTH, 2 * NA_WIDTH, 3 * NA_WIDTH,
             3 * NA_WIDTH + MLA_Q_RANK,
             3 * NA_WIDTH + MLA_Q_RANK + MLA_KV_RANK,
             3 * NA_WIDTH + MLA_Q_RANK + MLA_KV_RANK + MLA_ROPE_DIM)
IN_WIDTH = IN_SPLITS[-1] + 2 * D_MODEL

kernel_name = "hybrid_natten_mla_hmoe_encoder"


def rms_norm(x, g):
    xf = x.astype(jnp.float32)
    var = jnp.mean(xf * xf, axis=-1, keepdims=True)
    return (xf * lax.rsqrt(var + EPS)).astype(x.dtype) * g


def rope_tables(seq_len, dtype):
    inv = 1.0 / (ROPE_THETA ** (jnp.arange(0, MLA_ROPE_DIM, 2, dtype=jnp.float32) / MLA_ROPE_DIM))
    ang = jnp.arange(seq_len, dtype=jnp.float32)[:, None] * inv[None, :]
    return jnp.cos(ang).astype(dtype), jnp.sin(ang).astype(dtype)


def apply_rope(x, cos, sin):
    x1, x2 = jnp.split(x, 2, axis=-1)
    return jnp.concatenate([x1 * cos - x2 * sin, x1 * sin + x2 * cos], axis=-1)


def neighbourhood_attention(q, k, v, rpb):
    b, s, h, dh = q.shape
    rows = s // GRID_W
    kh = min(NA_KH_MAX, rows)
    qg = q.reshape(b, rows, GRID_W, h, dh)
    kg = k.reshape(b, rows, GRID_W, h, dh)
    vg = v.reshape(b, rows, GRID_W, h, dh)
    col = jnp.arange(GRID_W)
    col_start = jnp.clip(col - NA_KW // 2, 0, GRID_W - NA_KW)
    col_idx = col_start[:, None] + jnp.arange(NA_KW)[None, :]
    dc = col_idx - col[:, None]
    scale = dh ** -0.5

    def one_row(r):
        rs = jnp.clip(r - kh // 2, 0, rows - kh)
        q_r = lax.dynamic_index_in_dim(qg, r, axis=1, keepdims=False)
        k_rows = lax.dynamic_slice_in_dim(kg, rs, kh, axis=1)
        v_rows = lax.dynamic_slice_in_dim(vg, rs, kh, axis=1)
        k_nb = k_rows[:, :, col_idx]
        v_nb = v_rows[:, :, col_idx]
        dr = rs + jnp.arange(kh) - r
        bias = rpb[:, dr[:, None, None] + NA_KH_MAX - 1, dc[None] + NA_KW - 1]
        sc = jnp.einsum('bwhd,bxwyhd->bhwxy', q_r, k_nb,
                        preferred_element_type=jnp.float32) * scale
        sc = sc + bias.transpose(0, 2, 1, 3)[None].astype(jnp.float32)
        p = jax.nn.softmax(sc.reshape(b, h, GRID_W, kh * NA_KW), axis=-1)
        p = p.reshape(b, h, GRID_W, kh, NA_KW).astype(v.dtype)
        return jnp.einsum('bhwxy,bxwyhd->bwhd', p, v_nb)

    out = lax.map(one_row, jnp.arange(rows))
    return out.transpose(1, 0, 2, 3, 4).reshape(b, s, h * dh)


def mla_attention(c_q, c_kv, k_rope, q_norm_g, w_uq, kv_norm_g, w_ukv):
    b, s, _ = c_q.shape
    q = (rms_norm(c_q, q_norm_g) @ w_uq).reshape(b, s, MLA_HEADS, MLA_NOPE_DIM + MLA_ROPE_DIM)
    kv = (rms_norm(c_kv, kv_norm_g) @ w_ukv).reshape(b, s, MLA_HEADS, MLA_NOPE_DIM + MLA_V_DIM)
    q_nope, q_rope = jnp.split(q, [MLA_NOPE_DIM], axis=-1)
    k_nope, v = jnp.split(kv, [MLA_NOPE_DIM], axis=-1)
    cos, sin = rope_tables(s, q.dtype)
    q_rope = apply_rope(q_rope, cos[:, None, :], sin[:, None, :])
    k_rope = apply_rope(k_rope, cos, sin)
    scale = (MLA_NOPE_DIM + MLA_ROPE_DIM) ** -0.5
    nq = s // Q_BLOCK
    qn_blocks = q_nope.reshape(b, nq, Q_BLOCK, MLA_HEADS, MLA_NOPE_DIM).transpose(1, 0, 2, 3, 4)
    qr_blocks = q_rope.reshape(b, nq, Q_BLOCK, MLA_HEADS, MLA_ROPE_DIM).transpose(1, 0, 2, 3, 4)

    def one_block(args):
        qn, qr = args
        sc = (jnp.einsum('bqhd,bkhd->bhqk', qn, k_nope, preferred_element_type=jnp.float32)
              + jnp.einsum('bqhr,bkr->bhqk', qr, k_rope, preferred_element_type=jnp.float32))
        p = jax.nn.softmax(sc * scale, axis=-1).astype(v.dtype)
        return jnp.einsum('bhqk,bkhd->bqhd', p, v)

    out = lax.map(one_block, (qn_blocks, qr_blocks))
    return out.transpose(1, 0, 2, 3, 4).reshape(b, s, MLA_WIDTH)


def hierarchical_moe(h, wg, bg, we, be, w1, w3, w2):
    b, s, d = h.shape
    t = b * s
    hf = h.reshape(t, d)
    g_prob = jax.nn.softmax((hf @ wg).astype(jnp.float32) + bg, axis=-1)
    g_top, g_idx = lax.top_k(g_prob, 1)
    e_logits = ((hf @ we).astype(jnp.float32) + be).reshape(t, N_GROUPS, EXPERTS_PER_GROUP)
    e_logits = jnp.take_along_axis(e_logits, g_idx[:, :, None], axis=1)[:, 0]
    e_prob = jax.nn.softmax(e_logits, axis=-1)
    e_top, e_idx = lax.top_k(e_prob, TOP_K)
    weights = g_top * e_top / jnp.sum(e_top, axis=-1, keepdims=True)
    expert_id = (g_idx * EXPERTS_PER_GROUP + e_idx).reshape(-1)
    token_id = jnp.repeat(jnp.arange(t), TOP_K)
    w_flat = weights.reshape(-1)
    n_assign = t * TOP_K

    order = jnp.argsort(expert_id)
    sorted_e = expert_id[order]
    sorted_tok = token_id[order]
    counts = jnp.zeros((N_EXPERTS,), jnp.int32).at[expert_id].add(1)
    starts = jnp.cumsum(counts) - counts
    padded = (counts + MOE_BLOCK - 1) // MOE_BLOCK * MOE_BLOCK
    pad_ends = jnp.cumsum(padded)
    pad_starts = pad_ends - padded
    dest = pad_starts[sorted_e] + jnp.arange(n_assign) - starts[sorted_e]
    n_blocks = (n_assign + N_EXPERTS * (MOE_BLOCK - 1) + MOE_BLOCK - 1) // MOE_BLOCK
    buf = jnp.zeros((n_blocks * MOE_BLOCK, d), h.dtype).at[dest].set(hf[sorted_tok])
    block_expert = jnp.minimum(
        jnp.searchsorted(pad_ends, jnp.arange(n_blocks) * MOE_BLOCK, side='right'), N_EXPERTS - 1)

    def expert_block(args):
        xb, e = args
        return (jax.nn.silu(xb @ w1[e]) * (xb @ w3[e])) @ w2[e]

    y = lax.map(expert_block, (buf.reshape(n_blocks, MOE_BLOCK, d), block_expert)).reshape(-1, d)
    y_assign = y[dest] * w_flat[order][:, None].astype(y.dtype)
    out = jnp.zeros((t, d), y.dtype).at[sorted_tok].add(y_assign)
    return out.reshape(b, s, d)


def trunk(x, c, ada_w, ada_b, norm_mix_g, w_in, mla_q_norm_g, w_uq, mla_kv_norm_g, w_ukv,
          na_rpb, w_na_o, w_mla_o, w_out, norm_ffn_g, router_wg, router_bg, router_we,
          router_be, expert_w1, expert_w3, expert_w2, final_norm_g):
    b, s, _ = x.shape
    for l in range(DEPTH):
        mod = (jax.nn.silu(c) @ ada_w[l] + ada_b[l])[:, None, :]
        sh1, sc1, g1, sh2, sc2, g2 = jnp.split(mod, 6, axis=-1)
        h = rms_norm(x, norm_mix_g[l]) * (1 + sc1) + sh1
        proj = h @ w_in[l]
        q_na, k_na, v_na, c_q, c_kv, k_rope, gate_logits = jnp.split(proj, IN_SPLITS, axis=-1)
        y_na = neighbourhood_attention(
            q_na.reshape(b, s, NA_HEADS, NA_HEAD_DIM),
            k_na.reshape(b, s, NA_HEADS, NA_HEAD_DIM),
            v_na.reshape(b, s, NA_HEADS, NA_HEAD_DIM), na_rpb[l]) @ w_na_o[l]
        y_mla = mla_attention(c_q, c_kv, k_rope, mla_q_norm_g[l], w_uq[l],
                              mla_kv_norm_g[l], w_ukv[l]) @ w_mla_o[l]
        g_na, g_mla = jnp.split(jax.nn.sigmoid(gate_logits), 2, axis=-1)
        x = x + g1 * ((g_na * y_na + g_mla * y_mla) @ w_out[l])
        h2 = rms_norm(x, norm_ffn_g[l]) * (1 + sc2) + sh2
        x = x + g2 * hierarchical_moe(h2, router_wg[l], router_bg[l], router_we[l], router_be[l],
                                      expert_w1[l], expert_w3[l], expert_w2[l])
    return rms_norm(x, final_norm_g)


def setup_inputs(seed: int = 0) -> dict:
    key = jax.random.key(seed)
    ks = jax.random.split(key, 32)
    D = D_MODEL

    def nrm(k, shape, scale):
        return jax.random.normal(k, shape, jnp.float32) * scale

    def gain(k, shape):
        return 1.0 + 0.01 * jax.random.normal(k, shape, jnp.float32)

    return {
        "x_prompt": nrm(ks[0], (BATCH, SEQ, D), 1.0),
        "x_sample": nrm(ks[1], (DEC_BATCH, DEC_SEQ, D), 1.0),
        "c_prompt": nrm(ks[2], (BATCH, D), 1.0),
        "c_sample": nrm(ks[3], (DEC_BATCH, D), 1.0),
        "ada_w": nrm(ks[4], (DEPTH, D, 6 * D), 0.5 * D ** -0.5),
        "ada_b": nrm(ks[5], (DEPTH, 6 * D), 0.02),
        "norm_mix_g": gain(ks[6], (DEPTH, D)),
        "w_in": nrm(ks[7], (DEPTH, D, IN_WIDTH), D ** -0.5),
        "mla_q_norm_g": gain(ks[8], (DEPTH, MLA_Q_RANK)),
        "w_uq": nrm(ks[9], (DEPTH, MLA_Q_RANK, MLA_HEADS * (MLA_NOPE_DIM + MLA_ROPE_DIM)), MLA_Q_RANK ** -0.5),
        "mla_kv_norm_g": gain(ks[10], (DEPTH, MLA_KV_RANK)),
        "w_ukv": nrm(ks[11], (DEPTH, MLA_KV_RANK, MLA_HEADS * (MLA_NOPE_DIM + MLA_V_DIM)), MLA_KV_RANK ** -0.5),
        "na_rpb": nrm(ks[12], (DEPTH, NA_HEADS, 2 * NA_KH_MAX - 1, 2 * NA_KW - 1), 0.1),
        "w_na_o": nrm(ks[13], (DEPTH, NA_WIDTH, D), NA_WIDTH ** -0.5),
        "w_mla_o": nrm(ks[14], (DEPTH, MLA_WIDTH, D), MLA_WIDTH ** -0.5),
        "w_out": nrm(ks[15], (DEPTH, D, D), D ** -0.5),
        "norm_ffn_g": gain(ks[16], (DEPTH, D)),
        "router_wg": nrm(ks[17], (DEPTH, D, N_GROUPS), D ** -0.5),
        "router_bg": nrm(ks[18], (DEPTH, N_GROUPS), 0.01),
        "router_we": nrm(ks[19], (DEPTH, D, N_EXPERTS), D ** -0.5),
        "router_be": nrm(ks[20], (DEPTH, N_EXPERTS), 0.01),
        "expert_w1": nrm(ks[21], (DEPTH, N_EXPERTS, D, D_EXPERT), D ** -0.5),
        "expert_w3": nrm(ks[22], (DEPTH, N_EXPERTS, D, D_EXPERT), D ** -0.5),
        "expert_w2": nrm(ks[23], (DEPTH, N_EXPERTS, D_EXPERT, D), D_EXPERT ** -0.5),
        "final_norm_g": gain(ks[24], (D,)),
    }


def reference(x_prompt, x_sample, c_prompt, c_sample, ada_w, ada_b, norm_mix_g, w_in,
              mla_q_norm_g, w_uq, mla_kv_norm_g, w_ukv, na_rpb, w_na_o, w_mla_o, w_out,
              norm_ffn_g, router_wg, router_bg, router_we, router_be, expert_w1, expert_w3,
              expert_w2, final_norm_g):
    y_prompt = trunk(x_prompt, c_prompt, ada_w, ada_b, norm_mix_g, w_in, mla_q_norm_g, w_uq,
                     mla_kv_norm_g, w_ukv, na_rpb, w_na_o, w_mla_o, w_out, norm_ffn_g,
                     router_wg, router_bg, router_we, router_be, expert_w1, expert_w3,
                     expert_w2, final_norm_g)
    y_sample = trunk(x_sample, c_sample, ada_w, ada_b, norm_mix_g, w_in, mla_q_norm_g, w_uq,
                     mla_kv_norm_g, w_ukv, na_rpb, w_na_o, w_mla_o, w_out, norm_ffn_g,
                     router_wg, router_bg, router_we, router_be, expert_w1, expert_w3,
                     expert_w2, final_norm_g)
    return (y_prompt, y_sample)
```

```python
import contextlib
import numpy as np
import ml_dtypes
import concourse.bass as bass
import concourse.mybir as mybir
from concourse.bass_utils import run_bass_kernel_spmd

F32 = mybir.dt.float32
BF16 = mybir.dt.bfloat16
I32 = mybir.dt.int32
U8 = mybir.dt.uint8
AF = mybir.ActivationFunctionType
ALU = mybir.AluOpType
AX = mybir.AxisListType
NPBF = ml_dtypes.bfloat16

D = 1024
EPS = 1e-6
NEG = -30000.0
EPOCH = 30000


class Buf:
    __slots__ = ("name", "lw", "rd", "cnt", "last_dma", "sem", "multi")

    def __init__(self, name, multi=False):
        self.name = name
        self.lw = {}
        self.rd = {}
        self.cnt = 0
        self.last_dma = None
        self.sem = None
        self.multi = multi


class SemState:
    __slots__ = ("cnt", "last_dma", "sem")

    def __init__(self):
        self.cnt = 0
        self.last_dma = None
        self.sem = None


SEMREG = {}


class Op:
    __slots__ = ("idx", "eng", "fn", "deps", "isdma", "sbuf", "n", "inc", "val", "dval")


class Sched:
    def __init__(self, nc):
        self.nc = nc
        self.ops = []
        self.reg_requests = {}
        self.pool_regs = {}
        self.semreg = {}
        self.engh = {"pe": nc.tensor, "act": nc.scalar, "dve": nc.vector,
                     "pool": nc.gpsimd, "sp": nc.sync}

    def _key(self, o):
        return o.eng if not o.isdma else ("dma", id(o.sbuf))

    def _deps(self, o, reads, writes):
        deps = {}
        for b in reads:
            for w in b.lw.values():
                deps[w.idx] = w
        for b in writes:
            if not (b.multi and not b.rd):
                for w in b.lw.values():
                    deps[w.idx] = w
            for r in b.rd.values():
                deps[r.idx] = r
        o.deps = list(deps.values())
        k = self._key(o)
        for b in writes:
            if b.multi and not b.rd:
                b.lw[k] = o
            else:
                b.lw = {k: o}
            b.rd = {}
        for b in reads:
            if o in b.lw.values():
                continue
            b.rd[k] = o

    def op(self, eng, fn, reads=(), writes=()):
        o = Op()
        o.idx = len(self.ops)
        o.eng = eng
        o.fn = fn
        o.isdma = False
        o.inc = False
        o.val = None
        o.sbuf = None
        self._deps(o, reads, writes)
        self.ops.append(o)
        return o

    def dma(self, eng, fns, key, reads=(), writes=()):
        if not isinstance(fns, (list, tuple)):
            fns = [fns]
        o = Op()
        o.idx = len(self.ops)
        o.eng = eng
        o.fn = list(fns)
        o.isdma = True
        o.inc = False
        o.val = None
        ss = self.semreg.setdefault(key.name, SemState())
        o.sbuf = ss
        self._deps(o, reads, writes)
        if ss.last_dma is not None and ss.last_dma not in o.deps:
            o.deps.append(ss.last_dma)
        ss.last_dma = o
        ss.cnt += len(fns)
        o.dval = ss.cnt * 16
        self.ops.append(o)
        return o

    def emit(self, final_bufs=()):
        nc = self.nc
        ops = self.ops
        for o in ops:
            for d in o.deps:
                if d.isdma:
                    continue
                if d.eng == o.eng and o.eng == "pe" and not o.isdma:
                    continue
                d.inc = True
        cnt = {e: 0 for e in self.engh}
        for o in ops:
            if not o.isdma and o.inc:
                cnt[o.eng] += 1
                o.val = cnt[o.eng]
        es = contextlib.ExitStack()
        engsems = {}
        for e in self.engh:
            n = max(1, (cnt[e] + EPOCH - 1) // EPOCH)
            engsems[e] = [es.enter_context(nc.semaphore(f"s_{e}_{i}")) for i in range(n)]
        dmabufs = {}
        for o in ops:
            if o.isdma and id(o.sbuf) not in dmabufs:
                dmabufs[id(o.sbuf)] = o.sbuf
        for i, b in enumerate(dmabufs.values()):
            b.sem = es.enter_context(nc.semaphore(f"d_{i}"))
        self.n_sems = sum(len(v) for v in engsems.values()) + len(dmabufs)
        per = {e: [] for e in self.engh}
        for o in ops:
            per[o.eng].append(o)

        def run_engine(ename, eh):
            if ename == "pool":
                for nm, val in getattr(self, "reg_requests", {}).items():
                    r = eh.alloc_register(nm)
                    eh.reg_mov(r, val)
                    self.pool_regs[nm] = r
            known_eng = {e: 0 for e in self.engh}
            known_dma = {}
            for o in per[ename]:
                need_eng = {}
                need_dma = {}
                for d in o.deps:
                    if d.isdma:
                        k = id(d.sbuf)
                        if d.dval > known_dma.get(k, 0) and d.dval > need_dma.get(k, (0, None))[0]:
                            need_dma[k] = (d.dval, d.sbuf)
                    else:
                        if d.val is None:
                            continue
                        if d.val > known_eng[d.eng] and d.val > need_eng.get(d.eng, 0):
                            need_eng[d.eng] = d.val
                for e, v in need_eng.items():
                    eh.wait_ge(engsems[e][(v - 1) // EPOCH], (v - 1) % EPOCH + 1)
                    known_eng[e] = v
                for k, (v, b) in need_dma.items():
                    eh.wait_ge(b.sem, v)
                    known_dma[k] = v
                if o.isdma:
                    for f in o.fn:
                        f(eh).then_inc(o.sbuf.sem, 16)
                else:
                    ins = o.fn(eh)
                    if o.inc:
                        ins.then_inc(engsems[ename][(o.val - 1) // EPOCH], 1)
            if ename == "sp":
                for b in final_bufs:
                    ss = self.semreg.get(b.name)
                    if ss is not None and ss.sem is not None and ss.cnt > 0:
                        eh.wait_ge(ss.sem, ss.cnt * 16)

        with nc.Block() as block:
            @block.tensor
            def _(e):
                run_engine("pe", e)

            @block.scalar
            def _(e):
                run_engine("act", e)

            @block.vector
            def _(e):
                run_engine("dve", e)

            @block.gpsimd
            def _(e):
                run_engine("pool", e)

            @block.sync
            def _(e):
                run_engine("sp", e)
        es.close()


class Arena:
    def __init__(self, t, nbytes):
        self.t = t
        self.n = nbytes
        self.off = 0
        self.regions = []

    def mark(self):
        return self.off

    def reset(self, m=0):
        self.off = m

    def alloc(self, name, cols, dtype, parts=128):
        esz = {F32: 4, BF16: 2, I32: 4, U8: 1}[dtype]
        nb = cols * esz
        start = (self.off + 63) // 64 * 64
        end = start + nb
        assert end <= self.n, f"arena overflow {name}: {end} > {self.n}"
        self.off = end
        b = Buf(name)
        keep = []
        for (s, e, ob) in self.regions:
            if s < end and start < e:
                for w in ob.lw.values():
                    b.rd[("old", w.idx)] = w
                for r in ob.rd.values():
                    b.rd[("old", r.idx)] = r
                if s < start or e > end:
                    keep.append((s, e, ob))
            else:
                keep.append((s, e, ob))
        keep.append((start, end, b))
        self.regions = keep
        ap = self.t[0:parts, start:end].bitcast(dtype)
        return ap, b


IN_Q, IN_K, IN_V, IN_CQ, IN_CKV, IN_KR, IN_G = 0, 512, 1024, 1536, 1792, 1920, 1952


def build(T, depth, dbg=None, stop=None):
    NT = T // 128
    NG = T // 512
    NB = (2 * T + 32 * 127 + 127) // 128
    SLOT_T = NT // 4
    nc = bass.Bass("TRN2", target_bir_lowering=False)

    def din(name, shape, dt=F32):
        return nc.dram_tensor(name, list(shape), dt, kind="ExternalInput").ap()

    def dscr(name, shape, dt):
        kind = "ExternalOutput" if (dbg and name in dbg) else "Internal"
        return nc.dram_tensor(name, list(shape), dt, kind=kind).ap()

    x_in = din("x", [T, D])
    cT_in = din("cT", [128, 32])
    ada_w = din("ada_w", [depth, D, 6 * D])
    ada_b = din("ada_b", [depth, 6 * D])
    g_mix = din("norm_mix_g", [depth, D])
    g_ffn = din("norm_ffn_g", [depth, D])
    g_fin = din("final_norm_g", [1, D])
    w_in = din("w_in", [depth, D, 4000])
    g_q = din("mla_q_norm_g", [depth, 256])
    g_kv = din("mla_kv_norm_g", [depth, 128])
    w_uq = din("w_uq", [depth, 256, 768])
    w_uqr = din("w_uq_rot", [depth, 256, 768])
    w_ukvk = din("w_ukv_k", [depth, 128, 512])
    w_ukvv = din("w_ukv_v", [depth, 128, 512])
    tt_in = din("na_tt", [depth, 8, 128, 9 * 128])
    w_nao = din("w_na_o", [depth, 512, D])
    w_mlao = din("w_mla_o", [depth, 512, D])
    w_out = din("w_out", [depth, D, D])
    w_r = din("w_router", [depth, D, 36])
    b_r = din("b_router", [depth, 36])
    w1r = din("w1r", [depth * 32 * 128, 4096])
    w3r = din("w3r", [depth * 32 * 128, 4096])
    w2r = din("w2r", [depth * 32 * 128, 4096])
    identb_in = din("identb", [128, 128], BF16)
    identf_in = din("identf", [128, 128])
    tril_in = din("tril", [128, 128], BF16)
    ones_in = din("onesb", [128, 128], BF16)
    kseg_in = din("kseg", [4, T], BF16)
    qseg_in = din("qseg", [4, T], BF16)
    krow_in = din("krow", [32, T], BF16)
    qrow_in = din("qrow", [32, T], BF16)
    cosk_in = din("cosk", [128, NT * 16])
    sink_in = din("sink", [128, NT * 16])
    cosq_in = din("cosq", [32, T])
    sinq_in = din("sinq", [32, T])
    iotap_in = din("iotap", [128, 1])
    blk_in = din("blkstart", [128, NB])

    y_out = nc.dram_tensor("y", [T, D], F32, kind="ExternalOutput").ap()
    dbg_out = {}

    X = dscr("X", [T, D], F32)
    MODS = dscr("MODS", [depth, 4, 6 * D], F32)
    QA = dscr("QA", [8, 64, T], BF16)
    KA = dscr("KA", [8, 64, T], BF16)
    VNA = dscr("VNA", [T, 512], BF16)
    CQT = dscr("CQT", [256, T], BF16)
    CKVT = dscr("CKVT", [128, T], BF16)
    KRT = dscr("KRT", [32, T], BF16)
    GATE = dscr("GATE", [T, 2048], BF16)
    QM = dscr("QM", [8, 96, T], BF16)
    KM = dscr("KM", [8, 64, T], BF16)
    VM = dscr("VM", [8, T, 64], BF16)
    NAO = dscr("NAO", [T, 512], BF16)
    MLAO = dscr("MLAO", [T, 512], BF16)
    H2 = dscr("H2", [T, D], BF16)
    BUFD = dscr("BUFD", [NB * 128, D], BF16)
    YB = dscr("YB", [NB * 128, D], F32)

    def MB(n):
        return Buf(n, multi=True)
    XB = [MB(f"X{t}") for t in range(NT)]
    MODSB = MB("MODS")
    QAB, KAB, VNAB, CQTB, CKVTB, KRTB, GATEB = (MB(n) for n in ("QA", "KA", "VNA", "CQT", "CKVT", "KRT", "GATE"))
    QMB, KMB, VMB, NAOB, MLAOB, H2B, BUFDB, YBB = (MB(n) for n in ("QM", "KM", "VM", "NAO", "MLAO", "H2", "BUFD", "YB"))

    es = contextlib.ExitStack()
    ARN = 196 * 1024
    sb = es.enter_context(nc.sbuf_tensor("arena", [128, ARN], U8))
    psh = [es.enter_context(nc.psum_tensor(f"ps{i}", [128, 512], F32)) for i in range(8)]
    ps = [h[:, :] for h in psh]
    psbf = [p.bitcast(BF16) for p in ps]
    psb = [Buf(f"ps{i}") for i in range(8)]
    A = Arena(sb, ARN)
    S = Sched(nc)
    S.reg_requests["nb"] = NB * 128 - 1
    final_bufs = []

    def load(dst, dstB, src, reads=(), extra_writes=()):
        return S.dma("sp", lambda e: e.dma_start(out=dst, in_=src), dstB, reads=list(reads),
                     writes=[dstB] + list(extra_writes))

    def store(dst, src, srcB, dstB):
        return S.dma("sp", lambda e: e.dma_start(out=dst, in_=src), srcB, reads=[srcB], writes=[dstB])

    identb, identbB = A.alloc("identb", 128, BF16)
    identf, identfB = A.alloc("identf", 128, F32)
    tril, trilB = A.alloc("tril", 128, BF16)
    onesb, onesbB = A.alloc("onesb", 128, BF16)
    scT, scTB = A.alloc("scT", 32, F32)
    iotap, iotapB = A.alloc("iotap", 1, F32)
    load(identb, identbB, identb_in)
    load(identf, identfB, identf_in)
    load(tril, trilB, tril_in)
    load(onesb, onesbB, ones_in)
    load(iotap, iotapB, iotap_in)
    load(scT, scTB, cT_in)
    S.op("act", lambda e: e.activation(out=scT, in_=scT, func=AF.Silu), reads=[scTB], writes=[scTB])
    scT3 = scT.rearrange("p (k s) -> p k s", k=8)
    PERSIST = A.mark()

    def rms_rstd(ssum, ssB, n, rstd, rstdB, width=1):
        S.op("dve", lambda e: e.tensor_scalar(out=rstd, in0=ssum, scalar1=1.0 / n, scalar2=EPS,
                                              op0=ALU.mult, op1=ALU.add), reads=[ssB], writes=[rstdB])
        S.op("act", lambda e: e.activation(out=rstd, in_=rstd, func=AF.Sqrt), reads=[rstdB], writes=[rstdB])
        S.op("dve", lambda e: e.reciprocal(out=rstd, in_=rstd), reads=[rstdB], writes=[rstdB])

    def bload(dst, dstB, row_ap, parts=128, reads=()):
        n = row_ap.shape[-1]
        return S.dma("sp", lambda e: e.dma_start(out=dst, in_=row_ap.broadcast_to([parts, n])), dstB,
                     reads=list(reads), writes=[dstB])

    def phase_mod(l):
        A.reset(PERSIST)
        mods, modsB = A.alloc("mods", 6 * D, F32, parts=4)
        adab, adabB = A.alloc("adab", 6 * D, F32, parts=4)
        aw = [A.alloc(f"aw{i}", 8 * 512, F32) for i in range(2)]
        bload(adab, adabB, ada_b[l:l + 1, :], parts=4)
        awsrc = ada_w[l].rearrange("(k p) n -> p k n", p=128)
        for blk in range(12):
            awt, awtB = aw[blk % 2]
            awt3 = awt.rearrange("p (k n) -> p k n", k=8)
            load(awt3, awtB, awsrc[:, :, blk * 512:(blk + 1) * 512])
            bk = 6 + blk % 2
            for k in range(8):
                S.op("pe", lambda e, k=k, awt3=awt3, bk=bk: e.matmul(
                    ps[bk][0:4, :], lhsT=scT3[:, k, :], rhs=awt3[:, k, :], start=(k == 0), stop=(k == 7)),
                    reads=[scTB, awtB], writes=[psb[bk]])
            S.op("dve", lambda e, blk=blk, bk=bk: e.tensor_tensor(
                out=mods[:, blk * 512:(blk + 1) * 512], in0=ps[bk][0:4, :],
                in1=adab[:, blk * 512:(blk + 1) * 512], op=ALU.add),
                reads=[psb[bk], adabB], writes=[modsB])
        store(MODS[l], mods, modsB, MODSB)

    def mod_row(l, s, j):
        return MODS[l, s:s + 1, j * D:(j + 1) * D]

    def phase_a(l):
        A.reset(PERSIST)
        win, winB = A.alloc("win", 8 * 4000, BF16)
        win3 = win.rearrange("p (k n) -> p k n", k=8)
        wst = [A.alloc(f"wst{i}", 8 * 500, F32) for i in range(2)]
        wsrc = w_in[l].rearrange("(k p) n -> p k n", p=128)
        for blk in range(8):
            st, stB = wst[blk % 2]
            st3 = st.rearrange("p (k n) -> p k n", k=8)
            load(st3, stB, wsrc[:, :, blk * 500:(blk + 1) * 500])
            S.op("pool", lambda e, st3=st3, blk=blk: e.tensor_copy(
                out=win3[:, :, blk * 500:(blk + 1) * 500], in_=st3), reads=[stB], writes=[winB])
        gq, gqB = A.alloc("gq", 256, F32)
        gkv, gkvB = A.alloc("gkv", 128, F32)
        bload(gq, gqB, g_q[l:l + 1, :])
        bload(gkv, gkvB, g_kv[l:l + 1, :])
        gm, gmB = A.alloc("gm", D, F32)
        bload(gm, gmB, g_mix[l:l + 1, :])
        cosk, coskB = A.alloc("cosk", NT * 16, F32)
        sink, sinkB = A.alloc("sink", NT * 16, F32)
        load(cosk, coskB, cosk_in)
        load(sink, sinkB, sink_in)
        G1, G1B = A.alloc("G1", D, F32)
        SH1, SH1B = A.alloc("SH1", D, F32)
        xt = [A.alloc(f"xt{i}", D, F32) for i in range(2)]
        junk, junkB = A.alloc("junk", D, F32)
        tmp, tmpB = A.alloc("tmp", D, F32)
        hb = [A.alloc(f"hb{i}", D, BF16) for i in range(2)]
        hT = [A.alloc(f"hT{i}", 8 * 512, BF16) for i in range(2)]
        ss, ssB = A.alloc("ss", 4, F32)
        rstd, rstdB = A.alloc("rstd", 4, F32)
        qst = [A.alloc(f"qst{i}", 512, BF16) for i in range(2)]
        vst = [A.alloc(f"vst{i}", 512, BF16) for i in range(2)]
        gst = [A.alloc(f"gst{i}", 2048, BF16) for i in range(2)]
        latb, latbB = A.alloc("latb", 416, BF16)
        rt, rtB = A.alloc("rt", 64, F32)
        latT = [A.alloc(f"latT{i}", 4 * 512, BF16) for i in range(2)]
        tokbank = [3, 4, 5]
        tokctr = [0]
        fmctr = [0]

        def nexttok():
            b = tokbank[tokctr[0] % 3]
            tokctr[0] += 1
            return b

        for g in range(NG):
            hTg, hTgB = hT[g % 2]
            hT3 = hTg.rearrange("p (k t) -> p k t", k=8)
            lT, lTB = latT[g % 2]
            lT3 = lT.rearrange("p (c t) -> p c t", c=4)
            for j in range(4):
                t = 4 * g + j
                if t % SLOT_T == 0:
                    s = t // SLOT_T
                    bload(G1, G1B, mod_row(l, s, 1), reads=[MODSB])
                    bload(SH1, SH1B, mod_row(l, s, 0), reads=[MODSB])
                    S.op("dve", lambda e: e.scalar_tensor_tensor(
                        out=G1, in0=G1, scalar=1.0, in1=gm, op0=ALU.add, op1=ALU.mult),
                        reads=[G1B, gmB], writes=[G1B])
                xti, xtiB = xt[t % 2]
                src = x_in if l == 0 else X
                load(xti, xtiB, src[t * 128:(t + 1) * 128, :], reads=([] if l == 0 else [XB[t]]))
                S.op("act", lambda e, xti=xti: e.activation(out=junk, in_=xti, func=AF.Square,
                                                            accum_out=ss[:, 0:1]),
                     reads=[xtiB], writes=[junkB, ssB])
                rms_rstd(ss[:, 0:1], ssB, D, rstd[:, 0:1], rstdB)
                S.op("dve", lambda e, xti=xti: e.scalar_tensor_tensor(
                    out=tmp, in0=xti, scalar=rstd[:, 0:1], in1=G1, op0=ALU.mult, op1=ALU.mult),
                    reads=[xtiB, rstdB, G1B], writes=[tmpB])
                hbi, hbiB = hb[t % 2]
                S.op("pool", lambda e, hbi=hbi: e.tensor_tensor(out=hbi, in0=tmp, in1=SH1, op=ALU.add),
                     reads=[tmpB, SH1B], writes=[hbiB])
                for k in range(8):
                    S.op("pe", lambda e, k=k, hbi=hbi: e.transpose(
                        out=psbf[0][:, k * 128:(k + 1) * 128], in_=hbi[:, k * 128:(k + 1) * 128],
                        identity=identb), reads=[hbiB, identbB], writes=[psb[0]])
                S.op("act", lambda e, j=j, hT3=hT3: e.copy(
                    out=hT3[:, :, j * 128:(j + 1) * 128],
                    in_=psbf[0].rearrange("p (k t) -> p k t", k=8)), reads=[psb[0]], writes=[hTgB])
                bk = nexttok()
                for k in range(8):
                    S.op("pe", lambda e, k=k, j=j, hT3=hT3, bk=bk: e.matmul(
                        ps[bk][:, :], lhsT=hT3[:, k, j * 128:(j + 1) * 128], rhs=win3[:, k, IN_V:IN_V + 512],
                        start=(k == 0), stop=(k == 7)), reads=[hTgB, winB], writes=[psb[bk]])
                vs, vsB = vst[t % 2]
                S.op("dve", lambda e, vs=vs, bk=bk: e.tensor_copy(out=vs, in_=ps[bk][:, :]),
                     reads=[psb[bk]], writes=[vsB])
                store(VNA[t * 128:(t + 1) * 128, :], vs, vsB, VNAB)
                gs, gsB = gst[t % 2]
                for q4 in range(4):
                    bk = nexttok()
                    for k in range(8):
                        S.op("pe", lambda e, k=k, j=j, hT3=hT3, bk=bk, q4=q4: e.matmul(
                            ps[bk][:, :], lhsT=hT3[:, k, j * 128:(j + 1) * 128],
                            rhs=win3[:, k, IN_G + q4 * 512:IN_G + (q4 + 1) * 512],
                            start=(k == 0), stop=(k == 7)), reads=[hTgB, winB], writes=[psb[bk]])
                    S.op("act", lambda e, gs=gs, bk=bk, q4=q4: e.activation(
                        out=gs[:, q4 * 512:(q4 + 1) * 512], in_=ps[bk][:, :], func=AF.Sigmoid),
                        reads=[psb[bk]], writes=[gsB])
                store(GATE[t * 128:(t + 1) * 128, :], gs, gsB, GATEB)
                bk = nexttok()
                for k in range(8):
                    S.op("pe", lambda e, k=k, j=j, hT3=hT3, bk=bk: e.matmul(
                        ps[bk][:, 0:416], lhsT=hT3[:, k, j * 128:(j + 1) * 128], rhs=win3[:, k, IN_CQ:IN_CQ + 416],
                        start=(k == 0), stop=(k == 7)), reads=[hTgB, winB], writes=[psb[bk]])
                lat = ps[bk]
                S.op("act", lambda e, lat=lat: e.activation(out=junk[:, 0:256], in_=lat[:, 0:256], func=AF.Square,
                                                            accum_out=ss[:, 1:2]),
                     reads=[psb[bk]], writes=[junkB, ssB])
                S.op("act", lambda e, lat=lat: e.activation(out=junk[:, 256:384], in_=lat[:, 256:384],
                                                            func=AF.Square, accum_out=ss[:, 2:3]),
                     reads=[psb[bk]], writes=[junkB, ssB])
                rms_rstd(ss[:, 1:2], ssB, 256, rstd[:, 1:2], rstdB)
                rms_rstd(ss[:, 2:3], ssB, 128, rstd[:, 2:3], rstdB)
                S.op("dve", lambda e, lat=lat: e.scalar_tensor_tensor(
                    out=latb[:, 0:256], in0=lat[:, 0:256], scalar=rstd[:, 1:2], in1=gq,
                    op0=ALU.mult, op1=ALU.mult), reads=[psb[bk], rstdB, gqB], writes=[latbB])
                S.op("dve", lambda e, lat=lat: e.scalar_tensor_tensor(
                    out=latb[:, 256:384], in0=lat[:, 256:384], scalar=rstd[:, 2:3], in1=gkv,
                    op0=ALU.mult, op1=ALU.mult), reads=[psb[bk], rstdB, gkvB], writes=[latbB])
                ck = cosk[:, t * 16:(t + 1) * 16]
                sk = sink[:, t * 16:(t + 1) * 16]
                x1 = lat[:, 384:400]
                x2 = lat[:, 400:416]
                for (o_, a_, b_) in ((0, x1, ck), (16, x2, sk), (32, x1, sk), (48, x2, ck)):
                    S.op("dve", lambda e, o_=o_, a_=a_, b_=b_: e.tensor_tensor(
                        out=rt[:, o_:o_ + 16], in0=a_, in1=b_, op=ALU.mult),
                        reads=[psb[bk], coskB, sinkB], writes=[rtB])
                S.op("dve", lambda e: e.tensor_tensor(out=latb[:, 384:400], in0=rt[:, 0:16], in1=rt[:, 16:32],
                                                      op=ALU.subtract), reads=[rtB], writes=[latbB])
                S.op("dve", lambda e: e.tensor_tensor(out=latb[:, 400:416], in0=rt[:, 32:48], in1=rt[:, 48:64],
                                                      op=ALU.add), reads=[rtB], writes=[latbB])
                for c4 in range(3):
                    S.op("pe", lambda e, c4=c4: e.transpose(
                        out=psbf[1][:, c4 * 128:(c4 + 1) * 128], in_=latb[:, c4 * 128:(c4 + 1) * 128],
                        identity=identb), reads=[latbB, identbB], writes=[psb[1]])
                S.op("pe", lambda e: e.transpose(out=psbf[1][0:32, 384:512], in_=latb[:, 384:416], identity=identb),
                     reads=[latbB, identbB], writes=[psb[1]])
                S.op("act", lambda e, j=j, lT3=lT3: e.copy(
                    out=lT3[:, 0:3, j * 128:(j + 1) * 128],
                    in_=psbf[1][:, 0:384].rearrange("p (c t) -> p c t", c=3)), reads=[psb[1]], writes=[lTB])
                S.op("act", lambda e, j=j, lT3=lT3: e.copy(
                    out=lT3[0:32, 3, j * 128:(j + 1) * 128], in_=psbf[1][0:32, 384:512]),
                    reads=[psb[1]], writes=[lTB])
            gsl = slice(g * 512, (g + 1) * 512)
            for which, col0, scale, dst, dstB in ((0, IN_Q, 0.125, QA, QAB), (1, IN_K, 1.0, KA, KAB)):
                for m in range(4):
                    bk = 6 + fmctr[0] % 2
                    fmctr[0] += 1
                    for k in range(8):
                        S.op("pe", lambda e, k=k, m=m, bk=bk, col0=col0, hT3=hT3: e.matmul(
                            ps[bk][:, :], lhsT=win3[:, k, col0 + m * 128:col0 + (m + 1) * 128], rhs=hT3[:, k, :],
                            start=(k == 0), stop=(k == 7)), reads=[hTgB, winB], writes=[psb[bk]])
                    qs, qsB = qst[fmctr[0] % 2]
                    S.op("act", lambda e, qs=qs, bk=bk, scale=scale: e.activation(
                        out=qs, in_=ps[bk][:, :], func=AF.Copy, scale=scale), reads=[psb[bk]], writes=[qsB])
                    store(dst[2 * m:2 * m + 2, :, gsl].rearrange("h d t -> (h d) t"), qs, qsB, dstB)
            store(CQT[0:128, gsl], lT3[:, 0, :], lTB, CQTB)
            store(CQT[128:256, gsl], lT3[:, 1, :], lTB, CQTB)
            store(CKVT[:, gsl], lT3[:, 2, :], lTB, CKVTB)
            store(KRT[:, gsl], lT3[0:32, 3, :], lTB, KRTB)

    def phase_c1(l):
        A.reset(PERSIST)
        cq, cqB = A.alloc("cq", 2 * T, BF16)
        cq3 = cq.rearrange("p (c t) -> p c t", c=2)
        ckv, ckvB = A.alloc("ckv", T, BF16)
        load(cq3[:, 0, :], cqB, CQT[0:128, :], reads=[CQTB])
        load(cq3[:, 1, :], cqB, CQT[128:256, :], reads=[CQTB])
        load(ckv, ckvB, CKVT, reads=[CKVTB])
        wst, wstB = A.alloc("wst", 2 * 768, F32)
        wq, wqB = A.alloc("wq", 2 * 768, BF16)
        wqr, wqrB = A.alloc("wqr", 2 * 768, BF16)
        wk, wkB = A.alloc("wk", 512, BF16)
        wv, wvB = A.alloc("wv", 512, BF16)
        wst3 = wst.rearrange("p (k n) -> p k n", k=2)
        wq3 = wq.rearrange("p (k n) -> p k n", k=2)
        wqr3 = wqr.rearrange("p (k n) -> p k n", k=2)
        load(wst3, wstB, w_uq[l].rearrange("(k p) n -> p k n", p=128))
        S.op("dve", lambda e: e.tensor_copy(out=wq, in_=wst), reads=[wstB], writes=[wqB])
        load(wst3, wstB, w_uqr[l].rearrange("(k p) n -> p k n", p=128))
        S.op("dve", lambda e: e.tensor_copy(out=wqr, in_=wst), reads=[wstB], writes=[wqrB])
        load(wst[:, 0:512], wstB, w_ukvk[l])
        S.op("dve", lambda e: e.tensor_copy(out=wk, in_=wst[:, 0:512]), reads=[wstB], writes=[wkB])
        load(wst[:, 0:512], wstB, w_ukvv[l])
        S.op("dve", lambda e: e.tensor_copy(out=wv, in_=wst[:, 0:512]), reads=[wstB], writes=[wvB])
        cs = [A.alloc(f"cs{i}", 512, F32, parts=96) for i in range(2)]
        sn = [A.alloc(f"sn{i}", 512, F32, parts=96) for i in range(2)]
        qst = [A.alloc(f"qst{i}", 512, BF16, parts=96) for i in range(2)]
        kst = [A.alloc(f"kst{i}", 512, BF16, parts=64) for i in range(2)]
        vst = [A.alloc(f"vst{i}", 512, BF16) for i in range(2)]
        t1, t1B = A.alloc("t1", 512, F32, parts=96)
        t2, t2B = A.alloc("t2", 512, F32, parts=96)
        ctr = 0
        for g in range(NG):
            gsl = slice(g * 512, (g + 1) * 512)
            csg, csgB = cs[g % 2]
            sng, sngB = sn[g % 2]
            load(csg[64:96, :], csgB, cosq_in[:, gsl])
            load(sng[64:96, :], sngB, sinq_in[:, gsl])
            for h in range(8):
                ba, bb, bk_ = 0 + 3 * (ctr % 2), 1 + 3 * (ctr % 2), 2 + 3 * (ctr % 2)
                for k in range(2):
                    S.op("pe", lambda e, k=k, h=h, ba=ba, gsl=gsl: e.matmul(
                        ps[ba][0:96, :], lhsT=wq3[:, k, h * 96:(h + 1) * 96], rhs=cq3[:, k, gsl],
                        start=(k == 0), stop=(k == 1)), reads=[wqB, cqB], writes=[psb[ba]])
                for k in range(2):
                    S.op("pe", lambda e, k=k, h=h, bb=bb, gsl=gsl: e.matmul(
                        ps[bb][0:96, :], lhsT=wqr3[:, k, h * 96:(h + 1) * 96], rhs=cq3[:, k, gsl],
                        start=(k == 0), stop=(k == 1)), reads=[wqrB, cqB], writes=[psb[bb]])
                S.op("pe", lambda e, h=h, bk_=bk_, gsl=gsl: e.matmul(
                    ps[bk_][0:64, :], lhsT=wk[:, h * 64:(h + 1) * 64], rhs=ckv[:, gsl], start=True, stop=True),
                    reads=[wkB, ckvB], writes=[psb[bk_]])
                qs, qsB = qst[ctr % 2]
                ks, ksB = kst[ctr % 2]
                S.op("act", lambda e, qs=qs, ba=ba: e.copy(out=qs[0:64, :], in_=ps[ba][0:64, :]),
                     reads=[psb[ba]], writes=[qsB])
                S.op("dve", lambda e, ba=ba, csg=csg: e.tensor_tensor(
                    out=t1[64:96, :], in0=ps[ba][64:96, :], in1=csg[64:96, :], op=ALU.mult),
                    reads=[psb[ba], csgB], writes=[t1B])
                S.op("dve", lambda e, bb=bb, sng=sng: e.tensor_tensor(
                    out=t2[64:96, :], in0=ps[bb][64:96, :], in1=sng[64:96, :], op=ALU.mult),
                    reads=[psb[bb], sngB], writes=[t2B])
                S.op("dve", lambda e, qs=qs: e.tensor_tensor(
                    out=qs[64:96, :], in0=t1[64:96, :], in1=t2[64:96, :], op=ALU.add),
                    reads=[t1B, t2B], writes=[qsB])
                S.op("act", lambda e, ks=ks, bk_=bk_: e.copy(out=ks, in_=ps[bk_][0:64, :]),
                     reads=[psb[bk_]], writes=[ksB])
                store(QM[h, :, gsl], qs, qsB, QMB)
                store(KM[h, :, gsl], ks, ksB, KMB)
                ctr += 1
        for t in range(NT):
            bk = 6 + t % 2
            S.op("pe", lambda e, t=t, bk=bk: e.matmul(
                ps[bk][:, :], lhsT=ckv[:, t * 128:(t + 1) * 128], rhs=wv, start=True, stop=True),
                reads=[ckvB, wvB], writes=[psb[bk]])
            vs, vsB = vst[t % 2]
            S.op("dve", lambda e, vs=vs, bk=bk: e.tensor_copy(out=vs, in_=ps[bk][:, :]),
                 reads=[psb[bk]], writes=[vsB])
            store(VM[:, t * 128:(t + 1) * 128, :].rearrange("h t d -> t h d"),
                  vs.rearrange("p (h d) -> p h d", h=8), vsB, VMB)

    def phase_c2(l, QT, KT, first):
        A.reset(ATT_MARK)
        V = [A.alloc(f"V{i}", NT * 65, BF16) for i in range(2)]
        PT = [A.alloc(f"PT{i}", 512, BF16) for i in range(3)]
        otf, otfB = A.alloc("otf", 512, F32, parts=65)
        rc, rcB = A.alloc("rc", 4, F32)
        mo = [A.alloc(f"mo{i}", 4 * 64, BF16) for i in range(2)]
        for i in range(2):
            Vi, ViB = V[i]
            S.op("pool", lambda e, Vi=Vi: e.memset(Vi, 1.0), writes=[ViB])
        sc = 96.0 ** -0.5
        for i in range(2):
            load(KT[i][0][64:96, :], KT[i][1], KRT, reads=[KRTB])
        pctr = 0
        octr = 0
        for h in range(8):
            QTh, QThB = QT[h % 2]
            KTh, KThB = KT[h % 2]
            Vh, VhB = V[h % 2]
            Vh3 = Vh.rearrange("p (t d) -> p t d", d=65)
            load(QTh[0:96, :], QThB, QM[h], reads=[QMB])
            load(KTh[0:64, :], KThB, KM[h], reads=[KMB])
            S.dma("sp", lambda e, Vh3=Vh3, h=h: e.dma_start(
                out=Vh3[:, :, 0:64], in_=VM[h].rearrange("(t p) d -> p t d", p=128)), VhB,
                reads=[VMB], writes=[VhB])
            for g in range(NG):
                gsl = slice(g * 512, (g + 1) * 512)
                ob = 6 + octr % 2
                octr += 1
                pend = None
                for kt in range(NT + 1):
                    if kt < NT:
                        sbk = pctr % 3
                        pt, ptB = PT[pctr % 3]
                        pctr += 1
                        S.op("pe", lambda e, kt=kt, sbk=sbk, KTh=KTh, QTh=QTh, gsl=gsl: e.matmul(
                            ps[sbk][:, :], lhsT=KTh[0:100, kt * 128:(kt + 1) * 128], rhs=QTh[0:100, gsl],
                            start=True, stop=True), reads=[KThB, QThB], writes=[psb[sbk]])
                        S.op("act", lambda e, pt=pt, sbk=sbk: e.activation(
                            out=pt, in_=ps[sbk][:, :], func=AF.Exp, scale=sc), reads=[psb[sbk]], writes=[ptB])
                    if pend is not None:
                        pkt, ppt, pptB = pend
                        S.op("pe", lambda e, pkt=pkt, ppt=ppt, Vh3=Vh3, ob=ob: e.matmul(
                            ps[ob][0:65, :], lhsT=Vh3[:, pkt, :], rhs=ppt, start=(pkt == 0), stop=(pkt == NT - 1)),
                            reads=[VhB, pptB], writes=[psb[ob]])
                    pend = (kt, pt, ptB) if kt < NT else None
                S.op("dve", lambda e, ob=ob: e.tensor_copy(out=otf, in_=ps[ob][0:65, :]),
                     reads=[psb[ob]], writes=[otfB])
                for j in range(4):
                    S.op("pe", lambda e, j=j: e.transpose(
                        out=ps[5][:, j * 65:(j + 1) * 65], in_=otf[:, j * 128:(j + 1) * 128],
                        identity=identf[0:65, 0:65]), reads=[otfB, identfB], writes=[psb[5]])
                p5 = ps[5][:, 0:260].rearrange("p (j d) -> p j d", d=65)
                S.op("dve", lambda e, p5=p5: e.reciprocal(out=rc, in_=p5[:, :, 64]), reads=[psb[5]], writes=[rcB])
                mg, mgB = mo[octr % 2]
                mg3 = mg.rearrange("p (j d) -> p j d", d=64)
                S.op("dve", lambda e, p5=p5, mg3=mg3: e.tensor_tensor(
                    out=mg3, in0=p5[:, :, 0:64], in1=rc.unsqueeze(2).broadcast_to([128, 4, 64]), op=ALU.mult),
                    reads=[psb[5], rcB], writes=[mgB])
                store(MLAO[gsl, h * 64:(h + 1) * 64].rearrange("(j p) d -> p j d", p=128), mg3, mgB, MLAOB)

    def phase_b(l, QN, KN):
        A.reset(ATT_MARK)
        VN = [A.alloc(f"VN{i}", NT * 65, BF16) for i in range(2)]
        ttf, ttfB = A.alloc("ttf", 9 * 128, F32)
        ttb = [A.alloc(f"ttb{i}", 9 * 128, BF16) for i in range(2)]
        PN = [A.alloc(f"PN{i}", 9 * 128, BF16) for i in range(2)]
        rc, rcB = A.alloc("rcn", 1, F32)
        no = [A.alloc(f"no{i}", 4 * 64, BF16) for i in range(2)]
        for i in range(2):
            Vi, ViB = VN[i]
            S.op("pool", lambda e, Vi=Vi: e.memset(Vi, 1.0), writes=[ViB])
        pctr = 0
        for h in range(8):
            QNh, QNhB = QN[h % 2]
            KNh, KNhB = KN[h % 2]
            Vh, VhB = VN[h % 2]
            Vh3 = Vh.rearrange("p (t d) -> p t d", d=65)
            tb, tbB = ttb[h % 2]
            tb3 = tb.rearrange("p (d q) -> p d q", d=9)
            load(QNh[0:64, :], QNhB, QA[h], reads=[QAB])
            load(KNh[0:64, :], KNhB, KA[h], reads=[KAB])
            S.dma("sp", lambda e, Vh3=Vh3, h=h: e.dma_start(
                out=Vh3[:, :, 0:64], in_=VNA[:, h * 64:(h + 1) * 64].rearrange("(t p) d -> p t d", p=128)), VhB,
                reads=[VNAB], writes=[VhB])
            load(ttf, ttfB, tt_in[l, h])
            S.op("pool", lambda e, tb=tb: e.tensor_copy(out=tb, in_=ttf), reads=[ttfB], writes=[tbB])
            for p in range(NT):
                tiles = [a for a in range(p - 4, p + 5) if 0 <= a < NT]
                pn, pnB = PN[pctr % 2]
                sb0 = 0 + 3 * (pctr % 2)
                ob = 6 + pctr % 2
                pctr += 1
                psl = slice(p * 128, (p + 1) * 128)
                for i, a in enumerate(tiles):
                    bk = sb0 + i // 4
                    csl = slice((i % 4) * 128, (i % 4 + 1) * 128)
                    S.op("pe", lambda e, a=a, bk=bk, csl=csl, KNh=KNh, QNh=QNh, psl=psl: e.matmul(
                        ps[bk][:, csl], lhsT=KNh[0:96, a * 128:(a + 1) * 128], rhs=QNh[0:96, psl],
                        start=True, stop=False), reads=[KNhB, QNhB], writes=[psb[bk]])
                    S.op("pe", lambda e, a=a, bk=bk, csl=csl, tb3=tb3, p=p: e.matmul(
                        ps[bk][:, csl], lhsT=identb, rhs=tb3[:, a - p + 4, :], start=False, stop=True),
                        reads=[identbB, tbB], writes=[psb[bk]])
                nt_ = len(tiles)
                for b0 in range(0, nt_, 4):
                    n4 = min(4, nt_ - b0)
                    bk = sb0 + b0 // 4
                    S.op("act", lambda e, pn=pn, bk=bk, b0=b0, n4=n4: e.activation(
                        out=pn[:, b0 * 128:(b0 + n4) * 128], in_=ps[bk][:, 0:n4 * 128], func=AF.Exp),
                        reads=[psb[bk]], writes=[pnB])
                for i, a in enumerate(tiles):
                    S.op("pe", lambda e, i=i, a=a, pn=pn, Vh3=Vh3, ob=ob, nt_=nt_: e.matmul(
                        ps[ob][:, 0:65], lhsT=pn[:, i * 128:(i + 1) * 128], rhs=Vh3[:, a, :],
                        start=(i == 0), stop=(i == nt_ - 1)), reads=[pnB, VhB], writes=[psb[ob]])
                S.op("dve", lambda e, ob=ob: e.reciprocal(out=rc, in_=ps[ob][:, 64:65]),
                     reads=[psb[ob]], writes=[rcB])
                ng, ngB = no[(p // 4) % 2]
                S.op("dve", lambda e, ob=ob, ng=ng, p=p: e.tensor_scalar(
                    out=ng[:, (p % 4) * 64:(p % 4 + 1) * 64], in0=ps[ob][:, 0:64], scalar1=rc[:, 0:1],
                    scalar2=None, op0=ALU.mult), reads=[psb[ob], rcB], writes=[ngB])
                if p % 4 == 3:
                    p0 = p - 3
                    store(NAO[p0 * 128:(p0 + 4) * 128, h * 64:(h + 1) * 64].rearrange("(j p) d -> p j d", p=128),
                          ng.rearrange("p (j d) -> p j d", d=64), ngB, NAOB)

    def load_w_bf16(dst3, dstB, src3, st, stB, ncols, k, eng="pool"):
        for c0 in range(0, ncols, 512):
            cw = min(512, ncols - c0)
            st3 = st[:, 0:k * cw].rearrange("p (k n) -> p k n", k=k)
            load(st3, stB, src3[:, :, c0:c0 + cw])
            S.op(eng, lambda e, st3=st3, c0=c0, cw=cw: e.tensor_copy(out=dst3[:, :, c0:c0 + cw], in_=st3),
                 reads=[stB], writes=[dstB])

    def phase_de(l, R):
        A.reset(MOE_MARK)
        st, stB = A.alloc("st", 8 * 512, F32)
        wna, wnaB = A.alloc("wna", 4 * D, BF16)
        wml, wmlB = A.alloc("wml", 4 * D, BF16)
        wo, woB = A.alloc("wo", 8 * D, BF16)
        wr, wrB = A.alloc("wr", 8 * 36, BF16)
        wna3 = wna.rearrange("p (k n) -> p k n", k=4)
        wml3 = wml.rearrange("p (k n) -> p k n", k=4)
        wo3 = wo.rearrange("p (k n) -> p k n", k=8)
        wr3 = wr.rearrange("p (k n) -> p k n", k=8)
        load_w_bf16(wna3, wnaB, w_nao[l].rearrange("(k p) n -> p k n", p=128), st, stB, D, 4)
        load_w_bf16(wml3, wmlB, w_mlao[l].rearrange("(k p) n -> p k n", p=128), st, stB, D, 4)
        load_w_bf16(wo3, woB, w_out[l].rearrange("(k p) n -> p k n", p=128), st, stB, D, 8)
        load_w_bf16(wr3, wrB, w_r[l].rearrange("(k p) n -> p k n", p=128), st, stB, 36, 8)
        brt, brtB = A.alloc("brt", 36, F32)
        bload(brt, brtB, b_r[l:l + 1, :])
        gf, gfB = A.alloc("gf", D, F32)
        bload(gf, gfB, g_ffn[l:l + 1, :])
        g1b, g1bB = A.alloc("g1b", D, F32)
        G2, G2B = A.alloc("G2", D, F32)
        SH2, SH2B = A.alloc("SH2", D, F32)
        ao = [A.alloc(f"ao{i}", 1024, BF16) for i in range(2)]
        gt = [A.alloc(f"gt{i}", 2048, BF16) for i in range(2)]
        xt = [A.alloc(f"xd{i}", D, F32) for i in range(2)]
        aT, aTB = A.alloc("aT", 8 * 128, BF16)
        m1, m1B = A.alloc("m1", D, F32)
        m2, m2B = A.alloc("m2", D, F32)
        mgb, mgbB = A.alloc("mgb", D, BF16)
        mT, mTB = A.alloc("mT", 8 * 128, BF16)
        xn, xnB = A.alloc("xn", D, F32)
        junk, junkB = A.alloc("junkd", D, F32)
        ss, ssB = A.alloc("ssd", 1, F32)
        rstd, rstdB = A.alloc("rstdd", 1, F32)
        h2b = [A.alloc(f"h2b{i}", D, BF16) for i in range(2)]
        h2T, h2TB = A.alloc("h2T", 8 * 128, BF16)
        sm, smB = A.alloc("sm", 256, F32)
        ab, abB = A.alloc("ab", 32, BF16)
        S.op("dve", lambda e: e.memset(R["carry"], 0.0), writes=[R["carryB"]])
        for t in range(NT):
            tsl = slice(t * 128, (t + 1) * 128)
            if t % SLOT_T == 0:
                s = t // SLOT_T
                bload(g1b, g1bB, mod_row(l, s, 2), reads=[MODSB])
                bload(G2, G2B, mod_row(l, s, 4), reads=[MODSB])
                bload(SH2, SH2B, mod_row(l, s, 3), reads=[MODSB])
                S.op("dve", lambda e: e.scalar_tensor_tensor(
                    out=G2, in0=G2, scalar=1.0, in1=gf, op0=ALU.add, op1=ALU.mult),
                    reads=[G2B, gfB], writes=[G2B])
            aoi, aoiB = ao[t % 2]
            gti, gtiB = gt[t % 2]
            xti, xtiB = xt[t % 2]
            load(aoi[:, 0:512], aoiB, NAO[tsl, :], reads=[NAOB])
            load(aoi[:, 512:1024], aoiB, MLAO[tsl, :], reads=[MLAOB])
            load(gti, gtiB, GATE[tsl, :], reads=[GATEB])
            src = x_in if l == 0 else X
            load(xti, xtiB, src[tsl, :], reads=([] if l == 0 else [XB[t]]))
            for k in range(8):
                S.op("pe", lambda e, k=k, aoi=aoi: e.transpose(
                    out=psbf[0][:, k * 128:(k + 1) * 128], in_=aoi[:, k * 128:(k + 1) * 128], identity=identb),
                    reads=[aoiB, identbB], writes=[psb[0]])
            S.op("act", lambda e: e.copy(out=aT, in_=psbf[0]), reads=[psb[0]], writes=[aTB])
            aT3 = aT.rearrange("p (k t) -> p k t", k=8)
            for hf in range(2):
                for (w3_, wB_, bk, k0) in ((wna3, wnaB, 1 + hf, 0), (wml3, wmlB, 3 + hf, 4)):
                    for k in range(4):
                        S.op("pe", lambda e, k=k, w3_=w3_, bk=bk, k0=k0, hf=hf: e.matmul(
                            ps[bk][:, :], lhsT=aT3[:, k0 + k, :], rhs=w3_[:, k, hf * 512:(hf + 1) * 512],
                            start=(k == 0), stop=(k == 3)), reads=[aTB, wB_], writes=[psb[bk]])
                hs = slice(hf * 512, (hf + 1) * 512)
                S.op("dve", lambda e, hf=hf, hs=hs, gti=gti: e.tensor_tensor(
                    out=m1[:, hs], in0=ps[1 + hf][:, :], in1=gti[:, hs], op=ALU.mult),
                    reads=[psb[1 + hf], gtiB], writes=[m1B])
                S.op("dve", lambda e, hf=hf, hs=hs, gti=gti: e.tensor_tensor(
                    out=m2[:, hs], in0=ps[3 + hf][:, :], in1=gti[:, 1024 + hf * 512:1024 + (hf + 1) * 512],
                    op=ALU.mult), reads=[psb[3 + hf], gtiB], writes=[m2B])
            S.op("pool", lambda e: e.tensor_tensor(out=mgb, in0=m1, in1=m2, op=ALU.add),
                 reads=[m1B, m2B], writes=[mgbB])
            for k in range(8):
                S.op("pe", lambda e, k=k: e.transpose(
                    out=psbf[5][:, k * 128:(k + 1) * 128], in_=mgb[:, k * 128:(k + 1) * 128], identity=identb),
                    reads=[mgbB, identbB], writes=[psb[5]])
            S.op("act", lambda e: e.copy(out=mT, in_=psbf[5]), reads=[psb[5]], writes=[mTB])
            mT3 = mT.rearrange("p (k t) -> p k t", k=8)
            for hf in range(2):
                bk = 6 + hf
                for k in range(8):
                    S.op("pe", lambda e, k=k, bk=bk, hf=hf: e.matmul(
                        ps[bk][:, :], lhsT=mT3[:, k, :], rhs=wo3[:, k, hf * 512:(hf + 1) * 512],
                        start=(k == 0), stop=(k == 7)), reads=[mTB, woB], writes=[psb[bk]])
                hs = slice(hf * 512, (hf + 1) * 512)
                S.op("dve", lambda e, bk=bk, hs=hs: e.tensor_tensor(
                    out=m1[:, hs], in0=ps[bk][:, :], in1=g1b[:, hs], op=ALU.mult),
                    reads=[psb[bk], g1bB], writes=[m1B])
            S.op("pool", lambda e, xti=xti: e.tensor_tensor(out=xn, in0=m1, in1=xti, op=ALU.add),
                 reads=[m1B, xtiB], writes=[xnB])
            store(X[tsl, :], xn, xnB, XB[t])
            S.op("act", lambda e: e.activation(out=junk, in_=xn, func=AF.Square, accum_out=ss),
                 reads=[xnB], writes=[junkB, ssB])
            rms_rstd(ss, ssB, D, rstd, rstdB)
            S.op("dve", lambda e: e.scalar_tensor_tensor(
                out=m2, in0=xn, scalar=rstd[:, 0:1], in1=G2, op0=ALU.mult, op1=ALU.mult),
                reads=[xnB, rstdB, G2B], writes=[m2B])
            hbi, hbiB = h2b[t % 2]
            S.op("pool", lambda e, hbi=hbi: e.tensor_tensor(out=hbi, in0=m2, in1=SH2, op=ALU.add),
                 reads=[m2B, SH2B], writes=[hbiB])
            store(H2[tsl, :], hbi, hbiB, H2B)
            for k in range(8):
                S.op("pe", lambda e, k=k, hbi=hbi: e.transpose(
                    out=psbf[0][:, k * 128:(k + 1) * 128], in_=hbi[:, k * 128:(k + 1) * 128], identity=identb),
                    reads=[hbiB, identbB], writes=[psb[0]])
            S.op("act", lambda e: e.copy(out=h2T, in_=psbf[0]), reads=[psb[0]], writes=[h2TB])
            h2T3 = h2T.rearrange("p (k t) -> p k t", k=8)
            for k in range(8):
                S.op("pe", lambda e, k=k: e.matmul(
                    ps[5][:, 0:36], lhsT=h2T3[:, k, :], rhs=wr3[:, k, :], start=(k == 0), stop=(k == 7)),
                    reads=[h2TB, wrB], writes=[psb[5]])
            route_tile(t, R, ps[5], psb[5], brt, brtB, sm, smB, ab, abB)

    def route_tile(t, R, lgp, lgpB, brt, brtB, sm, smB, ab, abB):
        M1, M2, WA, WB, POS, carry = R["M1"], R["M2"], R["WA"], R["WB"], R["POS"], R["carry"]
        RB = R["RB"]
        M1t = M1[:, t * 32:(t + 1) * 32]
        M2t = M2[:, t * 32:(t + 1) * 32]

        def dve(fn, reads, writes):
            S.op("dve", fn, reads=reads, writes=writes)
        dve(lambda e: e.tensor_tensor(out=sm[:, 0:36], in0=lgp[:, 0:36], in1=brt, op=ALU.add),
            [lgpB, brtB], [smB])
        dve(lambda e: e.reduce_max(out=sm[:, 36:37], in_=sm[:, 0:4], axis=AX.X), [smB], [smB])
        dve(lambda e: e.tensor_scalar(out=sm[:, 37:38], in0=sm[:, 36:37], scalar1=-1.0, scalar2=None,
                                      op0=ALU.mult), [smB], [smB])
        S.op("act", lambda e: e.activation(out=sm[:, 116:120], in_=sm[:, 0:4], func=AF.Exp, bias=sm[:, 37:38],
                                           accum_out=sm[:, 38:39]), reads=[smB], writes=[smB])
        dve(lambda e: e.reciprocal(out=sm[:, 39:40], in_=sm[:, 38:39]), [smB], [smB])
        dve(lambda e: e.tensor_scalar(out=sm[:, 40:44], in0=sm[:, 0:4], scalar1=sm[:, 36:37], scalar2=None,
                                      op0=ALU.is_equal), [smB], [smB])
        dve(lambda e: e.tensor_scalar(out=sm[:, 44:48], in0=sm[:, 40:44], scalar1=-1.0, scalar2=-NEG,
                                      op0=ALU.add, op1=ALU.mult), [smB], [smB])
        dve(lambda e: e.tensor_tensor(
            out=sm[:, 48:80].rearrange("p (g e) -> p g e", g=4),
            in0=sm[:, 4:36].rearrange("p (g e) -> p g e", g=4),
            in1=sm[:, 44:48].unsqueeze(2).broadcast_to([128, 4, 8]), op=ALU.add), [smB], [smB])
        dve(lambda e: e.reduce_max(out=sm[:, 80:81], in_=sm[:, 48:80], axis=AX.X), [smB], [smB])
        dve(lambda e: e.tensor_scalar(out=M1t, in0=sm[:, 48:80], scalar1=sm[:, 80:81], scalar2=None,
                                      op0=ALU.is_equal), [smB], [RB])
        dve(lambda e: e.scalar_tensor_tensor(out=sm[:, 84:116], in0=M1t, scalar=NEG, in1=sm[:, 48:80],
                                             op0=ALU.mult, op1=ALU.add), [smB, RB], [smB])
        dve(lambda e: e.reduce_max(out=sm[:, 81:82], in_=sm[:, 84:116], axis=AX.X), [smB], [smB])
        dve(lambda e: e.tensor_scalar(out=M2t, in0=sm[:, 84:116], scalar1=sm[:, 81:82], scalar2=None,
                                      op0=ALU.is_equal), [smB], [RB])
        dve(lambda e: e.tensor_tensor(out=sm[:, 82:83], in0=sm[:, 80:81], in1=sm[:, 81:82], op=ALU.subtract),
            [smB], [smB])
        S.op("act", lambda e: e.activation(out=sm[:, 83:84], in_=sm[:, 82:83], func=AF.Sigmoid),
             reads=[smB], writes=[smB])
        dve(lambda e: e.tensor_tensor(out=WA[:, t:t + 1], in0=sm[:, 83:84], in1=sm[:, 39:40], op=ALU.mult),
            [smB], [RB])
        dve(lambda e: e.tensor_tensor(out=WB[:, t:t + 1], in0=sm[:, 39:40], in1=WA[:, t:t + 1], op=ALU.subtract),
            [smB, RB], [RB])
        dve(lambda e: e.tensor_tensor(out=ab, in0=M1t, in1=M2t, op=ALU.add), [RB], [abB])
        S.op("pe", lambda e: e.matmul(ps[2][:, 0:32], lhsT=tril, rhs=ab, start=True, stop=True),
             reads=[trilB, abB], writes=[psb[2]])
        S.op("pe", lambda e: e.matmul(ps[2][:, 32:64], lhsT=onesb, rhs=ab, start=True, stop=True),
             reads=[onesbB, abB], writes=[psb[2]])
        dve(lambda e: e.tensor_tensor(out=POS[:, t * 32:(t + 1) * 32], in0=ps[2][:, 0:32], in1=carry, op=ALU.add),
            [psb[2], R["carryB"]], [RB])
        dve(lambda e: e.tensor_tensor(out=carry, in0=ps[2][:, 32:64], in1=carry, op=ALU.add),
            [psb[2], R["carryB"]], [R["carryB"]])

    def phase_fg(l, R, last):
        A.reset(MOE2_MARK)
        M1, M2, WA, WB, POS, carry, DI, RB = (R[k] for k in ("M1", "M2", "WA", "WB", "POS", "carry", "DI", "RB"))
        fz, fzB = A.alloc("fz", 8 * 32, F32)
        pst, pstB = A.alloc("pst", 32, F32)
        be_f, be_fB = A.alloc("be_f", NB, F32)
        bei, beiB = A.alloc("bei", NB, I32)
        blk, blkB = A.alloc("blk", NB, F32)
        big, bigB = A.alloc("big", NB * 32, F32)
        load(blk, blkB, blk_in)

        def dve(fn, reads, writes):
            S.op("dve", fn, reads=reads, writes=writes)
        cB = R["carryB"]
        dve(lambda e: e.tensor_tensor(
            out=big.rearrange("p (e b) -> p e b", e=32),
            in0=carry.unsqueeze(2).broadcast_to([128, 32, NB]),
            in1=blk.unsqueeze(1).broadcast_to([128, 32, NB]), op=ALU.is_gt), [cB, blkB], [bigB])
        dve(lambda e: e.reduce_sum(out=fz[:, 32:64], in_=big.rearrange("p (e b) -> p e b", e=32), axis=AX.X),
            [bigB], [fzB])
        dve(lambda e: e.tensor_scalar(out=fz[:, 64:96], in0=fz[:, 32:64], scalar1=128.0, scalar2=None,
                                      op0=ALU.mult), [fzB], [fzB])
        cur = 64
        for i, sh in enumerate((1, 2, 4, 8, 16)):
            nxt = 96 if cur != 96 else 128
            dve(lambda e, cur=cur, nxt=nxt: e.tensor_copy(out=fz[:, nxt:nxt + 32], in_=fz[:, cur:cur + 32]),
                [fzB], [fzB])
            dve(lambda e, cur=cur, nxt=nxt, sh=sh: e.tensor_tensor(
                out=fz[:, nxt + sh:nxt + 32], in0=fz[:, cur + sh:cur + 32], in1=fz[:, cur:cur + 32 - sh],
                op=ALU.add), [fzB], [fzB])
            cur = nxt
        pend = fz[:, cur:cur + 32]
        dve(lambda e: e.tensor_tensor(out=pst, in0=pend, in1=fz[:, 64:96], op=ALU.subtract), [fzB], [pstB])
        dve(lambda e: e.tensor_tensor(
            out=big.rearrange("p (b e) -> p b e", e=32),
            in0=pend.unsqueeze(1).broadcast_to([128, NB, 32]),
            in1=blk.unsqueeze(2).broadcast_to([128, NB, 32]), op=ALU.is_le), [fzB, blkB], [bigB])
        dve(lambda e: e.reduce_sum(out=be_f, in_=big.rearrange("p (b e) -> p b e", e=32), axis=AX.X),
            [bigB], [be_fB])
        dve(lambda e: e.tensor_scalar(out=be_f, in0=be_f, scalar1=31.0, scalar2=128.0, op0=ALU.min, op1=ALU.mult),
            [be_fB], [be_fB])
        dve(lambda e: e.tensor_scalar(out=be_f, in0=be_f, scalar1=iotap[:, 0:1], scalar2=float(l * 32 * 128),
                                      op0=ALU.add, op1=ALU.add), [be_fB, iotapB], [be_fB])
        dve(lambda e: e.tensor_copy(out=bei, in_=be_f), [be_fB], [beiB])
        hx = [A.alloc(f"hx{i}", D, BF16) for i in range(2)]
        df, dfB = A.alloc("df", 64, F32)
        for t in range(NT):
            tsl = slice(t * 128, (t + 1) * 128)
            hxi, hxiB = hx[t % 2]
            load(hxi, hxiB, H2[tsl, :], reads=[H2B])
            dve(lambda e, t=t: e.tensor_tensor(out=df[:, 0:32], in0=POS[:, t * 32:(t + 1) * 32], in1=pst, op=ALU.add),
                [RB, pstB], [dfB])
            for j, M in enumerate((M1, M2)):
                dve(lambda e, t=t, M=M: e.tensor_tensor(out=df[:, 32:64], in0=df[:, 0:32],
                                                        in1=M[:, t * 32:(t + 1) * 32], op=ALU.mult),
                    [dfB, RB], [dfB])
                dve(lambda e, t=t, j=j: e.reduce_sum(out=R["DF"][:, 2 * t + j:2 * t + j + 1], in_=df[:, 32:64],
                                                      axis=AX.X), [dfB], [R["DFB"]])
            dve(lambda e, t=t: e.tensor_copy(out=DI[:, 2 * t:2 * t + 2], in_=R["DF"][:, 2 * t:2 * t + 2]),
                [R["DFB"]], [R["DIB"]])
            if stop == "fg1":
                continue
            for j in range(2):
                S.dma("pool", lambda e, t=t, j=j, hxi=hxi: e.indirect_dma_start(
                    out=BUFD, out_offset=bass.IndirectOffsetOnAxis(ap=DI[:, 2 * t + j:2 * t + j + 1], axis=0),
                    in_=hxi, in_offset=None, bounds_check=S.pool_regs["nb"], oob_is_err=False), hxiB,
                    reads=[hxiB, R["DIB"]], writes=[BUFDB])
        if stop == "fg1":
            d1 = nc.dram_tensor("DBG_DI", [128, 2 * NT], I32, kind="ExternalOutput").ap()
            d2 = nc.dram_tensor("DBG_BEI", [128, NB], I32, kind="ExternalOutput").ap()
            d3 = nc.dram_tensor("DBG_F", [128, 64], F32, kind="ExternalOutput").ap()
            d4 = nc.dram_tensor("DBG_RT", [128, NT * 32 * 3 + 2 * NT], F32, kind="ExternalOutput").ap()
            S.dma("sp", lambda e: e.dma_start(out=d1, in_=DI), R["DIB"], reads=[R["DIB"]])
            S.dma("sp", lambda e: e.dma_start(out=d2, in_=bei), beiB, reads=[beiB])
            S.dma("sp", lambda e: e.dma_start(out=d3[:, 0:32], in_=carry), cB, reads=[cB])
            S.dma("sp", lambda e: e.dma_start(out=d3[:, 32:64], in_=pst), pstB, reads=[pstB])
            S.dma("sp", lambda e: e.dma_start(out=d4, in_=R["RT"]), RB, reads=[RB])
            final_bufs.extend([R["DIB"], beiB, cB, pstB, RB])
            return
        if stop == "fg2":
            return
        m_exp = A.mark()
        wf = [A.alloc(f"wf{i}", 4096, F32) for i in range(3)]
        wb_ = [[A.alloc(f"wb{i}_{j}", 4096, BF16) for j in range(2)] for i in range(3)]
        xb = [A.alloc(f"xb{i}", D, BF16) for i in range(2)]
        xbT, xbTB = A.alloc("xbT", 8 * 128, BF16)
        sl, slB = A.alloc("sl", 512, F32)
        hh, hhB = A.alloc("hh", 512, BF16)
        hhT, hhTB = A.alloc("hhT", 4 * 128, BF16)
        yst = [A.alloc(f"yst{i}", D, F32) for i in range(2)]
        casts = ("act", "dve", "pool")
        for b in range(NB):
            bsl = slice(b * 128, (b + 1) * 128)
            wbs = []
            for i, wsrc in enumerate((w1r, w3r, w2r)):
                wfi, wfiB = wf[i]
                wbi, wbiB = wb_[i][b % 2]
                S.dma("pool", lambda e, wfi=wfi, wsrc=wsrc, b=b: e.indirect_dma_start(
                    out=wfi, out_offset=None, in_=wsrc,
                    in_offset=bass.IndirectOffsetOnAxis(ap=bei[:, b:b + 1], axis=0)), wfiB,
                    reads=[beiB], writes=[wfiB])
                if casts[i] == "act":
                    S.op("act", lambda e, wbi=wbi, wfi=wfi: e.copy(out=wbi, in_=wfi), reads=[wfiB], writes=[wbiB])
                else:
                    S.op(casts[i], lambda e, wbi=wbi, wfi=wfi: e.tensor_copy(out=wbi, in_=wfi),
                         reads=[wfiB], writes=[wbiB])
                wbs.append((wbi, wbiB))
            xbi, xbiB = xb[b % 2]
            load(xbi, xbiB, BUFD[bsl, :], reads=[BUFDB])
            for k in range(8):
                S.op("pe", lambda e, k=k, xbi=xbi: e.transpose(
                    out=psbf[0][:, k * 128:(k + 1) * 128], in_=xbi[:, k * 128:(k + 1) * 128], identity=identb),
                    reads=[xbiB, identbB], writes=[psb[0]])
            S.op("act", lambda e: e.copy(out=xbT, in_=psbf[0]), reads=[psb[0]], writes=[xbTB])
            xbT3 = xbT.rearrange("p (k t) -> p k t", k=8)
            w13 = wbs[0][0].rearrange("p (k n) -> p k n", k=8)
            w33 = wbs[1][0].rearrange("p (k n) -> p k n", k=8)
            w23 = wbs[2][0].rearrange("p (k n) -> p k n", k=4)
            for (w_, wB_, bk) in ((w13, wbs[0][1], 1), (w33, wbs[1][1], 2)):
                for k in range(8):
                    S.op("pe", lambda e, k=k, w_=w_, bk=bk: e.matmul(
                        ps[bk][:, :], lhsT=xbT3[:, k, :], rhs=w_[:, k, :], start=(k == 0), stop=(k == 7)),
                        reads=[xbTB, wB_], writes=[psb[bk]])
            S.op("act", lambda e: e.activation(out=sl, in_=ps[1][:, :], func=AF.Silu), reads=[psb[1]], writes=[slB])
            S.op("dve", lambda e: e.tensor_tensor(out=hh, in0=ps[2][:, :], in1=sl, op=ALU.mult),
                 reads=[psb[2], slB], writes=[hhB])
            for k in range(4):
                S.op("pe", lambda e, k=k: e.transpose(
                    out=psbf[3][:, k * 128:(k + 1) * 128], in_=hh[:, k * 128:(k + 1) * 128], identity=identb),
                    reads=[hhB, identbB], writes=[psb[3]])
            S.op("act", lambda e: e.copy(out=hhT, in_=psbf[3][:, 0:512]), reads=[psb[3]], writes=[hhTB])
            hhT3 = hhT.rearrange("p (k t) -> p k t", k=4)
            ys, ysB = yst[b % 2]
            for hf in range(2):
                bk = 4 + hf
                for k in range(4):
                    S.op("pe", lambda e, k=k, bk=bk, hf=hf, w23=w23: e.matmul(
                        ps[bk][:, :], lhsT=hhT3[:, k, :], rhs=w23[:, k, hf * 512:(hf + 1) * 512],
                        start=(k == 0), stop=(k == 3)), reads=[hhTB, wbs[2][1]], writes=[psb[bk]])
                S.op("dve", lambda e, bk=bk, hf=hf, ys=ys: e.tensor_copy(out=ys[:, hf * 512:(hf + 1) * 512],
                                                                          in_=ps[bk][:, :]),
                     reads=[psb[bk]], writes=[ysB])
            store(YB[bsl, :], ys, ysB, YBB)
        if stop == "fg3":
            return
        A.reset(m_exp)
        g2b, g2bB = A.alloc("g2b", D, F32)
        y1 = [A.alloc(f"y1{i}", D, F32) for i in range(2)]
        y2 = [A.alloc(f"y2{i}", D, F32) for i in range(2)]
        xg = [A.alloc(f"xg{i}", D, F32) for i in range(2)]
        o1, o1B = A.alloc("o1", D, F32)
        xo = [A.alloc(f"xo{i}", D, F32) for i in range(2)]
        if last:
            gfin, gfinB = A.alloc("gfin", D, F32)
            bload(gfin, gfinB, g_fin[0:1, :])
            junk, junkB = A.alloc("junkg", D, F32)
            ss, ssB = A.alloc("ssg", 1, F32)
            rstd, rstdB = A.alloc("rstdg", 1, F32)
        for t in range(NT):
            tsl = slice(t * 128, (t + 1) * 128)
            if t % SLOT_T == 0:
                s = t // SLOT_T
                bload(g2b, g2bB, mod_row(l, s, 5), reads=[MODSB])
            ya, yaB = y1[t % 2]
            yb_, ybB = y2[t % 2]
            xi, xiB = xg[t % 2]
            for j, (yy, yyB) in enumerate(((ya, yaB), (yb_, ybB))):
                S.dma("pool", lambda e, yy=yy, t=t, j=j: e.indirect_dma_start(
                    out=yy, out_offset=None, in_=YB,
                    in_offset=bass.IndirectOffsetOnAxis(ap=DI[:, 2 * t + j:2 * t + j + 1], axis=0),
                    bounds_check=S.pool_regs["nb"], oob_is_err=False), yyB,
                    reads=[YBB, R["DIB"]], writes=[yyB])
            load(xi, xiB, X[tsl, :], reads=[XB[t]])
            dve(lambda e, ya=ya, t=t: e.tensor_scalar(out=o1, in0=ya, scalar1=WA[:, t:t + 1], scalar2=None,
                                                      op0=ALU.mult), [yaB, RB], [o1B])
            dve(lambda e, yb_=yb_, t=t: e.scalar_tensor_tensor(out=o1, in0=yb_, scalar=WB[:, t:t + 1], in1=o1,
                                                               op0=ALU.mult, op1=ALU.add), [ybB, RB, o1B], [o1B])
            S.op("pool", lambda e: e.tensor_tensor(out=o1, in0=o1, in1=g2b, op=ALU.mult),
                 reads=[o1B, g2bB], writes=[o1B])
            xoi, xoiB = xo[t % 2]
            dve(lambda e, xi=xi, xoi=xoi: e.tensor_tensor(out=xoi, in0=o1, in1=xi, op=ALU.add),
                [o1B, xiB], [xoiB])
            if not last:
                store(X[tsl, :], xoi, xoiB, XB[t])
            else:
                S.op("act", lambda e, xoi=xoi: e.activation(out=junk, in_=xoi, func=AF.Square, accum_out=ss),
                     reads=[xoiB], writes=[junkB, ssB])
                rms_rstd(ss, ssB, D, rstd, rstdB)
                dve(lambda e, xoi=xoi: e.scalar_tensor_tensor(
                    out=xoi, in0=xoi, scalar=rstd[:, 0:1], in1=gfin, op0=ALU.mult, op1=ALU.mult),
                    [xoiB, rstdB, gfinB], [xoiB])
                S.dma("sp", lambda e, xoi=xoi, tsl=tsl: e.dma_start(out=y_out[tsl, :], in_=xoi), xoiB,
                      reads=[xoiB])
                if xoiB not in final_bufs:
                    final_bufs.append(xoiB)

    ATT_MARK = PERSIST
    MOE_MARK = PERSIST
    MOE2_MARK = PERSIST
    for l in range(depth):
        phase_mod(l)
        if stop == "mod":
            break
        phase_a(l)
        if stop == "a":
            break
        phase_c1(l)
        if stop == "c1":
            break
        A.reset(PERSIST)
        QT = [A.alloc(f"QT{i}", T, BF16) for i in range(2)]
        KT = [A.alloc(f"KT{i}", T, BF16) for i in range(2)]
        for i in range(2):
            load(QT[i][0][96:100, :], QT[i][1], qseg_in)
            load(KT[i][0][96:100, :], KT[i][1], kseg_in)
        ATT_MARK = A.mark()
        phase_c2(l, QT, KT, True)
        if stop == "c2":
            break
        for i in range(2):
            load(QT[i][0][64:96, :], QT[i][1], qrow_in)
            load(KT[i][0][64:96, :], KT[i][1], krow_in)
        phase_b(l, QT, KT)
        if stop == "b":
            break
        A.reset(PERSIST)
        R = {}
        RT, R["RB"] = A.alloc("RT", NT * 32 * 3 + 2 * NT, F32)
        R["RT"] = RT
        R["M1"] = RT[:, 0:NT * 32]
        R["M2"] = RT[:, NT * 32:2 * NT * 32]
        R["POS"] = RT[:, 2 * NT * 32:3 * NT * 32]
        R["WA"] = RT[:, 3 * NT * 32:3 * NT * 32 + NT]
        R["WB"] = RT[:, 3 * NT * 32 + NT:3 * NT * 32 + 2 * NT]
        R["carry"], R["carryB"] = A.alloc("carry", 32, F32)
        R["DF"], R["DFB"] = A.alloc("DF", 2 * NT, F32)
        R["DI"], R["DIB"] = A.alloc("DI", 2 * NT, I32)
        MOE_MARK = A.mark()
        phase_de(l, R)
        if stop == "de":
            break
        MOE2_MARK = MOE_MARK
        phase_fg(l, R, l == depth - 1)
        if stop in ("fg1", "fg2", "fg3"):
            break

    S.emit(final_bufs=final_bufs)
    es.close()
    return nc, S


def _bf(a):
    return np.ascontiguousarray(a.astype(NPBF))


def core_consts(T, L):
    NT = T // 128
    tok = np.arange(T)
    seg = tok // L
    kseg = np.zeros((4, T), np.float32)
    kseg[seg, tok] = 1.0
    qseg = np.where(kseg > 0, 0.0, NEG).astype(np.float32)
    r = tok // 64
    rps = L // 64
    seg_lo = (r // rps) * rps
    rs = np.clip(r - 4, seg_lo, seg_lo + rps - 8)
    krow = np.zeros((32, T), np.float32)
    krow[r % 32, tok] = 1.0
    p = r // 2
    base = 2 * p - 8
    j = np.arange(32)[:, None]
    a_j = base[None, :] + ((j - base[None, :]) % 32)
    valid = (a_j >= rs[None, :]) & (a_j < rs[None, :] + 8) & (a_j < (2 * p + 10)[None, :])
    qrow = np.where(valid, 0.0, NEG).astype(np.float32)
    pos = (tok % L).astype(np.float32)
    inv = (1.0 / (np.float32(10000.0) ** (np.arange(0, 32, 2, dtype=np.float32) / np.float32(32)))).astype(np.float32)
    ang = (pos[:, None] * inv[None, :]).astype(np.float32)
    cos = np.cos(ang).astype(np.float32)
    sin = np.sin(ang).astype(np.float32)
    cosk = np.ascontiguousarray(cos.reshape(NT, 128, 16).transpose(1, 0, 2).reshape(128, NT * 16))
    sink = np.ascontiguousarray(sin.reshape(NT, 128, 16).transpose(1, 0, 2).reshape(128, NT * 16))
    cosq = np.ascontiguousarray(np.concatenate([cos.T, cos.T], 0))
    sinq = np.ascontiguousarray(np.concatenate([-sin.T, sin.T], 0))
    return dict(kseg=_bf(kseg), qseg=_bf(qseg), krow=_bf(krow), qrow=_bf(qrow),
                cosk=cosk, sink=sink, cosq=cosq, sinq=sinq)


def na_tables(rpb):
    depth = rpb.shape[0]
    key = np.arange(128)
    ra, ck = key // 64, key % 64
    qry = np.arange(128)
    rq, cq = qry // 64, qry % 64
    d = 2 * (np.arange(9) - 4)
    dr = d[None, :, None] + ra[:, None, None] - rq[None, None, :]
    dc = ck[:, None, None] - cq[None, None, :] + 0 * d[None, :, None]
    cs = np.clip(cq - 8, 0, 48)
    valid = (np.abs(dr) <= 7) & (ck[:, None, None] >= cs[None, None, :]) & (ck[:, None, None] < cs[None, None, :] + 16)
    dri = np.clip(dr + 7, 0, 14)
    dci = np.clip(dc + 15, 0, 30)
    g = rpb[:, :, dri, dci]
    out = np.where(valid[None, None], g, np.float32(NEG)).astype(np.float32)
    return np.ascontiguousarray(out.reshape(depth, 8, 128, 9 * 128))


def shared_inputs(T, depth, ada_w, ada_b, norm_mix_g, w_in, mla_q_norm_g, w_uq, mla_kv_norm_g, w_ukv, na_rpb,
                  w_na_o, w_mla_o, w_out, norm_ffn_g, router_wg, router_bg, router_we, router_be, expert_w1,
                  expert_w3, expert_w2, final_norm_g):
    f = lambda a: np.ascontiguousarray(np.asarray(a, dtype=np.float32))
    NB = (2 * T + 32 * 127 + 127) // 128
    w_uq = f(w_uq)[:depth]
    perm = np.arange(768)
    for h in range(8):
        b = h * 96 + 64
        perm[b:b + 16] = np.arange(b + 16, b + 32)
        perm[b + 16:b + 32] = np.arange(b, b + 16)
    w_ukv = f(w_ukv)[:depth].reshape(depth, 128, 8, 128)
    d = dict(
        ada_w=f(ada_w)[:depth], ada_b=f(ada_b)[:depth], norm_mix_g=f(norm_mix_g)[:depth],
        norm_ffn_g=f(norm_ffn_g)[:depth], final_norm_g=f(final_norm_g).reshape(1, D), w_in=f(w_in)[:depth],
        mla_q_norm_g=f(mla_q_norm_g)[:depth], mla_kv_norm_g=f(mla_kv_norm_g)[:depth],
        w_uq=w_uq, w_uq_rot=np.ascontiguousarray(w_uq[:, :, perm]),
        w_ukv_k=np.ascontiguousarray(w_ukv[:, :, :, 0:64].reshape(depth, 128, 512)),
        w_ukv_v=np.ascontiguousarray(w_ukv[:, :, :, 64:128].reshape(depth, 128, 512)),
        na_tt=na_tables(f(na_rpb)[:depth]),
        w_na_o=f(w_na_o)[:depth], w_mla_o=f(w_mla_o)[:depth], w_out=f(w_out)[:depth],
        w_router=np.ascontiguousarray(np.concatenate([f(router_wg)[:depth], f(router_we)[:depth]], -1)),
        b_router=np.ascontiguousarray(np.concatenate([f(router_bg)[:depth], f(router_be)[:depth]], -1)),
        w1r=np.ascontiguousarray(f(expert_w1)[:depth].reshape(depth, 32, 8, 128, 512).transpose(0, 1, 3, 2, 4)
                                 .reshape(depth * 32 * 128, 4096)),
        w3r=np.ascontiguousarray(f(expert_w3)[:depth].reshape(depth, 32, 8, 128, 512).transpose(0, 1, 3, 2, 4)
                                 .reshape(depth * 32 * 128, 4096)),
        w2r=np.ascontiguousarray(f(expert_w2)[:depth].reshape(depth, 32, 4, 128, 1024).transpose(0, 1, 3, 2, 4)
                                 .reshape(depth * 32 * 128, 4096)),
        identb=_bf(np.eye(128, dtype=np.float32)), identf=np.eye(128, dtype=np.float32),
        tril=_bf(np.triu(np.ones((128, 128), np.float32), 1)), onesb=_bf(np.ones((128, 128), np.float32)),
        iotap=np.arange(128, dtype=np.float32).reshape(128, 1),
        blkstart=np.ascontiguousarray(np.broadcast_to((np.arange(NB, dtype=np.float32) * 128)[None, :], (128, NB))),
    )
    return d


def core_inputs(T, L, x, cs):
    c4 = np.stack([np.asarray(c, np.float32) for c in cs], 0)
    cT = np.ascontiguousarray(c4.reshape(4, 8, 128).transpose(2, 1, 0).reshape(128, 32))
    d = dict(x=np.ascontiguousarray(np.asarray(x, np.float32)), cT=cT)
    d.update(core_consts(T, L))
    return d


_CACHE = {}


def kernel(x_prompt, x_sample, c_prompt, c_sample, **w):
    T, depth = 8192, 4
    x_prompt = np.asarray(x_prompt, np.float32)
    x_sample = np.asarray(x_sample, np.float32)
    c_prompt = np.asarray(c_prompt, np.float32)
    c_sample = np.asarray(c_sample, np.float32)
    if "nc" not in _CACHE:
        _CACHE["nc"] = build(T, depth)[0]
    nc = _CACHE["nc"]
    sh = shared_inputs(T, depth, **w)
    in_maps = []
    for i in range(4):
        m = dict(sh)
        m.update(core_inputs(T, 8192, x_prompt[i], [c_prompt[i]] * 4))
        in_maps.append(m)
    for i in range(4):
        j = i % 2
        m = dict(sh)
        m.update(core_inputs(T, 2048, x_sample[4 * j:4 * j + 4].reshape(T, D), [c_sample[4 * j + s] for s in range(4)]))
        in_maps.append(m)
    res = run_bass_kernel_spmd(nc, in_maps, core_ids=list(range(8)))
    ys = [r["y"] for r in res.results]
    y_prompt = np.stack(ys[0:4], 0).astype(np.float32)
    y_sample = np.concatenate([ys[4].reshape(4, 2048, D), ys[5].reshape(4, 2048, D)], 0).astype(np.float32)
    return (y_prompt, y_sample)
```

```python
import contextlib
import numpy as np
import ml_dtypes
import concourse.bass as bass
import concourse.mybir as mybir
from concourse.bass_utils import run_bass_kernel_spmd

F32 = mybir.dt.float32
BF16 = mybir.dt.bfloat16
I32 = mybir.dt.int32
U8 = mybir.dt.uint8
AF = mybir.ActivationFunctionType
ALU = mybir.AluOpType
AX = mybir.AxisListType
NPBF = ml_dtypes.bfloat16

D = 1024
EPS = 1e-6
NEG = -30000.0
EPOCH = 30000


class Buf:
    __slots__ = ("name", "lw", "rd", "cnt", "last_dma", "sem", "multi")

    def __init__(self, name, multi=False):
        self.name = name
        self.lw = {}
        self.rd = {}
        self.cnt = 0
        self.last_dma = None
        self.sem = None
        self.multi = multi


class SemState:
    __slots__ = ("cnt", "last_dma", "sem")

    def __init__(self):
        self.cnt = 0
        self.last_dma = None
        self.sem = None


SEMREG = {}


class Op:
    __slots__ = ("idx", "eng", "fn", "deps", "isdma", "sbuf", "n", "inc", "val", "dval")


class Sched:
    def __init__(self, nc):
        self.nc = nc
        self.ops = []
        self.reg_requests = {}
        self.pool_regs = {}
        self.semreg = {}
        self.engh = {"pe": nc.tensor, "act": nc.scalar, "dve": nc.vector,
                     "pool": nc.gpsimd, "sp": nc.sync}

    def _key(self, o):
        return o.eng if not o.isdma else ("dma", id(o.sbuf))

    def _deps(self, o, reads, writes):
        deps = {}
        for b in reads:
            for w in b.lw.values():
                deps[w.idx] = w
        for b in writes:
            if not (b.multi and not b.rd):
                for w in b.lw.values():
                    deps[w.idx] = w
            for r in b.rd.values():
                deps[r.idx] = r
        o.deps = list(deps.values())
        k = self._key(o)
        for b in writes:
            if b.multi and not b.rd:
                b.lw[k] = o
            else:
                b.lw = {k: o}
            b.rd = {}
        for b in reads:
            if o in b.lw.values():
                continue
            b.rd[k] = o

    def op(self, eng, fn, reads=(), writes=()):
        o = Op()
        o.idx = len(self.ops)
        o.eng = eng
        o.fn = fn
        o.isdma = False
        o.inc = False
        o.val = None
        o.sbuf = None
        self._deps(o, reads, writes)
        self.ops.append(o)
        return o

    def dma(self, eng, fns, key, reads=(), writes=()):
        if not isinstance(fns, (list, tuple)):
            fns = [fns]
        o = Op()
        o.idx = len(self.ops)
        o.eng = eng
        o.fn = list(fns)
        o.isdma = True
        o.inc = False
        o.val = None
        ss = self.semreg.setdefault(key.name, SemState())
        o.sbuf = ss
        self._deps(o, reads, writes)
        if ss.last_dma is not None and ss.last_dma not in o.deps:
            o.deps.append(ss.last_dma)
        ss.last_dma = o
        ss.cnt += len(fns)
        o.dval = ss.cnt * 16
        self.ops.append(o)
        return o

    def emit(self, final_bufs=()):
        nc = self.nc
        ops = self.ops
        for o in ops:
            for d in o.deps:
                if d.isdma:
                    continue
                if d.eng == o.eng and o.eng == "pe" and not o.isdma:
                    continue
                d.inc = True
        cnt = {e: 0 for e in self.engh}
        for o in ops:
            if not o.isdma and o.inc:
                cnt[o.eng] += 1
                o.val = cnt[o.eng]
        es = contextlib.ExitStack()
        engsems = {}
        for e in self.engh:
            n = max(1, (cnt[e] + EPOCH - 1) // EPOCH)
            engsems[e] = [es.enter_context(nc.semaphore(f"s_{e}_{i}")) for i in range(n)]
        dmabufs = {}
        for o in ops:
            if o.isdma and id(o.sbuf) not in dmabufs:
                dmabufs[id(o.sbuf)] = o.sbuf
        for i, b in enumerate(dmabufs.values()):
            b.sem = es.enter_context(nc.semaphore(f"d_{i}"))
        self.n_sems = sum(len(v) for v in engsems.values()) + len(dmabufs)
        per = {e: [] for e in self.engh}
        for o in ops:
            per[o.eng].append(o)

        def run_engine(ename, eh):
            if ename == "pool":
                for nm, val in getattr(self, "reg_requests", {}).items():
                    r = eh.alloc_register(nm)
                    eh.reg_mov(r, val)
                    self.pool_regs[nm] = r
            known_eng = {e: 0 for e in self.engh}
            known_dma = {}
            for o in per[ename]:
                need_eng = {}
                need_dma = {}
                for d in o.deps:
                    if d.isdma:
                        k = id(d.sbuf)
                        if d.dval > known_dma.get(k, 0) and d.dval > need_dma.get(k, (0, None))[0]:
                            need_dma[k] = (d.dval, d.sbuf)
                    else:
                        if d.val is None:
                            continue
                        if d.val > known_eng[d.eng] and d.val > need_eng.get(d.eng, 0):
                            need_eng[d.eng] = d.val
                for e, v in need_eng.items():
                    eh.wait_ge(engsems[e][(v - 1) // EPOCH], (v - 1) % EPOCH + 1)
                    known_eng[e] = v
                for k, (v, b) in need_dma.items():
                    eh.wait_ge(b.sem, v)
                    known_dma[k] = v
                if o.isdma:
                    for f in o.fn:
                        f(eh).then_inc(o.sbuf.sem, 16)
                else:
                    ins = o.fn(eh)
                    if o.inc:
                        ins.then_inc(engsems[ename][(o.val - 1) // EPOCH], 1)
            if ename == "sp":
                for b in final_bufs:
                    ss = self.semreg.get(b.name)
                    if ss is not None and ss.sem is not None and ss.cnt > 0:
                        eh.wait_ge(ss.sem, ss.cnt * 16)

        with nc.Block() as block:
            @block.tensor
            def _(e):
                run_engine("pe", e)

            @block.scalar
            def _(e):
                run_engine("act", e)

            @block.vector
            def _(e):
                run_engine("dve", e)

            @block.gpsimd
            def _(e):
                run_engine("pool", e)

            @block.sync
            def _(e):
                run_engine("sp", e)
        es.close()


class Arena:
    def __init__(self, t, nbytes):
        self.t = t
        self.n = nbytes
        self.off = 0
        self.regions = []

    def mark(self):
        return self.off

    def reset(self, m=0):
        self.off = m

    def alloc(self, name, cols, dtype, parts=128):
        esz = {F32: 4, BF16: 2, I32: 4, U8: 1}[dtype]
        nb = cols * esz
        start = (self.off + 63) // 64 * 64
        end = start + nb
        assert end <= self.n, f"arena overflow {name}: {end} > {self.n}"
        self.off = end
        b = Buf(name)
        keep = []
        for (s, e, ob) in self.regions:
            if s < end and start < e:
                for w in ob.lw.values():
                    b.rd[("old", w.idx)] = w
                for r in ob.rd.values():
                    b.rd[("old", r.idx)] = r
                if s < start or e > end:
                    keep.append((s, e, ob))
            else:
                keep.append((s, e, ob))
        keep.append((start, end, b))
        self.regions = keep
        ap = self.t[0:parts, start:end].bitcast(dtype)
        return ap, b


IN_Q, IN_K, IN_V, IN_CQ, IN_CKV, IN_KR, IN_G = 0, 512, 1024, 1536, 1792, 1920, 1952


def build(T, depth, dbg=None, stop=None):
    NT = T // 128
    NG = T // 512
    NB = (2 * T + 32 * 127 + 127) // 128
    SLOT_T = NT // 4
    nc = bass.Bass("TRN2", target_bir_lowering=False)

    def din(name, shape, dt=F32):
        return nc.dram_tensor(name, list(shape), dt, kind="ExternalInput").ap()

    def dscr(name, shape, dt):
        kind = "ExternalOutput" if (dbg and name in dbg) else "Internal"
        return nc.dram_tensor(name, list(shape), dt, kind=kind).ap()

    x_in = din("x", [T, D])
    cT_in = din("cT", [128, 32])
    ada_w = din("ada_w", [depth, D, 6 * D])
    ada_b = din("ada_b", [depth, 6 * D])
    g_mix = din("norm_mix_g", [depth, D])
    g_ffn = din("norm_ffn_g", [depth, D])
    g_fin = din("final_norm_g", [1, D])
    w_in = din("w_in", [depth, D, 4000])
    g_q = din("mla_q_norm_g", [depth, 256])
    g_kv = din("mla_kv_norm_g", [depth, 128])
    w_uq = din("w_uq", [depth, 256, 768])
    w_uqr = din("w_uq_rot", [depth, 256, 768])
    w_ukvk = din("w_ukv_k", [depth, 128, 512])
    w_ukvv = din("w_ukv_v", [depth, 128, 512])
    tt_in = din("na_tt", [depth, 8, 128, 9 * 128])
    w_nao = din("w_na_o", [depth, 512, D])
    w_mlao = din("w_mla_o", [depth, 512, D])
    w_out = din("w_out", [depth, D, D])
    w_r = din("w_router", [depth, D, 36])
    b_r = din("b_router", [depth, 36])
    w1r = din("w1r", [depth * 32 * 128, 4096])
    w3r = din("w3r", [depth * 32 * 128, 4096])
    w2r = din("w2r", [depth * 32 * 128, 4096])
    identb_in = din("identb", [128, 128], BF16)
    identf_in = din("identf", [128, 128])
    tril_in = din("tril", [128, 128], BF16)
    ones_in = din("onesb", [128, 128], BF16)
    kseg_in = din("kseg", [4, T], BF16)
    qseg_in = din("qseg", [4, T], BF16)
    krow_in = din("krow", [32, T], BF16)
    qrow_in = din("qrow", [32, T], BF16)
    cosk_in = din("cosk", [128, NT * 16])
    sink_in = din("sink", [128, NT * 16])
    cosq_in = din("cosq", [32, T])
    sinq_in = din("sinq", [32, T])
    iotap_in = din("iotap", [128, 1])
    blk_in = din("blkstart", [128, NB])

    y_out = nc.dram_tensor("y", [T, D], F32, kind="ExternalOutput").ap()
    dbg_out = {}

    X = dscr("X", [T, D], F32)
    MODS = dscr("MODS", [depth, 4, 6 * D], F32)
    QA = dscr("QA", [8, 64, T], BF16)
    KA = dscr("KA", [8, 64, T], BF16)
    VNA = dscr("VNA", [T, 512], BF16)
    CQT = dscr("CQT", [256, T], BF16)
    CKVT = dscr("CKVT", [128, T], BF16)
    KRT = dscr("KRT", [32, T], BF16)
    GATE = dscr("GATE", [T, 2048], BF16)
    QM = dscr("QM", [8, 96, T], BF16)
    KM = dscr("KM", [8, 64, T], BF16)
    VM = dscr("VM", [8, T, 64], BF16)
    NAO = dscr("NAO", [T, 512], BF16)
    MLAO = dscr("MLAO", [T, 512], BF16)
    H2 = dscr("H2", [T, D], BF16)
    BUFD = dscr("BUFD", [NB * 128, D], BF16)
    YB = dscr("YB", [NB * 128, D], F32)

    def MB(n):
        return Buf(n, multi=True)
    XB = [MB(f"X{t}") for t in range(NT)]
    MODSB = MB("MODS")
    QAB, KAB, VNAB, CQTB, CKVTB, KRTB, GATEB = (MB(n) for n in ("QA", "KA", "VNA", "CQT", "CKVT", "KRT", "GATE"))
    QMB, KMB, VMB, NAOB, MLAOB, H2B, BUFDB, YBB = (MB(n) for n in ("QM", "KM", "VM", "NAO", "MLAO", "H2", "BUFD", "YB"))

    es = contextlib.ExitStack()
    ARN = 196 * 1024
    sb = es.enter_context(nc.sbuf_tensor("arena", [128, ARN], U8))
    psh = [es.enter_context(nc.psum_tensor(f"ps{i}", [128, 1024], F32)) for i in range(4)]
    ps2 = [h[:, :] for h in psh]
    ps = [ps2[i // 2][:, (i % 2) * 512:(i % 2 + 1) * 512] for i in range(8)]
    psbf = [p.bitcast(BF16) for p in ps]
    psb = [Buf(f"ps{i}") for i in range(8)]
    A = Arena(sb, ARN)
    S = Sched(nc)
    S.reg_requests["nb"] = NB * 128 - 1
    S.reg_requests["wrows"] = depth * 32 * 128 - 1
    final_bufs = []

    def load(dst, dstB, src, reads=(), extra_writes=()):
        return S.dma("sp", lambda e: e.dma_start(out=dst, in_=src), dstB, reads=list(reads),
                     writes=[dstB] + list(extra_writes))

    def store(dst, src, srcB, dstB):
        return S.dma("sp", lambda e: e.dma_start(out=dst, in_=src), srcB, reads=[srcB], writes=[dstB])

    identb, identbB = A.alloc("identb", 128, BF16)
    identf, identfB = A.alloc("identf", 128, F32)
    tril, trilB = A.alloc("tril", 128, BF16)
    onesb, onesbB = A.alloc("onesb", 128, BF16)
    scT, scTB = A.alloc("scT", 32, F32)
    iotap, iotapB = A.alloc("iotap", 1, F32)
    load(identb, identbB, identb_in)
    load(identf, identfB, identf_in)
    load(tril, trilB, tril_in)
    load(onesb, onesbB, ones_in)
    load(iotap, iotapB, iotap_in)
    load(scT, scTB, cT_in)
    S.op("act", lambda e: e.activation(out=scT, in_=scT, func=AF.Silu), reads=[scTB], writes=[scTB])
    scT3 = scT.rearrange("p (k s) -> p k s", k=8)
    PERSIST = A.mark()

    def rms_rstd(ssum, ssB, n, rstd, rstdB, width=1):
        S.op("dve", lambda e: e.tensor_scalar(out=rstd, in0=ssum, scalar1=1.0 / n, scalar2=EPS,
                                              op0=ALU.mult, op1=ALU.add), reads=[ssB], writes=[rstdB])
        S.op("act", lambda e: e.activation(out=rstd, in_=rstd, func=AF.Sqrt), reads=[rstdB], writes=[rstdB])
        S.op("dve", lambda e: e.reciprocal(out=rstd, in_=rstd), reads=[rstdB], writes=[rstdB])

    def bload(dst, dstB, row_ap, parts=128, reads=()):
        n = row_ap.shape[-1]
        return S.dma("sp", lambda e: e.dma_start(out=dst, in_=row_ap.broadcast_to([parts, n])), dstB,
                     reads=list(reads), writes=[dstB])

    def phase_mod(l):
        A.reset(PERSIST)
        mods, modsB = A.alloc("mods", 6 * D, F32, parts=4)
        adab, adabB = A.alloc("adab", 6 * D, F32, parts=4)
        aw = [A.alloc(f"aw{i}", 8 * 512, F32) for i in range(2)]
        bload(adab, adabB, ada_b[l:l + 1, :], parts=4)
        awsrc = ada_w[l].rearrange("(k p) n -> p k n", p=128)
        for blk in range(12):
            awt, awtB = aw[blk % 2]
            awt3 = awt.rearrange("p (k n) -> p k n", k=8)
            load(awt3, awtB, awsrc[:, :, blk * 512:(blk + 1) * 512])
            bk = 6 + blk % 2
            for k in range(8):
                S.op("pe", lambda e, k=k, awt3=awt3, bk=bk: e.matmul(
                    ps[bk][0:4, :], lhsT=scT3[:, k, :], rhs=awt3[:, k, :], start=(k == 0), stop=(k == 7)),
                    reads=[scTB, awtB], writes=[psb[bk]])
            S.op("dve", lambda e, blk=blk, bk=bk: e.tensor_tensor(
                out=mods[:, blk * 512:(blk + 1) * 512], in0=ps[bk][0:4, :],
                in1=adab[:, blk * 512:(blk + 1) * 512], op=ALU.add),
                reads=[psb[bk], adabB], writes=[modsB])
        store(MODS[l], mods, modsB, MODSB)

    def mod_row(l, s, j):
        return MODS[l, s:s + 1, j * D:(j + 1) * D]

    def phase_a(l):
        A.reset(PERSIST)
        win, winB = A.alloc("win", 8 * 4000, BF16)
        win3 = win.rearrange("p (k n) -> p k n", k=8)
        wst = [A.alloc(f"wst{i}", 8 * 500, F32) for i in range(2)]
        wsrc = w_in[l].rearrange("(k p) n -> p k n", p=128)
        for blk in range(8):
            st, stB = wst[blk % 2]
            st3 = st.rearrange("p (k n) -> p k n", k=8)
            load(st3, stB, wsrc[:, :, blk * 500:(blk + 1) * 500])
            S.op("pool", lambda e, st3=st3, blk=blk: e.tensor_copy(
                out=win3[:, :, blk * 500:(blk + 1) * 500], in_=st3), reads=[stB], writes=[winB])
        gq, gqB = A.alloc("gq", 256, F32)
        gkv, gkvB = A.alloc("gkv", 128, F32)
        bload(gq, gqB, g_q[l:l + 1, :])
        bload(gkv, gkvB, g_kv[l:l + 1, :])
        gm, gmB = A.alloc("gm", D, F32)
        bload(gm, gmB, g_mix[l:l + 1, :])
        cosk, coskB = A.alloc("cosk", NT * 16, F32)
        sink, sinkB = A.alloc("sink", NT * 16, F32)
        load(cosk, coskB, cosk_in)
        load(sink, sinkB, sink_in)
        G1, G1B = A.alloc("G1", D, F32)
        SH1, SH1B = A.alloc("SH1", D, F32)
        xt = [A.alloc(f"xt{i}", D, F32) for i in range(2)]
        junk, junkB = A.alloc("junk", D, F32)
        tmp, tmpB = A.alloc("tmp", D, F32)
        hb = [A.alloc(f"hb{i}", D, BF16) for i in range(2)]
        hT = [A.alloc(f"hT{i}", 8 * 512, BF16) for i in range(2)]
        ss, ssB = A.alloc("ss", 4, F32)
        rstd, rstdB = A.alloc("rstd", 4, F32)
        qst = [A.alloc(f"qst{i}", 512, BF16) for i in range(2)]
        vst = [A.alloc(f"vst{i}", 512, BF16) for i in range(2)]
        gst = [A.alloc(f"gst{i}", 2048, BF16) for i in range(2)]
        latb, latbB = A.alloc("latb", 416, BF16)
        rt, rtB = A.alloc("rt", 64, F32)
        latT = [A.alloc(f"latT{i}", 4 * 512, BF16) for i in range(2)]
        tokbank = [3, 4, 5]
        tokctr = [0]
        fmctr = [0]

        def nexttok():
            b = tokbank[tokctr[0] % 3]
            tokctr[0] += 1
            return b

        ss2, ss2B = A.alloc("ss2", 4, F32)
        rstd2, rstd2B = A.alloc("rstd2", 4, F32)
        junk2, junk2B = A.alloc("junk2", 384, F32)

        hTs = [[Buf(f"hTs{i}_{j}") for j in range(4)] for i in range(2)]

        def prep_a(t):
            if t % SLOT_T == 0:
                s_ = t // SLOT_T
                bload(G1, G1B, mod_row(l, s_, 1), reads=[MODSB])
                bload(SH1, SH1B, mod_row(l, s_, 0), reads=[MODSB])
                S.op("dve", lambda e: e.scalar_tensor_tensor(
                    out=G1, in0=G1, scalar=1.0, in1=gm, op0=ALU.add, op1=ALU.mult),
                    reads=[G1B, gmB], writes=[G1B])
            xti, xtiB = xt[t % 2]
            src = x_in if l == 0 else X
            load(xti, xtiB, src[t * 128:(t + 1) * 128, :], reads=([] if l == 0 else [XB[t]]))
            S.op("act", lambda e: e.activation(out=junk, in_=xti, func=AF.Square, accum_out=ss[:, 0:1]),
                 reads=[xtiB], writes=[junkB, ssB])
            rms_rstd(ss[:, 0:1], ssB, D, rstd[:, 0:1], rstdB)
            S.op("dve", lambda e: e.scalar_tensor_tensor(
                out=tmp, in0=xti, scalar=rstd[:, 0:1], in1=G1, op0=ALU.mult, op1=ALU.mult),
                reads=[xtiB, rstdB, G1B], writes=[tmpB])
            hbi, hbiB = hb[t % 2]
            S.op("pool", lambda e: e.tensor_tensor(out=hbi, in0=tmp, in1=SH1, op=ALU.add),
                 reads=[tmpB, SH1B], writes=[hbiB])

        def tr_a(t):
            g, j = t // 4, t % 4
            hTg, hTgB = hT[g % 2]
            hT3 = hTg.rearrange("p (k t) -> p k t", k=8)
            hbi, hbiB = hb[t % 2]
            for k in range(8):
                S.op("pe", lambda e, k=k: e.transpose(
                    out=psbf[0][:, k * 128:(k + 1) * 128], in_=hbi[:, k * 128:(k + 1) * 128],
                    identity=identb), reads=[hbiB, identbB], writes=[psb[0]])
            S.op("act", lambda e: e.copy(
                out=hT3[:, :, j * 128:(j + 1) * 128],
                in_=psbf[0].rearrange("p (k t) -> p k t", k=8)), reads=[psb[0]], writes=[hTs[g % 2][j]])

        def mm_a(t):
            g, j = t // 4, t % 4
            hTg, hTgB = hT[g % 2]
            hT3 = hTg.rearrange("p (k t) -> p k t", k=8)
            lT, lTB = latT[g % 2]
            lT3 = lT.rearrange("p (c t) -> p c t", c=4)
            bk = nexttok()
            for k in range(8):
                S.op("pe", lambda e, k=k, bk=bk: e.matmul(
                    ps[bk], lhsT=hT3[:, k, j * 128:(j + 1) * 128], rhs=win3[:, k, IN_V:IN_V + 512],
                    start=(k == 0), stop=(k == 7)), reads=[hTs[g % 2][j], hTgB, winB], writes=[psb[bk]])
            vs, vsB = vst[t % 2]
            S.op("dve", lambda e, bk=bk: e.tensor_copy(out=vs, in_=ps[bk]), reads=[psb[bk]], writes=[vsB])
            store(VNA[t * 128:(t + 1) * 128, :], vs, vsB, VNAB)
            gs, gsB = gst[t % 2]
            for q4 in range(4):
                bk = nexttok()
                for k in range(8):
                    S.op("pe", lambda e, k=k, bk=bk, q4=q4: e.matmul(
                        ps[bk], lhsT=hT3[:, k, j * 128:(j + 1) * 128],
                        rhs=win3[:, k, IN_G + q4 * 512:IN_G + (q4 + 1) * 512],
                        start=(k == 0), stop=(k == 7)), reads=[hTs[g % 2][j], hTgB, winB], writes=[psb[bk]])
                S.op("act", lambda e, bk=bk, q4=q4: e.activation(
                    out=gs[:, q4 * 512:(q4 + 1) * 512], in_=ps[bk], func=AF.Sigmoid),
                    reads=[psb[bk]], writes=[gsB])
            store(GATE[t * 128:(t + 1) * 128, :], gs, gsB, GATEB)
            bk = nexttok()
            for k in range(8):
                S.op("pe", lambda e, k=k, bk=bk: e.matmul(
                    ps[bk][:, 0:416], lhsT=hT3[:, k, j * 128:(j + 1) * 128], rhs=win3[:, k, IN_CQ:IN_CQ + 416],
                    start=(k == 0), stop=(k == 7)), reads=[hTs[g % 2][j], hTgB, winB], writes=[psb[bk]])
            lat = ps[bk]
            latB = psb[bk]
            S.op("act", lambda e: e.activation(out=junk2[:, 0:256], in_=lat[:, 0:256], func=AF.Square,
                                               accum_out=ss2[:, 1:2]), reads=[latB], writes=[junk2B, ss2B])
            S.op("act", lambda e: e.activation(out=junk2[:, 256:384], in_=lat[:, 256:384],
                                               func=AF.Square, accum_out=ss2[:, 2:3]),
                 reads=[latB], writes=[junk2B, ss2B])
            rms_rstd(ss2[:, 1:2], ss2B, 256, rstd2[:, 1:2], rstd2B)
            rms_rstd(ss2[:, 2:3], ss2B, 128, rstd2[:, 2:3], rstd2B)
            S.op("dve", lambda e: e.scalar_tensor_tensor(
                out=latb[:, 0:256], in0=lat[:, 0:256], scalar=rstd2[:, 1:2], in1=gq,
                op0=ALU.mult, op1=ALU.mult), reads=[latB, rstd2B, gqB], writes=[latbB])
            S.op("dve", lambda e: e.scalar_tensor_tensor(
                out=latb[:, 256:384], in0=lat[:, 256:384], scalar=rstd2[:, 2:3], in1=gkv,
                op0=ALU.mult, op1=ALU.mult), reads=[latB, rstd2B, gkvB], writes=[latbB])
            ck = cosk[:, t * 16:(t + 1) * 16]
            sk = sink[:, t * 16:(t + 1) * 16]
            x1 = lat[:, 384:400]
            x2 = lat[:, 400:416]
            for (o_, a_, b_) in ((0, x1, ck), (16, x2, sk), (32, x1, sk), (48, x2, ck)):
                S.op("dve", lambda e, o_=o_, a_=a_, b_=b_: e.tensor_tensor(
                    out=rt[:, o_:o_ + 16], in0=a_, in1=b_, op=ALU.mult),
                    reads=[latB, coskB, sinkB], writes=[rtB])
            S.op("dve", lambda e: e.tensor_tensor(out=latb[:, 384:400], in0=rt[:, 0:16], in1=rt[:, 16:32],
                                                  op=ALU.subtract), reads=[rtB], writes=[latbB])
            S.op("dve", lambda e: e.tensor_tensor(out=latb[:, 400:416], in0=rt[:, 32:48], in1=rt[:, 48:64],
                                                  op=ALU.add), reads=[rtB], writes=[latbB])
            for c4 in range(3):
                S.op("pe", lambda e, c4=c4: e.transpose(
                    out=psbf[1][:, c4 * 128:(c4 + 1) * 128], in_=latb[:, c4 * 128:(c4 + 1) * 128],
                    identity=identb), reads=[latbB, identbB], writes=[psb[1]])
            S.op("pe", lambda e: e.transpose(out=psbf[1][0:32, 384:512], in_=latb[:, 384:416], identity=identb),
                 reads=[latbB, identbB], writes=[psb[1]])
            S.op("act", lambda e: e.copy(
                out=lT3[:, 0:3, j * 128:(j + 1) * 128],
                in_=psbf[1][:, 0:384].rearrange("p (c t) -> p c t", c=3)), reads=[psb[1]], writes=[lTB])
            S.op("act", lambda e: e.copy(
                out=lT3[0:32, 3, j * 128:(j + 1) * 128], in_=psbf[1][0:32, 384:512]),
                reads=[psb[1]], writes=[lTB])

        def fm_a(g):
            hTg, hTgB = hT[g % 2]
            hT3 = hTg.rearrange("p (k t) -> p k t", k=8)
            lT, lTB = latT[g % 2]
            lT3 = lT.rearrange("p (c t) -> p c t", c=4)
            gsl = slice(g * 512, (g + 1) * 512)
            for which, col0, scale, dst, dstB in ((0, IN_Q, 0.125, QA, QAB), (1, IN_K, 1.0, KA, KAB)):
                for m in range(4):
                    bk = 6 + fmctr[0] % 2
                    fmctr[0] += 1
                    for k in range(8):
                        S.op("pe", lambda e, k=k, m=m, bk=bk, col0=col0: e.matmul(
                            ps[bk], lhsT=win3[:, k, col0 + m * 128:col0 + (m + 1) * 128], rhs=hT3[:, k, :],
                            start=(k == 0), stop=(k == 7)), reads=hTs[g % 2] + [hTgB, winB], writes=[psb[bk]])
                    qs, qsB = qst[fmctr[0] % 2]
                    S.op("act", lambda e, qs=qs, bk=bk, scale=scale: e.activation(
                        out=qs, in_=ps[bk], func=AF.Copy, scale=scale), reads=[psb[bk]], writes=[qsB])
                    store(dst[2 * m:2 * m + 2, :, gsl].rearrange("h d t -> (h d) t"), qs, qsB, dstB)
            store(CQT[0:128, gsl], lT3[:, 0, :], lTB, CQTB)
            store(CQT[128:256, gsl], lT3[:, 1, :], lTB, CQTB)
            store(CKVT[:, gsl], lT3[:, 2, :], lTB, CKVTB)
            store(KRT[:, gsl], lT3[0:32, 3, :], lTB, KRTB)

        prep_a(0)
        tr_a(0)
        for t in range(NT):
            if t + 1 < NT:
                prep_a(t + 1)
            mm_a(t)
            if t + 1 < NT:
                tr_a(t + 1)
            if t % 4 == 3:
                fm_a(t // 4)

    def phase_c1(l):
        A.reset(PERSIST)
        cq, cqB = A.alloc("cq", 2 * T, BF16)
        cq3 = cq.rearrange("p (c t) -> p c t", c=2)
        ckv, ckvB = A.alloc("ckv", T, BF16)
        load(cq3[:, 0, :], cqB, CQT[0:128, :], reads=[CQTB])
        load(cq3[:, 1, :], cqB, CQT[128:256, :], reads=[CQTB])
        load(ckv, ckvB, CKVT, reads=[CKVTB])
        wst, wstB = A.alloc("wst", 2 * 768, F32)
        wq, wqB = A.alloc("wq", 2 * 768, BF16)
        wqr, wqrB = A.alloc("wqr", 2 * 768, BF16)
        wk, wkB = A.alloc("wk", 512, BF16)
        wv, wvB = A.alloc("wv", 512, BF16)
        wst3 = wst.rearrange("p (k n) -> p k n", k=2)
        wq3 = wq.rearrange("p (k n) -> p k n", k=2)
        wqr3 = wqr.rearrange("p (k n) -> p k n", k=2)
        load(wst3, wstB, w_uq[l].rearrange("(k p) n -> p k n", p=128))
        S.op("dve", lambda e: e.tensor_copy(out=wq, in_=wst), reads=[wstB], writes=[wqB])
        load(wst3, wstB, w_uqr[l].rearrange("(k p) n -> p k n", p=128))
        S.op("dve", lambda e: e.tensor_copy(out=wqr, in_=wst), reads=[wstB], writes=[wqrB])
        load(wst[:, 0:512], wstB, w_ukvk[l])
        S.op("dve", lambda e: e.tensor_copy(out=wk, in_=wst[:, 0:512]), reads=[wstB], writes=[wkB])
        load(wst[:, 0:512], wstB, w_ukvv[l])
        S.op("dve", lambda e: e.tensor_copy(out=wv, in_=wst[:, 0:512]), reads=[wstB], writes=[wvB])
        cs = [A.alloc(f"cs{i}", 512, F32, parts=96) for i in range(2)]
        sn = [A.alloc(f"sn{i}", 512, F32, parts=96) for i in range(2)]
        qst = [A.alloc(f"qst{i}", 512, BF16, parts=96) for i in range(2)]
        kst = [A.alloc(f"kst{i}", 512, BF16, parts=64) for i in range(2)]
        vst = [A.alloc(f"vst{i}", 512, BF16) for i in range(2)]
        t1, t1B = A.alloc("t1", 512, F32, parts=96)
        t2, t2B = A.alloc("t2", 512, F32, parts=96)
        ctr = 0
        for g in range(NG):
            gsl = slice(g * 512, (g + 1) * 512)
            csg, csgB = cs[g % 2]
            sng, sngB = sn[g % 2]
            load(csg[64:96, :], csgB, cosq_in[:, gsl])
            load(sng[64:96, :], sngB, sinq_in[:, gsl])
            for h in range(8):
                ba, bb, bk_ = 0 + 3 * (ctr % 2), 1 + 3 * (ctr % 2), 2 + 3 * (ctr % 2)
                for k in range(2):
                    S.op("pe", lambda e, k=k, h=h, ba=ba, gsl=gsl: e.matmul(
                        ps[ba][0:96, :], lhsT=wq3[:, k, h * 96:(h + 1) * 96], rhs=cq3[:, k, gsl],
                        start=(k == 0), stop=(k == 1)), reads=[wqB, cqB], writes=[psb[ba]])
                for k in range(2):
                    S.op("pe", lambda e, k=k, h=h, bb=bb, gsl=gsl: e.matmul(
                        ps[bb][0:96, :], lhsT=wqr3[:, k, h * 96:(h + 1) * 96], rhs=cq3[:, k, gsl],
                        start=(k == 0), stop=(k == 1)), reads=[wqrB, cqB], writes=[psb[bb]])
                S.op("pe", lambda e, h=h, bk_=bk_, gsl=gsl: e.matmul(
                    ps[bk_][0:64, :], lhsT=wk[:, h * 64:(h + 1) * 64], rhs=ckv[:, gsl], start=True, stop=True),
                    reads=[wkB, ckvB], writes=[psb[bk_]])
                qs, qsB = qst[ctr % 2]
                ks, ksB = kst[ctr % 2]
                S.op("act", lambda e, qs=qs, ba=ba: e.copy(out=qs[0:64, :], in_=ps[ba][0:64, :]),
                     reads=[psb[ba]], writes=[qsB])
                S.op("dve", lambda e, ba=ba, csg=csg: e.tensor_tensor(
                    out=t1[64:96, :], in0=ps[ba][64:96, :], in1=csg[64:96, :], op=ALU.mult),
                    reads=[psb[ba], csgB], writes=[t1B])
                S.op("dve", lambda e, bb=bb, sng=sng: e.tensor_tensor(
                    out=t2[64:96, :], in0=ps[bb][64:96, :], in1=sng[64:96, :], op=ALU.mult),
                    reads=[psb[bb], sngB], writes=[t2B])
                S.op("dve", lambda e, qs=qs: e.tensor_tensor(
                    out=qs[64:96, :], in0=t1[64:96, :], in1=t2[64:96, :], op=ALU.add),
                    reads=[t1B, t2B], writes=[qsB])
                S.op("act", lambda e, ks=ks, bk_=bk_: e.copy(out=ks, in_=ps[bk_][0:64, :]),
                     reads=[psb[bk_]], writes=[ksB])
                store(QM[h, :, gsl], qs, qsB, QMB)
                store(KM[h, :, gsl], ks, ksB, KMB)
                ctr += 1
        for t in range(NT):
            bk = 6 + t % 2
            S.op("pe", lambda e, t=t, bk=bk: e.matmul(
                ps[bk][:, :], lhsT=ckv[:, t * 128:(t + 1) * 128], rhs=wv, start=True, stop=True),
                reads=[ckvB, wvB], writes=[psb[bk]])
            vs, vsB = vst[t % 2]
            S.op("dve", lambda e, vs=vs, bk=bk: e.tensor_copy(out=vs, in_=ps[bk][:, :]),
                 reads=[psb[bk]], writes=[vsB])
            store(VM[:, t * 128:(t + 1) * 128, :].rearrange("h t d -> t h d"),
                  vs.rearrange("p (h d) -> p h d", h=8), vsB, VMB)

    def phase_c2(l, QT, KT, first):
        A.reset(ATT_MARK)
        V = [A.alloc(f"V{i}", NT * 65, BF16) for i in range(2)]
        PT = [A.alloc(f"PT{i}", 1024, BF16) for i in range(3)]
        otf, otfB = A.alloc("otf", 512, F32, parts=65)
        rc, rcB = A.alloc("rc", 4, F32)
        mo = [A.alloc(f"mo{i}", 4 * 64, BF16) for i in range(2)]
        for i in range(2):
            Vi, ViB = V[i]
            S.op("pool", lambda e, Vi=Vi: e.memset(Vi, 1.0), writes=[ViB])
        sc = 96.0 ** -0.5
        for i in range(2):
            load(KT[i][0][64:96, :], KT[i][1], KRT, reads=[KRTB])
        pctr = 0
        octr = 0
        NKP = NT // 2
        deferred = []
        for h in range(8):
            QTh, QThB = QT[h % 2]
            KTh, KThB = KT[h % 2]
            Vh, VhB = V[h % 2]
            Vh3 = Vh.rearrange("p (t d) -> p t d", d=65)
            load(QTh[0:96, :], QThB, QM[h], reads=[QMB])
            load(KTh[0:64, :], KThB, KM[h], reads=[KMB])
            S.dma("sp", lambda e, Vh3=Vh3, h=h: e.dma_start(
                out=Vh3[:, :, 0:64], in_=VM[h].rearrange("(t p) d -> p t d", p=128)), VhB,
                reads=[VMB], writes=[VhB])
            for g in range(NG):
                gsl = slice(g * 512, (g + 1) * 512)
                ob = 6 + octr % 2
                octr += 1
                pend = []

                def emit_pv(item, ob=ob, Vh3=Vh3, VhB=VhB):
                    kp, ppt, pptB = item
                    for u in range(2):
                        kt = 2 * kp + u
                        S.op("pe", lambda e, kt=kt, u=u, ppt=ppt, Vh3=Vh3, ob=ob: e.matmul(
                            ps[ob][0:65, :], lhsT=Vh3[:, kt, :], rhs=ppt[:, u * 512:(u + 1) * 512],
                            start=(kt == 0), stop=(kt == NT - 1)),
                            reads=[VhB, pptB], writes=[psb[ob]])

                for kp in range(NKP):
                    sj = pctr % 2
                    pt, ptB = PT[pctr % 3]
                    pctr += 1
                    for u in range(2):
                        kt = 2 * kp + u
                        S.op("pe", lambda e, kt=kt, u=u, sj=sj, KTh=KTh, QTh=QTh, gsl=gsl: e.matmul(
                            ps[2 * sj + u], lhsT=KTh[0:100, kt * 128:(kt + 1) * 128], rhs=QTh[0:100, gsl],
                            start=True, stop=True), reads=[KThB, QThB], writes=[psb[2 * sj], psb[2 * sj + 1]])
                    S.op("act", lambda e, pt=pt, sj=sj: e.activation(
                        out=pt, in_=ps2[sj], func=AF.Exp, scale=sc),
                        reads=[psb[2 * sj], psb[2 * sj + 1]], writes=[ptB])
                    pend.append((kp, pt, ptB))
                    if len(pend) > 1:
                        emit_pv(pend.pop(0))
                    if kp == min(2, NKP - 1) and deferred:
                        deferred.pop(0)()
                while pend:
                    emit_pv(pend.pop(0))
                def epilogue(ob=ob, gsl=gsl, h=h, octr=octr):
                    S.op("dve", lambda e: e.tensor_copy(out=otf, in_=ps[ob][0:65, :]),
                         reads=[psb[ob]], writes=[otfB])
                    for j in range(4):
                        S.op("pe", lambda e, j=j: e.transpose(
                            out=ps[5][:, j * 65:(j + 1) * 65], in_=otf[:, j * 128:(j + 1) * 128],
                            identity=identf[0:65, 0:65]), reads=[otfB, identfB], writes=[psb[5]])
                    p5 = ps[5][:, 0:260].rearrange("p (j d) -> p j d", d=65)
                    S.op("dve", lambda e: e.reciprocal(out=rc, in_=p5[:, :, 64]), reads=[psb[5]], writes=[rcB])
                    mg, mgB = mo[octr % 2]
                    mg3 = mg.rearrange("p (j d) -> p j d", d=64)
                    S.op("dve", lambda e: e.tensor_tensor(
                        out=mg3, in0=p5[:, :, 0:64], in1=rc.unsqueeze(2).broadcast_to([128, 4, 64]), op=ALU.mult),
                        reads=[psb[5], rcB], writes=[mgB])
                    store(MLAO[gsl, h * 64:(h + 1) * 64].rearrange("(j p) d -> p j d", p=128), mg3, mgB, MLAOB)
                deferred.append(epilogue)

        while deferred:
            deferred.pop(0)()

    def phase_b(l, QN, KN):
        A.reset(ATT_MARK)
        VN = [A.alloc(f"VN{i}", NT * 65, BF16) for i in range(2)]
        ttf, ttfB = A.alloc("ttf", 9 * 128, F32)
        ttb = [A.alloc(f"ttb{i}", 9 * 128, BF16) for i in range(2)]
        PN = [A.alloc(f"PN{i}", 9 * 128, BF16) for i in range(2)]
        rc, rcB = A.alloc("rcn", 1, F32)
        no = [A.alloc(f"no{i}", 4 * 64, BF16) for i in range(2)]
        for i in range(2):
            Vi, ViB = VN[i]
            S.op("pool", lambda e, Vi=Vi: e.memset(Vi, 1.0), writes=[ViB])
        pctr = 0
        for h in range(8):
            QNh, QNhB = QN[h % 2]
            KNh, KNhB = KN[h % 2]
            Vh, VhB = VN[h % 2]
            Vh3 = Vh.rearrange("p (t d) -> p t d", d=65)
            tb, tbB = ttb[h % 2]
            tb3 = tb.rearrange("p (d q) -> p d q", d=9)
            load(QNh[0:64, :], QNhB, QA[h], reads=[QAB])
            load(KNh[0:64, :], KNhB, KA[h], reads=[KAB])
            S.dma("sp", lambda e, Vh3=Vh3, h=h: e.dma_start(
                out=Vh3[:, :, 0:64], in_=VNA[:, h * 64:(h + 1) * 64].rearrange("(t p) d -> p t d", p=128)), VhB,
                reads=[VNAB], writes=[VhB])
            load(ttf, ttfB, tt_in[l, h])
            S.op("pool", lambda e, tb=tb: e.tensor_copy(out=tb, in_=ttf), reads=[ttfB], writes=[tbB])
            def emit_s(p, KNh=KNh, KNhB=KNhB, QNh=QNh, QNhB=QNhB, tb3=tb3, tbB=tbB):
                tiles = [a for a in range(p - 4, p + 5) if 0 <= a < NT]
                pn, pnB = PN[p % 2]
                sb0 = 3 * (p % 2)
                psl = slice(p * 128, (p + 1) * 128)
                for i, a in enumerate(tiles):
                    bk = sb0 + i // 4
                    csl = slice((i % 4) * 128, (i % 4 + 1) * 128)
                    S.op("pe", lambda e, a=a, bk=bk, csl=csl, psl=psl: e.matmul(
                        ps[bk][:, csl], lhsT=KNh[0:96, a * 128:(a + 1) * 128], rhs=QNh[0:96, psl],
                        start=True, stop=False), reads=[KNhB, QNhB], writes=[psb[bk]])
                    S.op("pe", lambda e, a=a, bk=bk, csl=csl, p=p: e.matmul(
                        ps[bk][:, csl], lhsT=identb, rhs=tb3[:, a - p + 4, :], start=False, stop=True),
                        reads=[identbB, tbB], writes=[psb[bk]])
                nt_ = len(tiles)
                for b0 in range(0, nt_, 4):
                    n4 = min(4, nt_ - b0)
                    bk = sb0 + b0 // 4
                    S.op("act", lambda e, pn=pn, bk=bk, b0=b0, n4=n4: e.activation(
                        out=pn[:, b0 * 128:(b0 + n4) * 128], in_=ps[bk][:, 0:n4 * 128], func=AF.Exp),
                        reads=[psb[bk]], writes=[pnB])

            def emit_pv(p, Vh3=Vh3, VhB=VhB, h=h):
                tiles = [a for a in range(p - 4, p + 5) if 0 <= a < NT]
                nt_ = len(tiles)
                pn, pnB = PN[p % 2]
                ob = 6 + p % 2
                for i, a in enumerate(tiles):
                    S.op("pe", lambda e, i=i, a=a, pn=pn, ob=ob, nt_=nt_: e.matmul(
                        ps[ob][:, 0:65], lhsT=pn[:, i * 128:(i + 1) * 128], rhs=Vh3[:, a, :],
                        start=(i == 0), stop=(i == nt_ - 1)), reads=[pnB, VhB], writes=[psb[ob]])
                S.op("dve", lambda e, ob=ob: e.reciprocal(out=rc, in_=ps[ob][:, 64:65]),
                     reads=[psb[ob]], writes=[rcB])
                ng, ngB = no[(p // 4) % 2]
                S.op("dve", lambda e, ob=ob, ng=ng, p=p: e.tensor_scalar(
                    out=ng[:, (p % 4) * 64:(p % 4 + 1) * 64], in0=ps[ob][:, 0:64], scalar1=rc[:, 0:1],
                    scalar2=None, op0=ALU.mult), reads=[psb[ob], rcB], writes=[ngB])
                if p % 4 == 3:
                    p0 = p - 3
                    store(NAO[p0 * 128:(p0 + 4) * 128, h * 64:(h + 1) * 64].rearrange("(j p) d -> p j d", p=128),
                          ng.rearrange("p (j d) -> p j d", d=64), ngB, NAOB)

            emit_s(0)
            for p in range(NT):
                if p + 1 < NT:
                    emit_s(p + 1)
                emit_pv(p)

    def load_w_bf16(dst3, dstB, src3, st, stB, ncols, k, eng="pool"):
        for c0 in range(0, ncols, 512):
            cw = min(512, ncols - c0)
            st3 = st[:, 0:k * cw].rearrange("p (k n) -> p k n", k=k)
            load(st3, stB, src3[:, :, c0:c0 + cw])
            S.op(eng, lambda e, st3=st3, c0=c0, cw=cw: e.tensor_copy(out=dst3[:, :, c0:c0 + cw], in_=st3),
                 reads=[stB], writes=[dstB])

    def phase_de(l, R):
        A.reset(MOE_MARK)
        st, stB = A.alloc("st", 8 * 512, F32)
        wna, wnaB = A.alloc("wna", 4 * D, BF16)
        wml, wmlB = A.alloc("wml", 4 * D, BF16)
        wo, woB = A.alloc("wo", 8 * D, BF16)
        wr, wrB = A.alloc("wr", 8 * 36, BF16)
        wna3 = wna.rearrange("p (k n) -> p k n", k=4)
        wml3 = wml.rearrange("p (k n) -> p k n", k=4)
        wo3 = wo.rearrange("p (k n) -> p k n", k=8)
        wr3 = wr.rearrange("p (k n) -> p k n", k=8)
        load_w_bf16(wna3, wnaB, w_nao[l].rearrange("(k p) n -> p k n", p=128), st, stB, D, 4)
        load_w_bf16(wml3, wmlB, w_mlao[l].rearrange("(k p) n -> p k n", p=128), st, stB, D, 4)
        load_w_bf16(wo3, woB, w_out[l].rearrange("(k p) n -> p k n", p=128), st, stB, D, 8)
        load_w_bf16(wr3, wrB, w_r[l].rearrange("(k p) n -> p k n", p=128), st, stB, 36, 8)
        brt, brtB = A.alloc("brt", 36, F32)
        bload(brt, brtB, b_r[l:l + 1, :])
        gf, gfB = A.alloc("gf", D, F32)
        bload(gf, gfB, g_ffn[l:l + 1, :])
        g1b, g1bB = A.alloc("g1b", D, F32)
        G2, G2B = A.alloc("G2", D, F32)
        SH2, SH2B = A.alloc("SH2", D, F32)
        ao = [A.alloc(f"ao{i}", 1024, BF16) for i in range(2)]
        gt = [A.alloc(f"gt{i}", 2048, BF16) for i in range(2)]
        xt = [A.alloc(f"xd{i}", D, F32) for i in range(2)]
        aT, aTB = A.alloc("aT", 8 * 128, BF16)
        m1, m1B = A.alloc("m1", D, F32)
        m2, m2B = A.alloc("m2", D, F32)
        mgb, mgbB = A.alloc("mgb", D, BF16)
        mT, mTB = A.alloc("mT", 8 * 128, BF16)
        xn, xnB = A.alloc("xn", D, F32)
        junk, junkB = A.alloc("junkd", D, F32)
        ss, ssB = A.alloc("ssd", 1, F32)
        rstd, rstdB = A.alloc("rstdd", 1, F32)
        h2b = [A.alloc(f"h2b{i}", D, BF16) for i in range(2)]
        h2T, h2TB = A.alloc("h2T", 8 * 128, BF16)
        sm, smB = A.alloc("sm", 256, F32)
        ab, abB = A.alloc("ab", 32, BF16)
        S.op("dve", lambda e: e.memset(R["carry"], 0.0), writes=[R["carryB"]])
        mgbs = [A.alloc(f"mgb{i}", D, BF16) for i in range(2)]
        zt, ztB = A.alloc("zt", D, F32)
        aT3 = aT.rearrange("p (k t) -> p k t", k=8)
        mT3 = mT.rearrange("p (k t) -> p k t", k=8)
        h2T3 = h2T.rearrange("p (k t) -> p k t", k=8)

        def s1(t):
            tsl = slice(t * 128, (t + 1) * 128)
            aoi, aoiB = ao[t % 2]
            gti, gtiB = gt[t % 2]
            xti, xtiB = xt[t % 2]
            load(aoi[:, 0:512], aoiB, NAO[tsl, :], reads=[NAOB])
            load(aoi[:, 512:1024], aoiB, MLAO[tsl, :], reads=[MLAOB])
            load(gti, gtiB, GATE[tsl, :], reads=[GATEB])
            src = x_in if l == 0 else X
            load(xti, xtiB, src[tsl, :], reads=([] if l == 0 else [XB[t]]))
            for k in range(8):
                S.op("pe", lambda e, k=k: e.transpose(
                    out=psbf[0][:, k * 128:(k + 1) * 128], in_=aoi[:, k * 128:(k + 1) * 128], identity=identb),
                    reads=[aoiB, identbB], writes=[psb[0]])
            S.op("act", lambda e: e.copy(out=aT, in_=psbf[0]), reads=[psb[0]], writes=[aTB])
            for hf in range(2):
                hs = slice(hf * 512, (hf + 1) * 512)
                for (w3_, wB_, bk, k0) in ((wna3, wnaB, 1, 0), (wml3, wmlB, 2, 4)):
                    for k in range(4):
                        S.op("pe", lambda e, k=k, w3_=w3_, bk=bk, k0=k0, hs=hs: e.matmul(
                            ps[bk], lhsT=aT3[:, k0 + k, :], rhs=w3_[:, k, hs],
                            start=(k == 0), stop=(k == 3)), reads=[aTB, wB_], writes=[psb[bk]])
                S.op("dve", lambda e, hs=hs: e.tensor_tensor(
                    out=m1[:, hs], in0=ps[1], in1=gti[:, hs], op=ALU.mult),
                    reads=[psb[1], gtiB], writes=[m1B])
                S.op("dve", lambda e, hf=hf, hs=hs: e.tensor_tensor(
                    out=m2[:, hs], in0=ps[2], in1=gti[:, 1024 + hf * 512:1024 + (hf + 1) * 512],
                    op=ALU.mult), reads=[psb[2], gtiB], writes=[m2B])
            mg_, mg_B = mgbs[t % 2]
            S.op("pool", lambda e: e.tensor_tensor(out=mg_, in0=m1, in1=m2, op=ALU.add),
                 reads=[m1B, m2B], writes=[mg_B])

        def s2(t):
            tsl = slice(t * 128, (t + 1) * 128)
            if t % SLOT_T == 0:
                s_ = t // SLOT_T
                bload(g1b, g1bB, mod_row(l, s_, 2), reads=[MODSB])
                bload(G2, G2B, mod_row(l, s_, 4), reads=[MODSB])
                bload(SH2, SH2B, mod_row(l, s_, 3), reads=[MODSB])
                S.op("dve", lambda e: e.scalar_tensor_tensor(
                    out=G2, in0=G2, scalar=1.0, in1=gf, op0=ALU.add, op1=ALU.mult),
                    reads=[G2B, gfB], writes=[G2B])
            xti, xtiB = xt[t % 2]
            mg_, mg_B = mgbs[t % 2]
            for k in range(8):
                S.op("pe", lambda e, k=k: e.transpose(
                    out=psbf[3][:, k * 128:(k + 1) * 128], in_=mg_[:, k * 128:(k + 1) * 128], identity=identb),
                    reads=[mg_B, identbB], writes=[psb[3]])
            S.op("act", lambda e: e.copy(out=mT, in_=psbf[3]), reads=[psb[3]], writes=[mTB])
            for hf in range(2):
                bk = 4 + hf
                hs = slice(hf * 512, (hf + 1) * 512)
                for k in range(8):
                    S.op("pe", lambda e, k=k, bk=bk, hs=hs: e.matmul(
                        ps[bk], lhsT=mT3[:, k, :], rhs=wo3[:, k, hs],
                        start=(k == 0), stop=(k == 7)), reads=[mTB, woB], writes=[psb[bk]])
                S.op("dve", lambda e, bk=bk, hs=hs: e.tensor_tensor(
                    out=zt[:, hs], in0=ps[bk], in1=g1b[:, hs], op=ALU.mult),
                    reads=[psb[bk], g1bB], writes=[ztB])
            S.op("pool", lambda e: e.tensor_tensor(out=xn, in0=zt, in1=xti, op=ALU.add),
                 reads=[ztB, xtiB], writes=[xnB])
            store(X[tsl, :], xn, xnB, XB[t])
            S.op("act", lambda e: e.activation(out=junk, in_=xn, func=AF.Square, accum_out=ss),
                 reads=[xnB], writes=[junkB, ssB])
            rms_rstd(ss, ssB, D, rstd, rstdB)
            S.op("dve", lambda e: e.scalar_tensor_tensor(
                out=zt, in0=xn, scalar=rstd[:, 0:1], in1=G2, op0=ALU.mult, op1=ALU.mult),
                reads=[xnB, rstdB, G2B], writes=[ztB])
            hbi, hbiB = h2b[t % 2]
            S.op("pool", lambda e: e.tensor_tensor(out=hbi, in0=zt, in1=SH2, op=ALU.add),
                 reads=[ztB, SH2B], writes=[hbiB])
            store(H2[tsl, :], hbi, hbiB, H2B)

        def s3(t):
            hbi, hbiB = h2b[t % 2]
            for k in range(8):
                S.op("pe", lambda e, k=k: e.transpose(
                    out=psbf[6][:, k * 128:(k + 1) * 128], in_=hbi[:, k * 128:(k + 1) * 128], identity=identb),
                    reads=[hbiB, identbB], writes=[psb[6]])
            S.op("act", lambda e: e.copy(out=h2T, in_=psbf[6]), reads=[psb[6]], writes=[h2TB])
            for k in range(8):
                S.op("pe", lambda e, k=k: e.matmul(
                    ps[7][:, 0:36], lhsT=h2T3[:, k, :], rhs=wr3[:, k, :], start=(k == 0), stop=(k == 7)),
                    reads=[h2TB, wrB], writes=[psb[7]])
            route_tile(t, R, ps[7], psb[7], brt, brtB, sm, smB, ab, abB)

        for i in range(NT + 2):
            if i < NT:
                s1(i)
            if 0 <= i - 1 < NT:
                s2(i - 1)
            if 0 <= i - 2 < NT:
                s3(i - 2)

    def route_tile(t, R, lgp, lgpB, brt, brtB, sm, smB, ab, abB):
        M1, M2, WA, WB, POS, carry = R["M1"], R["M2"], R["WA"], R["WB"], R["POS"], R["carry"]
        RB = R["RB"]
        M1t = M1[:, t * 32:(t + 1) * 32]
        M2t = M2[:, t * 32:(t + 1) * 32]

        def dve(fn, reads, writes):
            S.op("dve", fn, reads=reads, writes=writes)
        dve(lambda e: e.tensor_tensor(out=sm[:, 0:36], in0=lgp[:, 0:36], in1=brt, op=ALU.add),
            [lgpB, brtB], [smB])
        dve(lambda e: e.reduce_max(out=sm[:, 36:37], in_=sm[:, 0:4], axis=AX.X), [smB], [smB])
        dve(lambda e: e.tensor_scalar(out=sm[:, 37:38], in0=sm[:, 36:37], scalar1=-1.0, scalar2=None,
                                      op0=ALU.mult), [smB], [smB])
        S.op("act", lambda e: e.activation(out=sm[:, 116:120], in_=sm[:, 0:4], func=AF.Exp, bias=sm[:, 37:38],
                                           accum_out=sm[:, 38:39]), reads=[smB], writes=[smB])
        dve(lambda e: e.reciprocal(out=sm[:, 39:40], in_=sm[:, 38:39]), [smB], [smB])
        dve(lambda e: e.tensor_scalar(out=sm[:, 40:44], in0=sm[:, 0:4], scalar1=sm[:, 36:37], scalar2=None,
                                      op0=ALU.is_equal), [smB], [smB])
        dve(lambda e: e.tensor_scalar(out=sm[:, 44:48], in0=sm[:, 40:44], scalar1=-1.0, scalar2=-NEG,
                                      op0=ALU.add, op1=ALU.mult), [smB], [smB])
        dve(lambda e: e.tensor_tensor(
            out=sm[:, 48:80].rearrange("p (g e) -> p g e", g=4),
            in0=sm[:, 4:36].rearrange("p (g e) -> p g e", g=4),
            in1=sm[:, 44:48].unsqueeze(2).broadcast_to([128, 4, 8]), op=ALU.add), [smB], [smB])
        dve(lambda e: e.reduce_max(out=sm[:, 80:81], in_=sm[:, 48:80], axis=AX.X), [smB], [smB])
        dve(lambda e: e.tensor_scalar(out=M1t, in0=sm[:, 48:80], scalar1=sm[:, 80:81], scalar2=None,
                                      op0=ALU.is_equal), [smB], [RB])
        dve(lambda e: e.scalar_tensor_tensor(out=sm[:, 84:116], in0=M1t, scalar=NEG, in1=sm[:, 48:80],
                                             op0=ALU.mult, op1=ALU.add), [smB, RB], [smB])
        dve(lambda e: e.reduce_max(out=sm[:, 81:82], in_=sm[:, 84:116], axis=AX.X), [smB], [smB])
        dve(lambda e: e.tensor_scalar(out=M2t, in0=sm[:, 84:116], scalar1=sm[:, 81:82], scalar2=None,
                                      op0=ALU.is_equal), [smB], [RB])
        dve(lambda e: e.tensor_tensor(out=sm[:, 82:83], in0=sm[:, 80:81], in1=sm[:, 81:82], op=ALU.subtract),
            [smB], [smB])
        S.op("act", lambda e: e.activation(out=sm[:, 83:84], in_=sm[:, 82:83], func=AF.Sigmoid),
             reads=[smB], writes=[smB])
        dve(lambda e: e.tensor_tensor(out=WA[:, t:t + 1], in0=sm[:, 83:84], in1=sm[:, 39:40], op=ALU.mult),
            [smB], [RB])
        dve(lambda e: e.tensor_tensor(out=WB[:, t:t + 1], in0=sm[:, 39:40], in1=WA[:, t:t + 1], op=ALU.subtract),
            [smB, RB], [RB])
        dve(lambda e: e.tensor_tensor(out=ab, in0=M1t, in1=M2t, op=ALU.add), [RB], [abB])
        S.op("pe", lambda e: e.matmul(lgp[:, 64:96], lhsT=tril, rhs=ab, start=True, stop=True),
             reads=[trilB, abB], writes=[lgpB])
        S.op("pe", lambda e: e.matmul(lgp[:, 96:128], lhsT=onesb, rhs=ab, start=True, stop=True),
             reads=[onesbB, abB], writes=[lgpB])
        dve(lambda e: e.tensor_tensor(out=POS[:, t * 32:(t + 1) * 32], in0=lgp[:, 64:96], in1=carry, op=ALU.add),
            [lgpB, R["carryB"]], [RB])
        dve(lambda e: e.tensor_tensor(out=carry, in0=lgp[:, 96:128], in1=carry, op=ALU.add),
            [lgpB, R["carryB"]], [R["carryB"]])

    def phase_fg(l, R, last):
        A.reset(MOE2_MARK)
        M1, M2, WA, WB, POS, carry, DI, RB = (R[k] for k in ("M1", "M2", "WA", "WB", "POS", "carry", "DI", "RB"))
        fz, fzB = A.alloc("fz", 8 * 32, F32)
        pst, pstB = A.alloc("pst", 32, F32)
        be_f, be_fB = A.alloc("be_f", NB, F32)
        bei, beiB = A.alloc("bei", NB, I32)
        blk, blkB = A.alloc("blk", NB, F32)
        big, bigB = A.alloc("big", NB * 32, F32)
        load(blk, blkB, blk_in)

        def dve(fn, reads, writes):
            S.op("dve", fn, reads=reads, writes=writes)
        cB = R["carryB"]
        dve(lambda e: e.tensor_tensor(
            out=big.rearrange("p (e b) -> p e b", e=32),
            in0=carry.unsqueeze(2).broadcast_to([128, 32, NB]),
            in1=blk.unsqueeze(1).broadcast_to([128, 32, NB]), op=ALU.is_gt), [cB, blkB], [bigB])
        dve(lambda e: e.reduce_sum(out=fz[:, 32:64], in_=big.rearrange("p (e b) -> p e b", e=32), axis=AX.X),
            [bigB], [fzB])
        dve(lambda e: e.tensor_scalar(out=fz[:, 64:96], in0=fz[:, 32:64], scalar1=128.0, scalar2=None,
                                      op0=ALU.mult), [fzB], [fzB])
        cur = 64
        for i, sh in enumerate((1, 2, 4, 8, 16)):
            nxt = 96 if cur != 96 else 128
            dve(lambda e, cur=cur, nxt=nxt: e.tensor_copy(out=fz[:, nxt:nxt + 32], in_=fz[:, cur:cur + 32]),
                [fzB], [fzB])
            dve(lambda e, cur=cur, nxt=nxt, sh=sh: e.tensor_tensor(
                out=fz[:, nxt + sh:nxt + 32], in0=fz[:, cur + sh:cur + 32], in1=fz[:, cur:cur + 32 - sh],
                op=ALU.add), [fzB], [fzB])
            cur = nxt
        pend = fz[:, cur:cur + 32]
        dve(lambda e: e.tensor_tensor(out=pst, in0=pend, in1=fz[:, 64:96], op=ALU.subtract), [fzB], [pstB])
        dve(lambda e: e.tensor_tensor(
            out=big.rearrange("p (b e) -> p b e", e=32),
            in0=pend.unsqueeze(1).broadcast_to([128, NB, 32]),
            in1=blk.unsqueeze(2).broadcast_to([128, NB, 32]), op=ALU.is_le), [fzB, blkB], [bigB])
        dve(lambda e: e.reduce_sum(out=be_f, in_=big.rearrange("p (b e) -> p b e", e=32), axis=AX.X),
            [bigB], [be_fB])
        dve(lambda e: e.tensor_scalar(out=be_f, in0=be_f, scalar1=31.0, scalar2=128.0, op0=ALU.min, op1=ALU.mult),
            [be_fB], [be_fB])
        dve(lambda e: e.tensor_scalar(out=be_f, in0=be_f, scalar1=iotap[:, 0:1], scalar2=float(l * 32 * 128),
                                      op0=ALU.add, op1=ALU.add), [be_fB, iotapB], [be_fB])
        sameb, samebB = A.alloc("sameb", NB, F32)
        dve(lambda e: e.memset(sameb[:, 0:1], 0.0), [], [samebB])
        dve(lambda e: e.tensor_tensor(out=sameb[:, 1:NB], in0=be_f[:, 1:NB], in1=be_f[:, 0:NB - 1],
                                      op=ALU.is_equal), [be_fB], [samebB])
        dve(lambda e: e.scalar_tensor_tensor(out=be_f, in0=sameb, scalar=1.0e7, in1=be_f, op0=ALU.mult,
                                             op1=ALU.add), [samebB, be_fB], [be_fB])
        dve(lambda e: e.tensor_copy(out=bei, in_=be_f), [be_fB], [beiB])
        hx = [A.alloc(f"hx{i}", D, BF16) for i in range(2)]
        df, dfB = A.alloc("df", 64, F32)
        for t in range(NT):
            tsl = slice(t * 128, (t + 1) * 128)
            hxi, hxiB = hx[t % 2]
            load(hxi, hxiB, H2[tsl, :], reads=[H2B])
            dve(lambda e, t=t: e.tensor_tensor(out=df[:, 0:32], in0=POS[:, t * 32:(t + 1) * 32], in1=pst, op=ALU.add),
                [RB, pstB], [dfB])
            for j, M in enumerate((M1, M2)):
                dve(lambda e, t=t, M=M: e.tensor_tensor(out=df[:, 32:64], in0=df[:, 0:32],
                                                        in1=M[:, t * 32:(t + 1) * 32], op=ALU.mult),
                    [dfB, RB], [dfB])
                dve(lambda e, t=t, j=j: e.reduce_sum(out=R["DF"][:, 2 * t + j:2 * t + j + 1], in_=df[:, 32:64],
                                                      axis=AX.X), [dfB], [R["DFB"]])
            dve(lambda e, t=t: e.tensor_copy(out=DI[:, 2 * t:2 * t + 2], in_=R["DF"][:, 2 * t:2 * t + 2]),
                [R["DFB"]], [R["DIB"]])
            if stop == "fg1":
                continue
            for j in range(2):
                S.dma("pool", lambda e, t=t, j=j, hxi=hxi: e.indirect_dma_start(
                    out=BUFD, out_offset=bass.IndirectOffsetOnAxis(ap=DI[:, 2 * t + j:2 * t + j + 1], axis=0),
                    in_=hxi, in_offset=None, bounds_check=S.pool_regs["nb"], oob_is_err=False), hxiB,
                    reads=[hxiB, R["DIB"]], writes=[BUFDB])
        if stop == "fg1":
            d1 = nc.dram_tensor("DBG_DI", [128, 2 * NT], I32, kind="ExternalOutput").ap()
            d2 = nc.dram_tensor("DBG_BEI", [128, NB], I32, kind="ExternalOutput").ap()
            d3 = nc.dram_tensor("DBG_F", [128, 64], F32, kind="ExternalOutput").ap()
            d4 = nc.dram_tensor("DBG_RT", [128, NT * 32 * 3 + 2 * NT], F32, kind="ExternalOutput").ap()
            S.dma("sp", lambda e: e.dma_start(out=d1, in_=DI), R["DIB"], reads=[R["DIB"]])
            S.dma("sp", lambda e: e.dma_start(out=d2, in_=bei), beiB, reads=[beiB])
            S.dma("sp", lambda e: e.dma_start(out=d3[:, 0:32], in_=carry), cB, reads=[cB])
            S.dma("sp", lambda e: e.dma_start(out=d3[:, 32:64], in_=pst), pstB, reads=[pstB])
            S.dma("sp", lambda e: e.dma_start(out=d4, in_=R["RT"]), RB, reads=[RB])
            final_bufs.extend([R["DIB"], beiB, cB, pstB, RB])
            return
        if stop == "fg2":
            return
        m_exp = A.mark()
        wf = [A.alloc(f"wf{i}", 4096, F32) for i in range(3)]
        wb_ = [[A.alloc(f"wb{i}_{j}", 4096, BF16) for j in range(2)] for i in range(3)]
        xb = [A.alloc(f"xb{i}", D, BF16) for i in range(2)]
        xbT, xbTB = A.alloc("xbT", 8 * 128, BF16)
        sl, slB = A.alloc("sl", 512, F32)
        hh, hhB = A.alloc("hh", 512, BF16)
        hhT, hhTB = A.alloc("hhT", 4 * 128, BF16)
        yst = [A.alloc(f"yst{i}", D, F32) for i in range(2)]
        casts = ("act", "dve", "pool")
        for b in range(NB):
            bsl = slice(b * 128, (b + 1) * 128)
            wbs = []
            for i, wsrc in enumerate((w1r, w3r, w2r)):
                wfi, wfiB = wf[i]
                wbi, wbiB = wb_[i][b % 2]
                S.dma("pool", lambda e, wfi=wfi, wsrc=wsrc, b=b: e.indirect_dma_start(
                    out=wfi, out_offset=None, in_=wsrc,
                    in_offset=bass.IndirectOffsetOnAxis(ap=bei[:, b:b + 1], axis=0),
                    bounds_check=S.pool_regs["wrows"], oob_is_err=False), wfiB,
                    reads=[beiB], writes=[wfiB])
                if i == 0:
                    S.op("act", lambda e, wbi=wbi, wfi=wfi: e.copy(out=wbi, in_=wfi), reads=[wfiB], writes=[wbiB])
                elif i == 1:
                    S.op("dve", lambda e, wbi=wbi, wfi=wfi: e.tensor_copy(out=wbi, in_=wfi),
                         reads=[wfiB], writes=[wbiB])
                else:
                    S.op("act", lambda e, wbi=wbi, wfi=wfi: e.copy(out=wbi[:, 0:2048], in_=wfi[:, 0:2048]),
                         reads=[wfiB], writes=[wbiB])
                    S.op("dve", lambda e, wbi=wbi, wfi=wfi: e.tensor_copy(out=wbi[:, 2048:4096], in_=wfi[:, 2048:4096]),
                         reads=[wfiB], writes=[wbiB])
                wbs.append((wbi, wbiB))
            xbi, xbiB = xb[b % 2]
            load(xbi, xbiB, BUFD[bsl, :], reads=[BUFDB])
            for k in range(8):
                S.op("pe", lambda e, k=k, xbi=xbi: e.transpose(
                    out=psbf[0][:, k * 128:(k + 1) * 128], in_=xbi[:, k * 128:(k + 1) * 128], identity=identb),
                    reads=[xbiB, identbB], writes=[psb[0]])
            S.op("act", lambda e: e.copy(out=xbT, in_=psbf[0]), reads=[psb[0]], writes=[xbTB])
            xbT3 = xbT.rearrange("p (k t) -> p k t", k=8)
            w13 = wbs[0][0].rearrange("p (k n) -> p k n", k=8)
            w33 = wbs[1][0].rearrange("p (k n) -> p k n", k=8)
            w23 = wbs[2][0].rearrange("p (k n) -> p k n", k=4)
            for (w_, wB_, bk) in ((w13, wbs[0][1], 1), (w33, wbs[1][1], 2)):
                for k in range(8):
                    S.op("pe", lambda e, k=k, w_=w_, bk=bk: e.matmul(
                        ps[bk][:, :], lhsT=xbT3[:, k, :], rhs=w_[:, k, :], start=(k == 0), stop=(k == 7)),
                        reads=[xbTB, wB_], writes=[psb[bk]])
            S.op("act", lambda e: e.activation(out=sl, in_=ps[1][:, :], func=AF.Silu), reads=[psb[1]], writes=[slB])
            S.op("dve", lambda e: e.tensor_tensor(out=hh, in0=ps[2][:, :], in1=sl, op=ALU.mult),
                 reads=[psb[2], slB], writes=[hhB])
            for k in range(4):
                S.op("pe", lambda e, k=k: e.transpose(
                    out=psbf[3][:, k * 128:(k + 1) * 128], in_=hh[:, k * 128:(k + 1) * 128], identity=identb),
                    reads=[hhB, identbB], writes=[psb[3]])
            S.op("act", lambda e: e.copy(out=hhT, in_=psbf[3][:, 0:512]), reads=[psb[3]], writes=[hhTB])
            hhT3 = hhT.rearrange("p (k t) -> p k t", k=4)
            ys, ysB = yst[b % 2]
            for hf in range(2):
                bk = 4 + hf
                for k in range(4):
                    S.op("pe", lambda e, k=k, bk=bk, hf=hf, w23=w23: e.matmul(
                        ps[bk][:, :], lhsT=hhT3[:, k, :], rhs=w23[:, k, hf * 512:(hf + 1) * 512],
                        start=(k == 0), stop=(k == 3)), reads=[hhTB, wbs[2][1]], writes=[psb[bk]])
                S.op("dve", lambda e, bk=bk, hf=hf, ys=ys: e.tensor_copy(out=ys[:, hf * 512:(hf + 1) * 512],
                                                                          in_=ps[bk][:, :]),
                     reads=[psb[bk]], writes=[ysB])
            store(YB[bsl, :], ys, ysB, YBB)
        if stop == "fg3":
            return
        A.reset(m_exp)
        g2b, g2bB = A.alloc("g2b", D, F32)
        y1 = [A.alloc(f"y1{i}", D, F32) for i in range(2)]
        y2 = [A.alloc(f"y2{i}", D, F32) for i in range(2)]
        xg = [A.alloc(f"xg{i}", D, F32) for i in range(2)]
        o1, o1B = A.alloc("o1", D, F32)
        xo = [A.alloc(f"xo{i}", D, F32) for i in range(2)]
        if last:
            gfin, gfinB = A.alloc("gfin", D, F32)
            bload(gfin, gfinB, g_fin[0:1, :])
            junk, junkB = A.alloc("junkg", D, F32)
            ss, ssB = A.alloc("ssg", 1, F32)
            rstd, rstdB = A.alloc("rstdg", 1, F32)
        for t in range(NT):
            tsl = slice(t * 128, (t + 1) * 128)
            if t % SLOT_T == 0:
                s = t // SLOT_T
                bload(g2b, g2bB, mod_row(l, s, 5), reads=[MODSB])
            ya, yaB = y1[t % 2]
            yb_, ybB = y2[t % 2]
            xi, xiB = xg[t % 2]
            for j, (yy, yyB) in enumerate(((ya, yaB), (yb_, ybB))):
                S.dma("pool", lambda e, yy=yy, t=t, j=j: e.indirect_dma_start(
                    out=yy, out_offset=None, in_=YB,
                    in_offset=bass.IndirectOffsetOnAxis(ap=DI[:, 2 * t + j:2 * t + j + 1], axis=0),
                    bounds_check=S.pool_regs["nb"], oob_is_err=False), yyB,
                    reads=[YBB, R["DIB"]], writes=[yyB])
            load(xi, xiB, X[tsl, :], reads=[XB[t]])
            dve(lambda e, ya=ya, t=t: e.tensor_scalar(out=o1, in0=ya, scalar1=WA[:, t:t + 1], scalar2=None,
                                                      op0=ALU.mult), [yaB, RB], [o1B])
            dve(lambda e, yb_=yb_, t=t: e.scalar_tensor_tensor(out=o1, in0=yb_, scalar=WB[:, t:t + 1], in1=o1,
                                                               op0=ALU.mult, op1=ALU.add), [ybB, RB, o1B], [o1B])
            S.op("pool", lambda e: e.tensor_tensor(out=o1, in0=o1, in1=g2b, op=ALU.mult),
                 reads=[o1B, g2bB], writes=[o1B])
            xoi, xoiB = xo[t % 2]
            dve(lambda e, xi=xi, xoi=xoi: e.tensor_tensor(out=xoi, in0=o1, in1=xi, op=ALU.add),
                [o1B, xiB], [xoiB])
            if not last:
                store(X[tsl, :], xoi, xoiB, XB[t])
            else:
                S.op("act", lambda e, xoi=xoi: e.activation(out=junk, in_=xoi, func=AF.Square, accum_out=ss),
                     reads=[xoiB], writes=[junkB, ssB])
                rms_rstd(ss, ssB, D, rstd, rstdB)
                dve(lambda e, xoi=xoi: e.scalar_tensor_tensor(
                    out=xoi, in0=xoi, scalar=rstd[:, 0:1], in1=gfin, op0=ALU.mult, op1=ALU.mult),
                    [xoiB, rstdB, gfinB], [xoiB])
                S.dma("sp", lambda e, xoi=xoi, tsl=tsl: e.dma_start(out=y_out[tsl, :], in_=xoi), xoiB,
                      reads=[xoiB])
                if xoiB not in final_bufs:
                    final_bufs.append(xoiB)

    ATT_MARK = PERSIST
    MOE_MARK = PERSIST
    MOE2_MARK = PERSIST
    for l in range(depth):
        phase_mod(l)
        if stop == "mod":
            break
        phase_a(l)
        if stop == "a":
            break
        phase_c1(l)
        if stop == "c1":
            break
        A.reset(PERSIST)
        QT = [A.alloc(f"QT{i}", T, BF16) for i in range(2)]
        KT = [A.alloc(f"KT{i}", T, BF16) for i in range(2)]
        for i in range(2):
            load(QT[i][0][96:100, :], QT[i][1], qseg_in)
            load(KT[i][0][96:100, :], KT[i][1], kseg_in)
        ATT_MARK = A.mark()
        phase_c2(l, QT, KT, True)
        if stop == "c2":
            break
        for i in range(2):
            load(QT[i][0][64:96, :], QT[i][1], qrow_in)
            load(KT[i][0][64:96, :], KT[i][1], krow_in)
        phase_b(l, QT, KT)
        if stop == "b":
            break
        A.reset(PERSIST)
        R = {}
        RT, R["RB"] = A.alloc("RT", NT * 32 * 3 + 2 * NT, F32)
        R["RT"] = RT
        R["M1"] = RT[:, 0:NT * 32]
        R["M2"] = RT[:, NT * 32:2 * NT * 32]
        R["POS"] = RT[:, 2 * NT * 32:3 * NT * 32]
        R["WA"] = RT[:, 3 * NT * 32:3 * NT * 32 + NT]
        R["WB"] = RT[:, 3 * NT * 32 + NT:3 * NT * 32 + 2 * NT]
        R["carry"], R["carryB"] = A.alloc("carry", 32, F32)
        R["DF"], R["DFB"] = A.alloc("DF", 2 * NT, F32)
        R["DI"], R["DIB"] = A.alloc("DI", 2 * NT, I32)
        MOE_MARK = A.mark()
        phase_de(l, R)
        if stop == "de":
            break
        MOE2_MARK = MOE_MARK
        phase_fg(l, R, l == depth - 1)
        if stop in ("fg1", "fg2", "fg3"):
            break

    S.emit(final_bufs=final_bufs)
    es.close()
    return nc, S


def _bf(a):
    return np.ascontiguousarray(a.astype(NPBF))


def core_consts(T, L):
    NT = T // 128
    tok = np.arange(T)
    seg = tok // L
    kseg = np.zeros((4, T), np.float32)
    kseg[seg, tok] = 1.0
    qseg = np.where(kseg > 0, 0.0, NEG).astype(np.float32)
    r = tok // 64
    rps = L // 64
    seg_lo = (r // rps) * rps
    rs = np.clip(r - 4, seg_lo, seg_lo + rps - 8)
    krow = np.zeros((32, T), np.float32)
    krow[r % 32, tok] = 1.0
    p = r // 2
    base = 2 * p - 8
    j = np.arange(32)[:, None]
    a_j = base[None, :] + ((j - base[None, :]) % 32)
    valid = (a_j >= rs[None, :]) & (a_j < rs[None, :] + 8) & (a_j < (2 * p + 10)[None, :])
    qrow = np.where(valid, 0.0, NEG).astype(np.float32)
    pos = (tok % L).astype(np.float32)
    inv = (1.0 / (np.float32(10000.0) ** (np.arange(0, 32, 2, dtype=np.float32) / np.float32(32)))).astype(np.float32)
    ang = (pos[:, None] * inv[None, :]).astype(np.float32)
    cos = np.cos(ang).astype(np.float32)
    sin = np.sin(ang).astype(np.float32)
    cosk = np.ascontiguousarray(cos.reshape(NT, 128, 16).transpose(1, 0, 2).reshape(128, NT * 16))
    sink = np.ascontiguousarray(sin.reshape(NT, 128, 16).transpose(1, 0, 2).reshape(128, NT * 16))
    cosq = np.ascontiguousarray(np.concatenate([cos.T, cos.T], 0))
    sinq = np.ascontiguousarray(np.concatenate([-sin.T, sin.T], 0))
    return dict(kseg=_bf(kseg), qseg=_bf(qseg), krow=_bf(krow), qrow=_bf(qrow),
                cosk=cosk, sink=sink, cosq=cosq, sinq=sinq)


def na_tables(rpb):
    depth = rpb.shape[0]
    key = np.arange(128)
    ra, ck = key // 64, key % 64
    qry = np.arange(128)
    rq, cq = qry // 64, qry % 64
    d = 2 * (np.arange(9) - 4)
    dr = d[None, :, None] + ra[:, None, None] - rq[None, None, :]
    dc = ck[:, None, None] - cq[None, None, :] + 0 * d[None, :, None]
    cs = np.clip(cq - 8, 0, 48)
    valid = (np.abs(dr) <= 7) & (ck[:, None, None] >= cs[None, None, :]) & (ck[:, None, None] < cs[None, None, :] + 16)
    dri = np.clip(dr + 7, 0, 14)
    dci = np.clip(dc + 15, 0, 30)
    g = rpb[:, :, dri, dci]
    out = np.where(valid[None, None], g, np.float32(NEG)).astype(np.float32)
    return np.ascontiguousarray(out.reshape(depth, 8, 128, 9 * 128))


def shared_inputs(T, depth, ada_w, ada_b, norm_mix_g, w_in, mla_q_norm_g, w_uq, mla_kv_norm_g, w_ukv, na_rpb,
                  w_na_o, w_mla_o, w_out, norm_ffn_g, router_wg, router_bg, router_we, router_be, expert_w1,
                  expert_w3, expert_w2, final_norm_g):
    f = lambda a: np.ascontiguousarray(np.asarray(a, dtype=np.float32))
    NB = (2 * T + 32 * 127 + 127) // 128
    w_uq = f(w_uq)[:depth]
    perm = np.arange(768)
    for h in range(8):
        b = h * 96 + 64
        perm[b:b + 16] = np.arange(b + 16, b + 32)
        perm[b + 16:b + 32] = np.arange(b, b + 16)
    w_ukv = f(w_ukv)[:depth].reshape(depth, 128, 8, 128)
    d = dict(
        ada_w=f(ada_w)[:depth], ada_b=f(ada_b)[:depth], norm_mix_g=f(norm_mix_g)[:depth],
        norm_ffn_g=f(norm_ffn_g)[:depth], final_norm_g=f(final_norm_g).reshape(1, D), w_in=f(w_in)[:depth],
        mla_q_norm_g=f(mla_q_norm_g)[:depth], mla_kv_norm_g=f(mla_kv_norm_g)[:depth],
        w_uq=w_uq, w_uq_rot=np.ascontiguousarray(w_uq[:, :, perm]),
        w_ukv_k=np.ascontiguousarray(w_ukv[:, :, :, 0:64].reshape(depth, 128, 512)),
        w_ukv_v=np.ascontiguousarray(w_ukv[:, :, :, 64:128].reshape(depth, 128, 512)),
        na_tt=na_tables(f(na_rpb)[:depth]),
        w_na_o=f(w_na_o)[:depth], w_mla_o=f(w_mla_o)[:depth], w_out=f(w_out)[:depth],
        w_router=np.ascontiguousarray(np.concatenate([f(router_wg)[:depth], f(router_we)[:depth]], -1)),
        b_router=np.ascontiguousarray(np.concatenate([f(router_bg)[:depth], f(router_be)[:depth]], -1)),
        w1r=np.ascontiguousarray(f(expert_w1)[:depth].reshape(depth, 32, 8, 128, 512).transpose(0, 1, 3, 2, 4)
                                 .reshape(depth * 32 * 128, 4096)),
        w3r=np.ascontiguousarray(f(expert_w3)[:depth].reshape(depth, 32, 8, 128, 512).transpose(0, 1, 3, 2, 4)
                                 .reshape(depth * 32 * 128, 4096)),
        w2r=np.ascontiguousarray(f(expert_w2)[:depth].reshape(depth, 32, 4, 128, 1024).transpose(0, 1, 3, 2, 4)
                                 .reshape(depth * 32 * 128, 4096)),
        identb=_bf(np.eye(128, dtype=np.float32)), identf=np.eye(128, dtype=np.float32),
        tril=_bf(np.triu(np.ones((128, 128), np.float32), 1)), onesb=_bf(np.ones((128, 128), np.float32)),
        iotap=np.arange(128, dtype=np.float32).reshape(128, 1),
        blkstart=np.ascontiguousarray(np.broadcast_to((np.arange(NB, dtype=np.float32) * 128)[None, :], (128, NB))),
    )
    return d


def core_inputs(T, L, x, cs):
    c4 = np.stack([np.asarray(c, np.float32) for c in cs], 0)
    cT = np.ascontiguousarray(c4.reshape(4, 8, 128).transpose(2, 1, 0).reshape(128, 32))
    d = dict(x=np.ascontiguousarray(np.asarray(x, np.float32)), cT=cT)
    d.update(core_consts(T, L))
    return d


_CACHE = {}


def kernel(x_prompt, x_sample, c_prompt, c_sample, **w):
    T, depth = 8192, 4
    x_prompt = np.asarray(x_prompt, np.float32)
    x_sample = np.asarray(x_sample, np.float32)
    c_prompt = np.asarray(c_prompt, np.float32)
    c_sample = np.asarray(c_sample, np.float32)
    if "nc" not in _CACHE:
        _CACHE["nc"] = build(T, depth)[0]
    nc = _CACHE["nc"]
    sh = shared_inputs(T, depth, **w)
    in_maps = []
    for i in range(4):
        m = dict(sh)
        m.update(core_inputs(T, 8192, x_prompt[i], [c_prompt[i]] * 4))
        in_maps.append(m)
    for i in range(4):
        j = i % 2
        m = dict(sh)
        m.update(core_inputs(T, 2048, x_sample[4 * j:4 * j + 4].reshape(T, D), [c_sample[4 * j + s] for s in range(4)]))
        in_maps.append(m)
    res = run_bass_kernel_spmd(nc, in_maps, core_ids=list(range(8)))
    ys = [r["y"] for r in res.results]
    y_prompt = np.stack(ys[0:4], 0).astype(np.float32)
    y_sample = np.concatenate([ys[4].reshape(4, 2048, D), ys[5].reshape(4, 2048, D)], 0).astype(np.float32)
    return (y_prompt, y_sample)
```

```python
import contextlib
import numpy as np
import ml_dtypes
import concourse.bass as bass
import concourse.mybir as mybir
from concourse.bass_utils import run_bass_kernel_spmd

F32 = mybir.dt.float32
BF16 = mybir.dt.bfloat16
I32 = mybir.dt.int32
U8 = mybir.dt.uint8
AF = mybir.ActivationFunctionType
ALU = mybir.AluOpType
AX = mybir.AxisListType
NPBF = ml_dtypes.bfloat16

D = 1024
EPS = 1e-6
NEG = -30000.0
EPOCH = 30000


class Buf:
    __slots__ = ("name", "lw", "rd", "cnt", "last_dma", "sem", "multi")

    def __init__(self, name, multi=False):
        self.name = name
        self.lw = {}
        self.rd = {}
        self.cnt = 0
        self.last_dma = None
        self.sem = None
        self.multi = multi


class SemState:
    __slots__ = ("cnt", "last_dma", "sem")

    def __init__(self):
        self.cnt = 0
        self.last_dma = None
        self.sem = None


SEMREG = {}
SEM_ALIAS = {n: "misc" for n in (
    "gq", "gkv", "gm", "cosk", "sink", "G1", "SH1", "brt", "gf", "g1b", "G2", "SH2", "g2b", "gfin", "blk",
    "adab", "mods", "identb", "identf", "tril", "onesb", "iotap", "scT", "ttf", "cq", "ckv")}


class Op:
    __slots__ = ("idx", "eng", "fn", "deps", "isdma", "sbuf", "n", "inc", "val", "dval")


class Sched:
    def __init__(self, nc):
        self.nc = nc
        self.ops = []
        self.reg_requests = {}
        self.pool_regs = {}
        self.semreg = {}
        self.engh = {"pe": nc.tensor, "act": nc.scalar, "dve": nc.vector,
                     "pool": nc.gpsimd, "sp": nc.sync}

    def _key(self, o):
        return o.eng if not o.isdma else ("dma", id(o.sbuf))

    def _deps(self, o, reads, writes):
        deps = {}
        for b in reads:
            for w in b.lw.values():
                deps[w.idx] = w
        for b in writes:
            if not (b.multi and not b.rd):
                for w in b.lw.values():
                    deps[w.idx] = w
            for r in b.rd.values():
                deps[r.idx] = r
        o.deps = list(deps.values())
        k = self._key(o)
        for b in writes:
            if b.multi and not b.rd:
                b.lw[k] = o
            else:
                b.lw = {k: o}
            b.rd = {}
        for b in reads:
            if o in b.lw.values():
                continue
            b.rd[k] = o

    def op(self, eng, fn, reads=(), writes=()):
        o = Op()
        o.idx = len(self.ops)
        o.eng = eng
        o.fn = fn
        o.isdma = False
        o.inc = False
        o.val = None
        o.sbuf = None
        self._deps(o, reads, writes)
        self.ops.append(o)
        return o

    def dma(self, eng, fns, key, reads=(), writes=()):
        if not isinstance(fns, (list, tuple)):
            fns = [fns]
        o = Op()
        o.idx = len(self.ops)
        o.eng = eng
        o.fn = list(fns)
        o.isdma = True
        o.inc = False
        o.val = None
        ss = self.semreg.setdefault(SEM_ALIAS.get(key.name, key.name), SemState())
        o.sbuf = ss
        self._deps(o, reads, writes)
        if ss.last_dma is not None and ss.last_dma not in o.deps:
            o.deps.append(ss.last_dma)
        ss.last_dma = o
        ss.cnt += len(fns)
        o.dval = ss.cnt * 16
        self.ops.append(o)
        return o

    def emit(self, final_bufs=()):
        nc = self.nc
        ops = self.ops
        for o in ops:
            for d in o.deps:
                if d.isdma:
                    continue
                if d.eng == o.eng and o.eng == "pe" and not o.isdma:
                    continue
                d.inc = True
        cnt = {e: 0 for e in self.engh}
        for o in ops:
            if not o.isdma and o.inc:
                cnt[o.eng] += 1
                o.val = cnt[o.eng]
        es = contextlib.ExitStack()
        engsems = {}
        for e in self.engh:
            n = max(1, (cnt[e] + EPOCH - 1) // EPOCH)
            engsems[e] = [es.enter_context(nc.semaphore(f"s_{e}_{i}")) for i in range(n)]
        dmabufs = {}
        for o in ops:
            if o.isdma and id(o.sbuf) not in dmabufs:
                dmabufs[id(o.sbuf)] = o.sbuf
        for i, b in enumerate(dmabufs.values()):
            b.sem = es.enter_context(nc.semaphore(f"d_{i}"))
        self.n_sems = sum(len(v) for v in engsems.values()) + len(dmabufs)
        per = {e: [] for e in self.engh}
        for o in ops:
            per[o.eng].append(o)

        def run_engine(ename, eh):
            if ename == "pool":
                for nm, val in getattr(self, "reg_requests", {}).items():
                    r = eh.alloc_register(nm)
                    eh.reg_mov(r, val)
                    self.pool_regs[nm] = r
            known_eng = {e: 0 for e in self.engh}
            known_dma = {}
            for o in per[ename]:
                need_eng = {}
                need_dma = {}
                for d in o.deps:
                    if d.isdma:
                        k = id(d.sbuf)
                        if d.dval > known_dma.get(k, 0) and d.dval > need_dma.get(k, (0, None))[0]:
                            need_dma[k] = (d.dval, d.sbuf)
                    else:
                        if d.val is None:
                            continue
                        if d.val > known_eng[d.eng] and d.val > need_eng.get(d.eng, 0):
                            need_eng[d.eng] = d.val
                for e, v in need_eng.items():
                    eh.wait_ge(engsems[e][(v - 1) // EPOCH], (v - 1) % EPOCH + 1)
                    known_eng[e] = v
                for k, (v, b) in need_dma.items():
                    eh.wait_ge(b.sem, v)
                    known_dma[k] = v
                if o.isdma:
                    for f in o.fn:
                        f(eh).then_inc(o.sbuf.sem, 16)
                else:
                    ins = o.fn(eh)
                    if o.inc:
                        ins.then_inc(engsems[ename][(o.val - 1) // EPOCH], 1)
            if ename == "sp":
                for b in final_bufs:
                    ss = self.semreg.get(SEM_ALIAS.get(b.name, b.name))
                    if ss is not None and ss.sem is not None and ss.cnt > 0:
                        eh.wait_ge(ss.sem, ss.cnt * 16)

        with nc.Block() as block:
            @block.tensor
            def _(e):
                run_engine("pe", e)

            @block.scalar
            def _(e):
                run_engine("act", e)

            @block.vector
            def _(e):
                run_engine("dve", e)

            @block.gpsimd
            def _(e):
                run_engine("pool", e)

            @block.sync
            def _(e):
                run_engine("sp", e)
        es.close()


class Arena:
    def __init__(self, t, nbytes):
        self.t = t
        self.n = nbytes
        self.off = 0
        self.regions = []

    def mark(self):
        return self.off

    def reset(self, m=0):
        self.off = m

    def alloc(self, name, cols, dtype, parts=128):
        esz = {F32: 4, BF16: 2, I32: 4, U8: 1}[dtype]
        nb = cols * esz
        start = (self.off + 63) // 64 * 64
        end = start + nb
        assert end <= self.n, f"arena overflow {name}: {end} > {self.n}"
        self.off = end
        b = Buf(name)
        keep = []
        for (s, e, ob) in self.regions:
            if s < end and start < e:
                for w in ob.lw.values():
                    b.rd[("old", w.idx)] = w
                for r in ob.rd.values():
                    b.rd[("old", r.idx)] = r
                if s < start or e > end:
                    keep.append((s, e, ob))
            else:
                keep.append((s, e, ob))
        keep.append((start, end, b))
        self.regions = keep
        ap = self.t[0:parts, start:end].bitcast(dtype)
        return ap, b


IN_Q, IN_K, IN_V, IN_CQ, IN_CKV, IN_KR, IN_G = 0, 512, 1024, 1536, 1792, 1920, 1952


def build(T, depth, dbg=None, stop=None):
    NT = T // 128
    NG = T // 512
    NB = (2 * T + 32 * 127 + 127) // 128
    SLOT_T = NT // 4
    nc = bass.Bass("TRN2", target_bir_lowering=False)

    def din(name, shape, dt=F32):
        return nc.dram_tensor(name, list(shape), dt, kind="ExternalInput").ap()

    def dscr(name, shape, dt):
        kind = "ExternalOutput" if (dbg and name in dbg) else "Internal"
        return nc.dram_tensor(name, list(shape), dt, kind=kind).ap()

    x_in = din("x", [T, D])
    cT_in = din("cT", [128, 32])
    ada_w = din("ada_w", [depth, D, 6 * D])
    ada_b = din("ada_b", [depth, 6 * D])
    g_mix = din("norm_mix_g", [depth, D])
    g_ffn = din("norm_ffn_g", [depth, D])
    g_fin = din("final_norm_g", [1, D])
    w_in = din("w_in", [depth, D, 4000])
    g_q = din("mla_q_norm_g", [depth, 256])
    g_kv = din("mla_kv_norm_g", [depth, 128])
    w_uq = din("w_uq", [depth, 256, 768])
    w_uqr = din("w_uq_rot", [depth, 256, 768])
    w_ukvk = din("w_ukv_k", [depth, 128, 512])
    w_ukvv = din("w_ukv_v", [depth, 128, 512])
    tt_in = din("na_tt", [depth, 8, 128, 9 * 128])
    w_nao = din("w_na_o", [depth, 512, D])
    w_mlao = din("w_mla_o", [depth, 512, D])
    w_out = din("w_out", [depth, D, D])
    w_r = din("w_router", [depth, D, 36])
    b_r = din("b_router", [depth, 36])
    w1r = din("w1r", [depth * 32 * 128, 4096])
    w3r = din("w3r", [depth * 32 * 128, 4096])
    w2r = din("w2r", [depth * 32 * 128, 4096])
    identb_in = din("identb", [128, 128], BF16)
    identf_in = din("identf", [128, 128])
    tril_in = din("tril", [128, 128], BF16)
    ones_in = din("onesb", [128, 128], BF16)
    kseg_in = din("kseg", [4, T], BF16)
    qseg_in = din("qseg", [4, T], BF16)
    krow_in = din("krow", [32, T], BF16)
    qrow_in = din("qrow", [32, T], BF16)
    cosk_in = din("cosk", [128, NT * 16])
    sink_in = din("sink", [128, NT * 16])
    cosq_in = din("cosq", [32, T])
    sinq_in = din("sinq", [32, T])
    iotap_in = din("iotap", [128, 1])
    blk_in = din("blkstart", [128, NB])

    y_out = nc.dram_tensor("y", [T, D], F32, kind="ExternalOutput").ap()
    dbg_out = {}

    X = dscr("X", [T, D], F32)
    MODS = dscr("MODS", [depth, 4, 6 * D], F32)
    QA = dscr("QA", [8, 64, T], BF16)
    KA = dscr("KA", [8, 64, T], BF16)
    VNA = dscr("VNA", [T, 512], BF16)
    CQT = dscr("CQT", [256, T], BF16)
    CKVT = dscr("CKVT", [128, T], BF16)
    KRT = dscr("KRT", [32, T], BF16)
    GATE = dscr("GATE", [T, 2048], BF16)
    QM = dscr("QM", [8, 96, T], BF16)
    KM = dscr("KM", [8, 64, T], BF16)
    VM = dscr("VM", [8, T, 64], BF16)
    NAO = dscr("NAO", [T, 512], BF16)
    MLAO = dscr("MLAO", [T, 512], BF16)
    H2 = dscr("H2", [T, D], BF16)
    BUFD = dscr("BUFD", [NB * 128, D], BF16)
    YB = dscr("YB", [NB * 128, D], F32)

    def MB(n):
        return Buf(n, multi=True)
    XB = [MB(f"X{t}") for t in range(NT)]
    MODSB = MB("MODS")
    QAB, KAB, VNAB, CQTB, CKVTB, KRTB, GATEB = (MB(n) for n in ("QA", "KA", "VNA", "CQT", "CKVT", "KRT", "GATE"))
    QMB, KMB, VMB, NAOB, MLAOB, H2B, BUFDB, YBB = (MB(n) for n in ("QM", "KM", "VM", "NAO", "MLAO", "H2", "BUFD", "YB"))

    es = contextlib.ExitStack()
    ARN = 204 * 1024
    sb = es.enter_context(nc.sbuf_tensor("arena", [128, ARN], U8))
    psh = [es.enter_context(nc.psum_tensor(f"ps{i}", [128, 1024], F32)) for i in range(4)]
    ps2 = [h[:, :] for h in psh]
    ps = [ps2[i // 2][:, (i % 2) * 512:(i % 2 + 1) * 512] for i in range(8)]
    psbf = [p.bitcast(BF16) for p in ps]
    psb = [Buf(f"ps{i}") for i in range(8)]
    A = Arena(sb, ARN)
    S = Sched(nc)
    S.reg_requests["nb"] = NB * 128 - 1
    S.reg_requests["wrows"] = depth * 32 * 128 - 1
    final_bufs = []

    def load(dst, dstB, src, reads=(), extra_writes=()):
        return S.dma("sp", lambda e: e.dma_start(out=dst, in_=src), dstB, reads=list(reads),
                     writes=[dstB] + list(extra_writes))

    def store(dst, src, srcB, dstB):
        return S.dma("sp", lambda e: e.dma_start(out=dst, in_=src), srcB, reads=[srcB], writes=[dstB])

    identb, identbB = A.alloc("identb", 128, BF16)
    identf, identfB = A.alloc("identf", 128, F32)
    tril, trilB = A.alloc("tril", 128, BF16)
    onesb, onesbB = A.alloc("onesb", 128, BF16)
    scT, scTB = A.alloc("scT", 32, F32)
    iotap, iotapB = A.alloc("iotap", 1, F32)
    load(identb, identbB, identb_in)
    load(identf, identfB, identf_in)
    load(tril, trilB, tril_in)
    load(onesb, onesbB, ones_in)
    load(iotap, iotapB, iotap_in)
    load(scT, scTB, cT_in)
    S.op("act", lambda e: e.activation(out=scT, in_=scT, func=AF.Silu), reads=[scTB], writes=[scTB])
    scT3 = scT.rearrange("p (k s) -> p k s", k=8)
    PERSIST = A.mark()

    def rms_rstd(ssum, ssB, n, rstd, rstdB, width=1):
        S.op("dve", lambda e: e.tensor_scalar(out=rstd, in0=ssum, scalar1=1.0 / n, scalar2=EPS,
                                              op0=ALU.mult, op1=ALU.add), reads=[ssB], writes=[rstdB])
        S.op("act", lambda e: e.activation(out=rstd, in_=rstd, func=AF.Sqrt), reads=[rstdB], writes=[rstdB])
        S.op("dve", lambda e: e.reciprocal(out=rstd, in_=rstd), reads=[rstdB], writes=[rstdB])

    def bload(dst, dstB, row_ap, parts=128, reads=()):
        n = row_ap.shape[-1]
        return S.dma("sp", lambda e: e.dma_start(out=dst, in_=row_ap.broadcast_to([parts, n])), dstB,
                     reads=list(reads), writes=[dstB])

    def phase_mod(l):
        A.reset(PERSIST)
        mods, modsB = A.alloc("mods", 6 * D, F32, parts=4)
        adab, adabB = A.alloc("adab", 6 * D, F32, parts=4)
        aw = [A.alloc(f"aw{i}", 8 * 512, F32) for i in range(2)]
        bload(adab, adabB, ada_b[l:l + 1, :], parts=4)
        awsrc = ada_w[l].rearrange("(k p) n -> p k n", p=128)
        for blk in range(12):
            awt, awtB = aw[blk % 2]
            awt3 = awt.rearrange("p (k n) -> p k n", k=8)
            load(awt3, awtB, awsrc[:, :, blk * 512:(blk + 1) * 512])
            bk = 6 + blk % 2
            for k in range(8):
                S.op("pe", lambda e, k=k, awt3=awt3, bk=bk: e.matmul(
                    ps[bk][0:4, :], lhsT=scT3[:, k, :], rhs=awt3[:, k, :], start=(k == 0), stop=(k == 7)),
                    reads=[scTB, awtB], writes=[psb[bk]])
            S.op("dve", lambda e, blk=blk, bk=bk: e.tensor_tensor(
                out=mods[:, blk * 512:(blk + 1) * 512], in0=ps[bk][0:4, :],
                in1=adab[:, blk * 512:(blk + 1) * 512], op=ALU.add),
                reads=[psb[bk], adabB], writes=[modsB])
        store(MODS[l], mods, modsB, MODSB)

    def mod_row(l, s, j):
        return MODS[l, s:s + 1, j * D:(j + 1) * D]

    def phase_a(l):
        A.reset(PERSIST)
        win, winB = A.alloc("win", 8 * 4000, BF16)
        win3 = win.rearrange("p (k n) -> p k n", k=8)
        wst = [A.alloc(f"wst{i}", 8 * 500, F32) for i in range(2)]
        wsrc = w_in[l].rearrange("(k p) n -> p k n", p=128)
        for blk in range(8):
            st, stB = wst[blk % 2]
            st3 = st.rearrange("p (k n) -> p k n", k=8)
            load(st3, stB, wsrc[:, :, blk * 500:(blk + 1) * 500])
            S.op("pool", lambda e, st3=st3, blk=blk: e.tensor_copy(
                out=win3[:, :, blk * 500:(blk + 1) * 500], in_=st3), reads=[stB], writes=[winB])
        gq, gqB = A.alloc("gq", 256, F32)
        gkv, gkvB = A.alloc("gkv", 128, F32)
        bload(gq, gqB, g_q[l:l + 1, :])
        bload(gkv, gkvB, g_kv[l:l + 1, :])
        gm, gmB = A.alloc("gm", D, F32)
        bload(gm, gmB, g_mix[l:l + 1, :])
        cosk, coskB = A.alloc("cosk", NT * 16, F32)
        sink, sinkB = A.alloc("sink", NT * 16, F32)
        load(cosk, coskB, cosk_in)
        load(sink, sinkB, sink_in)
        G1, G1B = A.alloc("G1", D, F32)
        SH1, SH1B = A.alloc("SH1", D, F32)
        xt = [A.alloc(f"xt{i}", D, F32) for i in range(2)]
        junk, junkB = A.alloc("junk", D, F32)
        tmp, tmpB = A.alloc("tmp", D, F32)
        hb = [A.alloc(f"hb{i}", D, BF16) for i in range(2)]
        hT = [A.alloc(f"hT{i}", 8 * 512, BF16) for i in range(2)]
        ss, ssB = A.alloc("ss", 4, F32)
        rstd, rstdB = A.alloc("rstd", 4, F32)
        qst = [A.alloc(f"qst{i}", 512, BF16) for i in range(2)]
        vst = [A.alloc(f"vst{i}", 512, BF16) for i in range(2)]
        gst = [A.alloc(f"gst{i}", 2048, BF16) for i in range(2)]
        latb, latbB = A.alloc("latb", 416, BF16)
        rt, rtB = A.alloc("rt", 64, F32)
        latT = [A.alloc(f"latT{i}", 4 * 512, BF16) for i in range(2)]
        tokbank = [3, 4, 5]
        tokctr = [0]
        fmctr = [0]

        def nexttok():
            b = tokbank[tokctr[0] % 3]
            tokctr[0] += 1
            return b

        ss2, ss2B = A.alloc("ss2", 4, F32)
        rstd2, rstd2B = A.alloc("rstd2", 4, F32)
        junk2, junk2B = A.alloc("junk2", 384, F32)

        hTs = [[Buf(f"hTs{i}_{j}") for j in range(4)] for i in range(2)]

        def prep_a(t):
            if t % SLOT_T == 0:
                s_ = t // SLOT_T
                bload(G1, G1B, mod_row(l, s_, 1), reads=[MODSB])
                bload(SH1, SH1B, mod_row(l, s_, 0), reads=[MODSB])
                S.op("dve", lambda e: e.scalar_tensor_tensor(
                    out=G1, in0=G1, scalar=1.0, in1=gm, op0=ALU.add, op1=ALU.mult),
                    reads=[G1B, gmB], writes=[G1B])
            xti, xtiB = xt[t % 2]
            src = x_in if l == 0 else X
            load(xti, xtiB, src[t * 128:(t + 1) * 128, :], reads=([] if l == 0 else [XB[t]]))
            S.op("act", lambda e: e.activation(out=junk, in_=xti, func=AF.Square, accum_out=ss[:, 0:1]),
                 reads=[xtiB], writes=[junkB, ssB])
            rms_rstd(ss[:, 0:1], ssB, D, rstd[:, 0:1], rstdB)
            S.op("dve", lambda e: e.scalar_tensor_tensor(
                out=tmp, in0=xti, scalar=rstd[:, 0:1], in1=G1, op0=ALU.mult, op1=ALU.mult),
                reads=[xtiB, rstdB, G1B], writes=[tmpB])
            hbi, hbiB = hb[t % 2]
            S.op("pool", lambda e: e.tensor_tensor(out=hbi, in0=tmp, in1=SH1, op=ALU.add),
                 reads=[tmpB, SH1B], writes=[hbiB])

        def tr_a(t):
            g, j = t // 4, t % 4
            hTg, hTgB = hT[g % 2]
            hT3 = hTg.rearrange("p (k t) -> p k t", k=8)
            hbi, hbiB = hb[t % 2]
            for k in range(8):
                S.op("pe", lambda e, k=k: e.transpose(
                    out=psbf[0][:, k * 128:(k + 1) * 128], in_=hbi[:, k * 128:(k + 1) * 128],
                    identity=identb), reads=[hbiB, identbB], writes=[psb[0]])
            S.op("act", lambda e: e.copy(
                out=hT3[:, :, j * 128:(j + 1) * 128],
                in_=psbf[0].rearrange("p (k t) -> p k t", k=8)), reads=[psb[0]], writes=[hTs[g % 2][j]])

        def mm_a(t):
            g, j = t // 4, t % 4
            hTg, hTgB = hT[g % 2]
            hT3 = hTg.rearrange("p (k t) -> p k t", k=8)
            lT, lTB = latT[g % 2]
            lT3 = lT.rearrange("p (c t) -> p c t", c=4)
            bk = nexttok()
            for k in range(8):
                S.op("pe", lambda e, k=k, bk=bk: e.matmul(
                    ps[bk], lhsT=hT3[:, k, j * 128:(j + 1) * 128], rhs=win3[:, k, IN_V:IN_V + 512],
                    start=(k == 0), stop=(k == 7)), reads=[hTs[g % 2][j], hTgB, winB], writes=[psb[bk]])
            vs, vsB = vst[t % 2]
            S.op("dve", lambda e, bk=bk: e.tensor_copy(out=vs, in_=ps[bk]), reads=[psb[bk]], writes=[vsB])
            store(VNA[t * 128:(t + 1) * 128, :], vs, vsB, VNAB)
            gs, gsB = gst[t % 2]
            for q4 in range(4):
                bk = nexttok()
                for k in range(8):
                    S.op("pe", lambda e, k=k, bk=bk, q4=q4: e.matmul(
                        ps[bk], lhsT=hT3[:, k, j * 128:(j + 1) * 128],
                        rhs=win3[:, k, IN_G + q4 * 512:IN_G + (q4 + 1) * 512],
                        start=(k == 0), stop=(k == 7)), reads=[hTs[g % 2][j], hTgB, winB], writes=[psb[bk]])
                S.op("act", lambda e, bk=bk, q4=q4: e.activation(
                    out=gs[:, q4 * 512:(q4 + 1) * 512], in_=ps[bk], func=AF.Sigmoid),
                    reads=[psb[bk]], writes=[gsB])
            store(GATE[t * 128:(t + 1) * 128, :], gs, gsB, GATEB)
            bk = nexttok()
            for k in range(8):
                S.op("pe", lambda e, k=k, bk=bk: e.matmul(
                    ps[bk][:, 0:416], lhsT=hT3[:, k, j * 128:(j + 1) * 128], rhs=win3[:, k, IN_CQ:IN_CQ + 416],
                    start=(k == 0), stop=(k == 7)), reads=[hTs[g % 2][j], hTgB, winB], writes=[psb[bk]])
            lat = ps[bk]
            latB = psb[bk]
            S.op("act", lambda e: e.activation(out=junk2[:, 0:256], in_=lat[:, 0:256], func=AF.Square,
                                               accum_out=ss2[:, 1:2]), reads=[latB], writes=[junk2B, ss2B])
            S.op("act", lambda e: e.activation(out=junk2[:, 256:384], in_=lat[:, 256:384],
                                               func=AF.Square, accum_out=ss2[:, 2:3]),
                 reads=[latB], writes=[junk2B, ss2B])
            rms_rstd(ss2[:, 1:2], ss2B, 256, rstd2[:, 1:2], rstd2B)
            rms_rstd(ss2[:, 2:3], ss2B, 128, rstd2[:, 2:3], rstd2B)
            S.op("dve", lambda e: e.scalar_tensor_tensor(
                out=latb[:, 0:256], in0=lat[:, 0:256], scalar=rstd2[:, 1:2], in1=gq,
                op0=ALU.mult, op1=ALU.mult), reads=[latB, rstd2B, gqB], writes=[latbB])
            S.op("dve", lambda e: e.scalar_tensor_tensor(
                out=latb[:, 256:384], in0=lat[:, 256:384], scalar=rstd2[:, 2:3], in1=gkv,
                op0=ALU.mult, op1=ALU.mult), reads=[latB, rstd2B, gkvB], writes=[latbB])
            ck = cosk[:, t * 16:(t + 1) * 16]
            sk = sink[:, t * 16:(t + 1) * 16]
            x1 = lat[:, 384:400]
            x2 = lat[:, 400:416]
            for (o_, a_, b_) in ((0, x1, ck), (16, x2, sk), (32, x1, sk), (48, x2, ck)):
                S.op("dve", lambda e, o_=o_, a_=a_, b_=b_: e.tensor_tensor(
                    out=rt[:, o_:o_ + 16], in0=a_, in1=b_, op=ALU.mult),
                    reads=[latB, coskB, sinkB], writes=[rtB])
            S.op("dve", lambda e: e.tensor_tensor(out=latb[:, 384:400], in0=rt[:, 0:16], in1=rt[:, 16:32],
                                                  op=ALU.subtract), reads=[rtB], writes=[latbB])
            S.op("dve", lambda e: e.tensor_tensor(out=latb[:, 400:416], in0=rt[:, 32:48], in1=rt[:, 48:64],
                                                  op=ALU.add), reads=[rtB], writes=[latbB])
            for c4 in range(3):
                S.op("pe", lambda e, c4=c4: e.transpose(
                    out=psbf[1][:, c4 * 128:(c4 + 1) * 128], in_=latb[:, c4 * 128:(c4 + 1) * 128],
                    identity=identb), reads=[latbB, identbB], writes=[psb[1]])
            S.op("pe", lambda e: e.transpose(out=psbf[1][0:32, 384:512], in_=latb[:, 384:416], identity=identb),
                 reads=[latbB, identbB], writes=[psb[1]])
            S.op("act", lambda e: e.copy(
                out=lT3[:, 0:3, j * 128:(j + 1) * 128],
                in_=psbf[1][:, 0:384].rearrange("p (c t) -> p c t", c=3)), reads=[psb[1]], writes=[lTB])
            S.op("act", lambda e: e.copy(
                out=lT3[0:32, 3, j * 128:(j + 1) * 128], in_=psbf[1][0:32, 384:512]),
                reads=[psb[1]], writes=[lTB])

        def fm_a(g):
            hTg, hTgB = hT[g % 2]
            hT3 = hTg.rearrange("p (k t) -> p k t", k=8)
            lT, lTB = latT[g % 2]
            lT3 = lT.rearrange("p (c t) -> p c t", c=4)
            gsl = slice(g * 512, (g + 1) * 512)
            for which, col0, scale, dst, dstB in ((0, IN_Q, 0.125, QA, QAB), (1, IN_K, 1.0, KA, KAB)):
                for m in range(4):
                    bk = 6 + fmctr[0] % 2
                    fmctr[0] += 1
                    for k in range(8):
                        S.op("pe", lambda e, k=k, m=m, bk=bk, col0=col0: e.matmul(
                            ps[bk], lhsT=win3[:, k, col0 + m * 128:col0 + (m + 1) * 128], rhs=hT3[:, k, :],
                            start=(k == 0), stop=(k == 7)), reads=hTs[g % 2] + [hTgB, winB], writes=[psb[bk]])
                    qs, qsB = qst[fmctr[0] % 2]
                    S.op("act", lambda e, qs=qs, bk=bk, scale=scale: e.activation(
                        out=qs, in_=ps[bk], func=AF.Copy, scale=scale), reads=[psb[bk]], writes=[qsB])
                    store(dst[2 * m:2 * m + 2, :, gsl].rearrange("h d t -> (h d) t"), qs, qsB, dstB)
            store(CQT[0:128, gsl], lT3[:, 0, :], lTB, CQTB)
            store(CQT[128:256, gsl], lT3[:, 1, :], lTB, CQTB)
            store(CKVT[:, gsl], lT3[:, 2, :], lTB, CKVTB)
            store(KRT[:, gsl], lT3[0:32, 3, :], lTB, KRTB)

        prep_a(0)
        tr_a(0)
        for t in range(NT):
            if t + 1 < NT:
                prep_a(t + 1)
            mm_a(t)
            if t + 1 < NT:
                tr_a(t + 1)
            if t % 4 == 3:
                fm_a(t // 4)

    def phase_c1(l):
        A.reset(PERSIST)
        cq, cqB = A.alloc("cq", 2 * T, BF16)
        cq3 = cq.rearrange("p (c t) -> p c t", c=2)
        ckv, ckvB = A.alloc("ckv", T, BF16)
        load(cq3[:, 0, :], cqB, CQT[0:128, :], reads=[CQTB])
        load(cq3[:, 1, :], cqB, CQT[128:256, :], reads=[CQTB])
        load(ckv, ckvB, CKVT, reads=[CKVTB])
        wst, wstB = A.alloc("wst", 2 * 768, F32)
        wq, wqB = A.alloc("wq", 2 * 768, BF16)
        wqr, wqrB = A.alloc("wqr", 2 * 768, BF16)
        wk, wkB = A.alloc("wk", 512, BF16)
        wv, wvB = A.alloc("wv", 512, BF16)
        wst3 = wst.rearrange("p (k n) -> p k n", k=2)
        wq3 = wq.rearrange("p (k n) -> p k n", k=2)
        wqr3 = wqr.rearrange("p (k n) -> p k n", k=2)
        load(wst3, wstB, w_uq[l].rearrange("(k p) n -> p k n", p=128))
        S.op("dve", lambda e: e.tensor_copy(out=wq, in_=wst), reads=[wstB], writes=[wqB])
        load(wst3, wstB, w_uqr[l].rearrange("(k p) n -> p k n", p=128))
        S.op("dve", lambda e: e.tensor_copy(out=wqr, in_=wst), reads=[wstB], writes=[wqrB])
        load(wst[:, 0:512], wstB, w_ukvk[l])
        S.op("dve", lambda e: e.tensor_copy(out=wk, in_=wst[:, 0:512]), reads=[wstB], writes=[wkB])
        load(wst[:, 0:512], wstB, w_ukvv[l])
        S.op("dve", lambda e: e.tensor_copy(out=wv, in_=wst[:, 0:512]), reads=[wstB], writes=[wvB])
        cs = [A.alloc(f"cs{i}", 512, F32, parts=96) for i in range(2)]
        sn = [A.alloc(f"sn{i}", 512, F32, parts=96) for i in range(2)]
        qst = [A.alloc(f"qst{i}", 512, BF16, parts=96) for i in range(2)]
        kst = [A.alloc(f"kst{i}", 512, BF16, parts=64) for i in range(2)]
        vst = [A.alloc(f"vst{i}", 512, BF16) for i in range(2)]
        t1, t1B = A.alloc("t1", 512, F32, parts=96)
        t2, t2B = A.alloc("t2", 512, F32, parts=96)
        ctr = 0
        for g in range(NG):
            gsl = slice(g * 512, (g + 1) * 512)
            csg, csgB = cs[g % 2]
            sng, sngB = sn[g % 2]
            load(csg[64:96, :], csgB, cosq_in[:, gsl])
            load(sng[64:96, :], sngB, sinq_in[:, gsl])
            for h in range(8):
                ba, bb, bk_ = 0 + 3 * (ctr % 2), 1 + 3 * (ctr % 2), 2 + 3 * (ctr % 2)
                for k in range(2):
                    S.op("pe", lambda e, k=k, h=h, ba=ba, gsl=gsl: e.matmul(
                        ps[ba][0:96, :], lhsT=wq3[:, k, h * 96:(h + 1) * 96], rhs=cq3[:, k, gsl],
                        start=(k == 0), stop=(k == 1)), reads=[wqB, cqB], writes=[psb[ba]])
                for k in range(2):
                    S.op("pe", lambda e, k=k, h=h, bb=bb, gsl=gsl: e.matmul(
                        ps[bb][0:96, :], lhsT=wqr3[:, k, h * 96:(h + 1) * 96], rhs=cq3[:, k, gsl],
                        start=(k == 0), stop=(k == 1)), reads=[wqrB, cqB], writes=[psb[bb]])
                S.op("pe", lambda e, h=h, bk_=bk_, gsl=gsl: e.matmul(
                    ps[bk_][0:64, :], lhsT=wk[:, h * 64:(h + 1) * 64], rhs=ckv[:, gsl], start=True, stop=True),
                    reads=[wkB, ckvB], writes=[psb[bk_]])
                qs, qsB = qst[ctr % 2]
                ks, ksB = kst[ctr % 2]
                S.op("act", lambda e, qs=qs, ba=ba: e.copy(out=qs[0:64, :], in_=ps[ba][0:64, :]),
                     reads=[psb[ba]], writes=[qsB])
                S.op("dve", lambda e, ba=ba, csg=csg: e.tensor_tensor(
                    out=t1[64:96, :], in0=ps[ba][64:96, :], in1=csg[64:96, :], op=ALU.mult),
                    reads=[psb[ba], csgB], writes=[t1B])
                S.op("dve", lambda e, bb=bb, sng=sng: e.tensor_tensor(
                    out=t2[64:96, :], in0=ps[bb][64:96, :], in1=sng[64:96, :], op=ALU.mult),
                    reads=[psb[bb], sngB], writes=[t2B])
                S.op("dve", lambda e, qs=qs: e.tensor_tensor(
                    out=qs[64:96, :], in0=t1[64:96, :], in1=t2[64:96, :], op=ALU.add),
                    reads=[t1B, t2B], writes=[qsB])
                S.op("act", lambda e, ks=ks, bk_=bk_: e.copy(out=ks, in_=ps[bk_][0:64, :]),
                     reads=[psb[bk_]], writes=[ksB])
                store(QM[h, :, gsl], qs, qsB, QMB)
                store(KM[h, :, gsl], ks, ksB, KMB)
                ctr += 1
        for t in range(NT):
            bk = 6 + t % 2
            S.op("pe", lambda e, t=t, bk=bk: e.matmul(
                ps[bk][:, :], lhsT=ckv[:, t * 128:(t + 1) * 128], rhs=wv, start=True, stop=True),
                reads=[ckvB, wvB], writes=[psb[bk]])
            vs, vsB = vst[t % 2]
            S.op("dve", lambda e, vs=vs, bk=bk: e.tensor_copy(out=vs, in_=ps[bk][:, :]),
                 reads=[psb[bk]], writes=[vsB])
            store(VM[:, t * 128:(t + 1) * 128, :].rearrange("h t d -> t h d"),
                  vs.rearrange("p (h d) -> p h d", h=8), vsB, VMB)

    def phase_c2(l, QT, KT, first):
        A.reset(ATT_MARK)
        V = [A.alloc(f"V{i}", NT * 65, BF16) for i in range(2)]
        PT = [A.alloc(f"PT{i}", 1024, BF16) for i in range(3)]
        otf, otfB = A.alloc("otf", 512, F32, parts=65)
        rc, rcB = A.alloc("rc", 4, F32)
        mo = [A.alloc(f"mo{i}", 4 * 64, BF16) for i in range(2)]
        for i in range(2):
            Vi, ViB = V[i]
            S.op("pool", lambda e, Vi=Vi: e.memset(Vi, 1.0), writes=[ViB])
        sc = 96.0 ** -0.5
        for i in range(2):
            load(KT[i][0][64:96, :], KT[i][1], KRT, reads=[KRTB])
        pctr = 0
        octr = 0
        NKP = NT // 2
        deferred = []
        for h in range(8):
            QTh, QThB = QT[h % 2]
            KTh, KThB = KT[h % 2]
            Vh, VhB = V[h % 2]
            Vh3 = Vh.rearrange("p (t d) -> p t d", d=65)
            load(QTh[0:96, :], QThB, QM[h], reads=[QMB])
            load(KTh[0:64, :], KThB, KM[h], reads=[KMB])
            S.dma("sp", lambda e, Vh3=Vh3, h=h: e.dma_start(
                out=Vh3[:, :, 0:64], in_=VM[h].rearrange("(t p) d -> p t d", p=128)), VhB,
                reads=[VMB], writes=[VhB])
            for g in range(NG):
                gsl = slice(g * 512, (g + 1) * 512)
                ob = 6 + octr % 2
                octr += 1
                pend = []

                def emit_pv(item, ob=ob, Vh3=Vh3, VhB=VhB):
                    kp, ppt, pptB = item
                    for u in range(2):
                        kt = 2 * kp + u
                        S.op("pe", lambda e, kt=kt, u=u, ppt=ppt, Vh3=Vh3, ob=ob: e.matmul(
                            ps[ob][0:65, :], lhsT=Vh3[:, kt, :], rhs=ppt[:, u * 512:(u + 1) * 512],
                            start=(kt == 0), stop=(kt == NT - 1)),
                            reads=[VhB, pptB], writes=[psb[ob]])

                for kp in range(NKP):
                    sj = pctr % 2
                    pt, ptB = PT[pctr % 3]
                    pctr += 1
                    for u in range(2):
                        kt = 2 * kp + u
                        S.op("pe", lambda e, kt=kt, u=u, sj=sj, KTh=KTh, QTh=QTh, gsl=gsl: e.matmul(
                            ps[2 * sj + u], lhsT=KTh[0:100, kt * 128:(kt + 1) * 128], rhs=QTh[0:100, gsl],
                            start=True, stop=True), reads=[KThB, QThB], writes=[psb[2 * sj], psb[2 * sj + 1]])
                    S.op("act", lambda e, pt=pt, sj=sj: e.activation(
                        out=pt, in_=ps2[sj], func=AF.Exp, scale=sc),
                        reads=[psb[2 * sj], psb[2 * sj + 1]], writes=[ptB])
                    pend.append((kp, pt, ptB))
                    if len(pend) > 1:
                        emit_pv(pend.pop(0))
                    if kp == min(2, NKP - 1) and deferred:
                        deferred.pop(0)()
                while pend:
                    emit_pv(pend.pop(0))
                def epilogue(ob=ob, gsl=gsl, h=h, octr=octr):
                    S.op("dve", lambda e: e.tensor_copy(out=otf, in_=ps[ob][0:65, :]),
                         reads=[psb[ob]], writes=[otfB])
                    for j in range(4):
                        S.op("pe", lambda e, j=j: e.transpose(
                            out=ps[5][:, j * 65:(j + 1) * 65], in_=otf[:, j * 128:(j + 1) * 128],
                            identity=identf[0:65, 0:65]), reads=[otfB, identfB], writes=[psb[5]])
                    p5 = ps[5][:, 0:260].rearrange("p (j d) -> p j d", d=65)
                    S.op("dve", lambda e: e.reciprocal(out=rc, in_=p5[:, :, 64]), reads=[psb[5]], writes=[rcB])
                    mg, mgB = mo[octr % 2]
                    mg3 = mg.rearrange("p (j d) -> p j d", d=64)
                    S.op("dve", lambda e: e.tensor_tensor(
                        out=mg3, in0=p5[:, :, 0:64], in1=rc.unsqueeze(2).broadcast_to([128, 4, 64]), op=ALU.mult),
                        reads=[psb[5], rcB], writes=[mgB])
                    store(MLAO[gsl, h * 64:(h + 1) * 64].rearrange("(j p) d -> p j d", p=128), mg3, mgB, MLAOB)
                deferred.append(epilogue)

        while deferred:
            deferred.pop(0)()

    def phase_b(l, QN, KN):
        A.reset(ATT_MARK)
        VN = [A.alloc(f"VN{i}", NT * 65, BF16) for i in range(2)]
        ttf, ttfB = A.alloc("ttf", 9 * 128, F32)
        ttb = [A.alloc(f"ttb{i}", 9 * 128, BF16) for i in range(2)]
        PN = [A.alloc(f"PN{i}", 9 * 128, BF16) for i in range(2)]
        rc, rcB = A.alloc("rcn", 1, F32)
        no = [A.alloc(f"no{i}", 4 * 64, BF16) for i in range(2)]
        for i in range(2):
            Vi, ViB = VN[i]
            S.op("pool", lambda e, Vi=Vi: e.memset(Vi, 1.0), writes=[ViB])
        pctr = 0
        for h in range(8):
            QNh, QNhB = QN[h % 2]
            KNh, KNhB = KN[h % 2]
            Vh, VhB = VN[h % 2]
            Vh3 = Vh.rearrange("p (t d) -> p t d", d=65)
            tb, tbB = ttb[h % 2]
            tb3 = tb.rearrange("p (d q) -> p d q", d=9)
            load(QNh[0:64, :], QNhB, QA[h], reads=[QAB])
            load(KNh[0:64, :], KNhB, KA[h], reads=[KAB])
            S.dma("sp", lambda e, Vh3=Vh3, h=h: e.dma_start(
                out=Vh3[:, :, 0:64], in_=VNA[:, h * 64:(h + 1) * 64].rearrange("(t p) d -> p t d", p=128)), VhB,
                reads=[VNAB], writes=[VhB])
            load(ttf, ttfB, tt_in[l, h])
            S.op("pool", lambda e, tb=tb: e.tensor_copy(out=tb, in_=ttf), reads=[ttfB], writes=[tbB])
            def emit_s(p, KNh=KNh, KNhB=KNhB, QNh=QNh, QNhB=QNhB, tb3=tb3, tbB=tbB):
                tiles = [a for a in range(p - 4, p + 5) if 0 <= a < NT]
                pn, pnB = PN[p % 2]
                sb0 = 3 * (p % 2)
                psl = slice(p * 128, (p + 1) * 128)
                for i, a in enumerate(tiles):
                    bk = sb0 + i // 4
                    csl = slice((i % 4) * 128, (i % 4 + 1) * 128)
                    S.op("pe", lambda e, a=a, bk=bk, csl=csl, psl=psl: e.matmul(
                        ps[bk][:, csl], lhsT=KNh[0:96, a * 128:(a + 1) * 128], rhs=QNh[0:96, psl],
                        start=True, stop=False), reads=[KNhB, QNhB], writes=[psb[bk]])
                    S.op("pe", lambda e, a=a, bk=bk, csl=csl, p=p: e.matmul(
                        ps[bk][:, csl], lhsT=identb, rhs=tb3[:, a - p + 4, :], start=False, stop=True),
                        reads=[identbB, tbB], writes=[psb[bk]])
                nt_ = len(tiles)
                for b0 in range(0, nt_, 4):
                    n4 = min(4, nt_ - b0)
                    bk = sb0 + b0 // 4
                    S.op("act", lambda e, pn=pn, bk=bk, b0=b0, n4=n4: e.activation(
                        out=pn[:, b0 * 128:(b0 + n4) * 128], in_=ps[bk][:, 0:n4 * 128], func=AF.Exp),
                        reads=[psb[bk]], writes=[pnB])

            def emit_pv(p, Vh3=Vh3, VhB=VhB, h=h):
                tiles = [a for a in range(p - 4, p + 5) if 0 <= a < NT]
                nt_ = len(tiles)
                pn, pnB = PN[p % 2]
                ob = 6 + p % 2
                for i, a in enumerate(tiles):
                    S.op("pe", lambda e, i=i, a=a, pn=pn, ob=ob, nt_=nt_: e.matmul(
                        ps[ob][:, 0:65], lhsT=pn[:, i * 128:(i + 1) * 128], rhs=Vh3[:, a, :],
                        start=(i == 0), stop=(i == nt_ - 1)), reads=[pnB, VhB], writes=[psb[ob]])
                S.op("dve", lambda e, ob=ob: e.reciprocal(out=rc, in_=ps[ob][:, 64:65]),
                     reads=[psb[ob]], writes=[rcB])
                ng, ngB = no[(p // 4) % 2]
                S.op("dve", lambda e, ob=ob, ng=ng, p=p: e.tensor_scalar(
                    out=ng[:, (p % 4) * 64:(p % 4 + 1) * 64], in0=ps[ob][:, 0:64], scalar1=rc[:, 0:1],
                    scalar2=None, op0=ALU.mult), reads=[psb[ob], rcB], writes=[ngB])
                if p % 4 == 3:
                    p0 = p - 3
                    store(NAO[p0 * 128:(p0 + 4) * 128, h * 64:(h + 1) * 64].rearrange("(j p) d -> p j d", p=128),
                          ng.rearrange("p (j d) -> p j d", d=64), ngB, NAOB)

            emit_s(0)
            for p in range(NT):
                if p + 1 < NT:
                    emit_s(p + 1)
                emit_pv(p)

    def load_w_bf16(dst3, dstB, src3, st, stB, ncols, k, eng="pool"):
        for c0 in range(0, ncols, 512):
            cw = min(512, ncols - c0)
            st3 = st[:, 0:k * cw].rearrange("p (k n) -> p k n", k=k)
            load(st3, stB, src3[:, :, c0:c0 + cw])
            S.op(eng, lambda e, st3=st3, c0=c0, cw=cw: e.tensor_copy(out=dst3[:, :, c0:c0 + cw], in_=st3),
                 reads=[stB], writes=[dstB])

    def phase_de(l, R):
        A.reset(MOE_MARK)
        st, stB = A.alloc("st", 8 * 512, F32)
        wna, wnaB = A.alloc("wna", 4 * D, BF16)
        wml, wmlB = A.alloc("wml", 4 * D, BF16)
        wo, woB = A.alloc("wo", 8 * D, BF16)
        wr, wrB = A.alloc("wr", 8 * 36, BF16)
        wna3 = wna.rearrange("p (k n) -> p k n", k=4)
        wml3 = wml.rearrange("p (k n) -> p k n", k=4)
        wo3 = wo.rearrange("p (k n) -> p k n", k=8)
        wr3 = wr.rearrange("p (k n) -> p k n", k=8)
        load_w_bf16(wna3, wnaB, w_nao[l].rearrange("(k p) n -> p k n", p=128), st, stB, D, 4)
        load_w_bf16(wml3, wmlB, w_mlao[l].rearrange("(k p) n -> p k n", p=128), st, stB, D, 4)
        load_w_bf16(wo3, woB, w_out[l].rearrange("(k p) n -> p k n", p=128), st, stB, D, 8)
        load_w_bf16(wr3, wrB, w_r[l].rearrange("(k p) n -> p k n", p=128), st, stB, 36, 8)
        brt, brtB = A.alloc("brt", 36, F32)
        bload(brt, brtB, b_r[l:l + 1, :])
        gf, gfB = A.alloc("gf", D, F32)
        bload(gf, gfB, g_ffn[l:l + 1, :])
        g1b, g1bB = A.alloc("g1b", D, F32)
        G2, G2B = A.alloc("G2", D, F32)
        SH2, SH2B = A.alloc("SH2", D, F32)
        ao = [A.alloc(f"ao{i}", 1024, BF16) for i in range(3)]
        gt = [A.alloc(f"gt{i}", 2048, BF16) for i in range(3)]
        xt = [A.alloc(f"xd{i}", D, F32) for i in range(4)]
        aT, aTB = A.alloc("aT", 8 * 128, BF16)
        m1, m1B = A.alloc("m1", D, F32)
        m2, m2B = A.alloc("m2", D, F32)
        mgb, mgbB = A.alloc("mgb", D, BF16)
        mT, mTB = A.alloc("mT", 8 * 128, BF16)
        xn, xnB = A.alloc("xn", D, F32)
        junk, junkB = A.alloc("junkd", D, F32)
        ss, ssB = A.alloc("ssd", 1, F32)
        rstd, rstdB = A.alloc("rstdd", 1, F32)
        h2b = [A.alloc(f"h2b{i}", D, BF16) for i in range(2)]
        h2T, h2TB = A.alloc("h2T", 8 * 128, BF16)
        sm, smB = A.alloc("sm", 256, F32)
        ab, abB = A.alloc("ab", 32, BF16)
        S.op("dve", lambda e: e.memset(R["carry"], 0.0), writes=[R["carryB"]])
        mgbs = [A.alloc(f"mgb{i}", D, BF16) for i in range(2)]
        zt, ztB = A.alloc("zt", D, F32)
        aT3 = aT.rearrange("p (k t) -> p k t", k=8)
        mT3 = mT.rearrange("p (k t) -> p k t", k=8)
        h2T3 = h2T.rearrange("p (k t) -> p k t", k=8)

        def s1_load(t):
            tsl = slice(t * 128, (t + 1) * 128)
            aoi, aoiB = ao[t % 3]
            gti, gtiB = gt[t % 3]
            xti, xtiB = xt[t % 4]
            load(aoi[:, 0:512], aoiB, NAO[tsl, :], reads=[NAOB])
            load(aoi[:, 512:1024], aoiB, MLAO[tsl, :], reads=[MLAOB])
            load(gti, gtiB, GATE[tsl, :], reads=[GATEB])
            src = x_in if l == 0 else X
            load(xti, xtiB, src[tsl, :], reads=([] if l == 0 else [XB[t]]))

        def s1(t):
            tsl = slice(t * 128, (t + 1) * 128)
            aoi, aoiB = ao[t % 3]
            gti, gtiB = gt[t % 3]
            xti, xtiB = xt[t % 4]
            for k in range(8):
                S.op("pe", lambda e, k=k: e.transpose(
                    out=psbf[0][:, k * 128:(k + 1) * 128], in_=aoi[:, k * 128:(k + 1) * 128], identity=identb),
                    reads=[aoiB, identbB], writes=[psb[0]])
            S.op("act", lambda e: e.copy(out=aT, in_=psbf[0]), reads=[psb[0]], writes=[aTB])
            for hf in range(2):
                hs = slice(hf * 512, (hf + 1) * 512)
                for (w3_, wB_, bk, k0) in ((wna3, wnaB, 1, 0), (wml3, wmlB, 2, 4)):
                    for k in range(4):
                        S.op("pe", lambda e, k=k, w3_=w3_, bk=bk, k0=k0, hs=hs: e.matmul(
                            ps[bk], lhsT=aT3[:, k0 + k, :], rhs=w3_[:, k, hs],
                            start=(k == 0), stop=(k == 3)), reads=[aTB, wB_], writes=[psb[bk]])
                S.op("dve", lambda e, hs=hs: e.tensor_tensor(
                    out=m1[:, hs], in0=ps[1], in1=gti[:, hs], op=ALU.mult),
                    reads=[psb[1], gtiB], writes=[m1B])
                S.op("dve", lambda e, hf=hf, hs=hs: e.tensor_tensor(
                    out=m2[:, hs], in0=ps[2], in1=gti[:, 1024 + hf * 512:1024 + (hf + 1) * 512],
                    op=ALU.mult), reads=[psb[2], gtiB], writes=[m2B])
            mg_, mg_B = mgbs[t % 2]
            S.op("pool", lambda e: e.tensor_tensor(out=mg_, in0=m1, in1=m2, op=ALU.add),
                 reads=[m1B, m2B], writes=[mg_B])

        def s2(t):
            tsl = slice(t * 128, (t + 1) * 128)
            if t % SLOT_T == 0:
                s_ = t // SLOT_T
                bload(g1b, g1bB, mod_row(l, s_, 2), reads=[MODSB])
                bload(G2, G2B, mod_row(l, s_, 4), reads=[MODSB])
                bload(SH2, SH2B, mod_row(l, s_, 3), reads=[MODSB])
                S.op("dve", lambda e: e.scalar_tensor_tensor(
                    out=G2, in0=G2, scalar=1.0, in1=gf, op0=ALU.add, op1=ALU.mult),
                    reads=[G2B, gfB], writes=[G2B])
            xti, xtiB = xt[t % 4]
            mg_, mg_B = mgbs[t % 2]
            for k in range(8):
                S.op("pe", lambda e, k=k: e.transpose(
                    out=psbf[3][:, k * 128:(k + 1) * 128], in_=mg_[:, k * 128:(k + 1) * 128], identity=identb),
                    reads=[mg_B, identbB], writes=[psb[3]])
            S.op("act", lambda e: e.copy(out=mT, in_=psbf[3]), reads=[psb[3]], writes=[mTB])
            for hf in range(2):
                bk = 4 + hf
                hs = slice(hf * 512, (hf + 1) * 512)
                for k in range(8):
                    S.op("pe", lambda e, k=k, bk=bk, hs=hs: e.matmul(
                        ps[bk], lhsT=mT3[:, k, :], rhs=wo3[:, k, hs],
                        start=(k == 0), stop=(k == 7)), reads=[mTB, woB], writes=[psb[bk]])
                S.op("dve", lambda e, bk=bk, hs=hs: e.tensor_tensor(
                    out=zt[:, hs], in0=ps[bk], in1=g1b[:, hs], op=ALU.mult),
                    reads=[psb[bk], g1bB], writes=[ztB])
            S.op("pool", lambda e: e.tensor_tensor(out=xn, in0=zt, in1=xti, op=ALU.add),
                 reads=[ztB, xtiB], writes=[xnB])
            store(X[tsl, :], xn, xnB, XB[t])
            S.op("act", lambda e: e.activation(out=junk, in_=xn, func=AF.Square, accum_out=ss),
                 reads=[xnB], writes=[junkB, ssB])
            rms_rstd(ss, ssB, D, rstd, rstdB)
            S.op("dve", lambda e: e.scalar_tensor_tensor(
                out=zt, in0=xn, scalar=rstd[:, 0:1], in1=G2, op0=ALU.mult, op1=ALU.mult),
                reads=[xnB, rstdB, G2B], writes=[ztB])
            hbi, hbiB = h2b[t % 2]
            S.op("pool", lambda e: e.tensor_tensor(out=hbi, in0=zt, in1=SH2, op=ALU.add),
                 reads=[ztB, SH2B], writes=[hbiB])
            store(H2[tsl, :], hbi, hbiB, H2B)

        def s3(t):
            hbi, hbiB = h2b[t % 2]
            for k in range(8):
                S.op("pe", lambda e, k=k: e.transpose(
                    out=psbf[6][:, k * 128:(k + 1) * 128], in_=hbi[:, k * 128:(k + 1) * 128], identity=identb),
                    reads=[hbiB, identbB], writes=[psb[6]])
            S.op("act", lambda e: e.copy(out=h2T, in_=psbf[6]), reads=[psb[6]], writes=[h2TB])
            for k in range(8):
                S.op("pe", lambda e, k=k: e.matmul(
                    ps[7][:, 0:36], lhsT=h2T3[:, k, :], rhs=wr3[:, k, :], start=(k == 0), stop=(k == 7)),
                    reads=[h2TB, wrB], writes=[psb[7]])
            route_tile(t, R, ps[7], psb[7], brt, brtB, sm, smB, ab, abB)

        s1_load(0)
        for i in range(NT + 2):
            if i + 1 < NT:
                s1_load(i + 1)
            if i < NT:
                s1(i)
            if 0 <= i - 1 < NT:
                s2(i - 1)
            if 0 <= i - 2 < NT:
                s3(i - 2)

    def route_tile(t, R, lgp, lgpB, brt, brtB, sm, smB, ab, abB):
        M1, M2, WA, WB, POS, carry = R["M1"], R["M2"], R["WA"], R["WB"], R["POS"], R["carry"]
        RB = R["RB"]
        M1t = M1[:, t * 32:(t + 1) * 32]
        M2t = M2[:, t * 32:(t + 1) * 32]

        def dve(fn, reads, writes):
            S.op("dve", fn, reads=reads, writes=writes)
        dve(lambda e: e.tensor_tensor(out=sm[:, 0:36], in0=lgp[:, 0:36], in1=brt, op=ALU.add),
            [lgpB, brtB], [smB])
        dve(lambda e: e.reduce_max(out=sm[:, 36:37], in_=sm[:, 0:4], axis=AX.X), [smB], [smB])
        dve(lambda e: e.tensor_scalar(out=sm[:, 37:38], in0=sm[:, 36:37], scalar1=-1.0, scalar2=None,
                                      op0=ALU.mult), [smB], [smB])
        S.op("act", lambda e: e.activation(out=sm[:, 116:120], in_=sm[:, 0:4], func=AF.Exp, bias=sm[:, 37:38],
                                           accum_out=sm[:, 38:39]), reads=[smB], writes=[smB])
        dve(lambda e: e.reciprocal(out=sm[:, 39:40], in_=sm[:, 38:39]), [smB], [smB])
        dve(lambda e: e.tensor_scalar(out=sm[:, 40:44], in0=sm[:, 0:4], scalar1=sm[:, 36:37], scalar2=None,
                                      op0=ALU.is_equal), [smB], [smB])
        dve(lambda e: e.tensor_scalar(out=sm[:, 44:48], in0=sm[:, 40:44], scalar1=-1.0, scalar2=-NEG,
                                      op0=ALU.add, op1=ALU.mult), [smB], [smB])
        dve(lambda e: e.tensor_tensor(
            out=sm[:, 48:80].rearrange("p (g e) -> p g e", g=4),
            in0=sm[:, 4:36].rearrange("p (g e) -> p g e", g=4),
            in1=sm[:, 44:48].unsqueeze(2).broadcast_to([128, 4, 8]), op=ALU.add), [smB], [smB])
        dve(lambda e: e.reduce_max(out=sm[:, 80:81], in_=sm[:, 48:80], axis=AX.X), [smB], [smB])
        dve(lambda e: e.tensor_scalar(out=M1t, in0=sm[:, 48:80], scalar1=sm[:, 80:81], scalar2=None,
                                      op0=ALU.is_equal), [smB], [RB])
        dve(lambda e: e.scalar_tensor_tensor(out=sm[:, 84:116], in0=M1t, scalar=NEG, in1=sm[:, 48:80],
                                             op0=ALU.mult, op1=ALU.add), [smB, RB], [smB])
        dve(lambda e: e.reduce_max(out=sm[:, 81:82], in_=sm[:, 84:116], axis=AX.X), [smB], [smB])
        dve(lambda e: e.tensor_scalar(out=M2t, in0=sm[:, 84:116], scalar1=sm[:, 81:82], scalar2=None,
                                      op0=ALU.is_equal), [smB], [RB])
        dve(lambda e: e.tensor_tensor(out=sm[:, 82:83], in0=sm[:, 80:81], in1=sm[:, 81:82], op=ALU.subtract),
            [smB], [smB])
        S.op("act", lambda e: e.activation(out=sm[:, 83:84], in_=sm[:, 82:83], func=AF.Sigmoid),
             reads=[smB], writes=[smB])
        dve(lambda e: e.tensor_tensor(out=WA[:, t:t + 1], in0=sm[:, 83:84], in1=sm[:, 39:40], op=ALU.mult),
            [smB], [RB])
        dve(lambda e: e.tensor_tensor(out=WB[:, t:t + 1], in0=sm[:, 39:40], in1=WA[:, t:t + 1], op=ALU.subtract),
            [smB, RB], [RB])
        dve(lambda e: e.tensor_tensor(out=ab, in0=M1t, in1=M2t, op=ALU.add), [RB], [abB])
        S.op("pe", lambda e: e.matmul(lgp[:, 64:96], lhsT=tril, rhs=ab, start=True, stop=True),
             reads=[trilB, abB], writes=[lgpB])
        S.op("pe", lambda e: e.matmul(lgp[:, 96:128], lhsT=onesb, rhs=ab, start=True, stop=True),
             reads=[onesbB, abB], writes=[lgpB])
        dve(lambda e: e.tensor_tensor(out=POS[:, t * 32:(t + 1) * 32], in0=lgp[:, 64:96], in1=carry, op=ALU.add),
            [lgpB, R["carryB"]], [RB])
        dve(lambda e: e.tensor_tensor(out=carry, in0=lgp[:, 96:128], in1=carry, op=ALU.add),
            [lgpB, R["carryB"]], [R["carryB"]])

    def phase_fg(l, R, last):
        A.reset(MOE2_MARK)
        M1, M2, WA, WB, POS, carry, DI, RB = (R[k] for k in ("M1", "M2", "WA", "WB", "POS", "carry", "DI", "RB"))
        fz, fzB = A.alloc("fz", 8 * 32, F32)
        pst, pstB = A.alloc("pst", 32, F32)
        be_f, be_fB = A.alloc("be_f", NB, F32)
        bei, beiB = A.alloc("bei", NB, I32)
        blk, blkB = A.alloc("blk", NB, F32)
        m_big = A.mark()
        big, bigB = A.alloc("big", NB * 32, F32)
        load(blk, blkB, blk_in)

        def dve(fn, reads, writes):
            S.op("dve", fn, reads=reads, writes=writes)
        cB = R["carryB"]
        dve(lambda e: e.tensor_tensor(
            out=big.rearrange("p (e b) -> p e b", e=32),
            in0=carry.unsqueeze(2).broadcast_to([128, 32, NB]),
            in1=blk.unsqueeze(1).broadcast_to([128, 32, NB]), op=ALU.is_gt), [cB, blkB], [bigB])
        dve(lambda e: e.reduce_sum(out=fz[:, 32:64], in_=big.rearrange("p (e b) -> p e b", e=32), axis=AX.X),
            [bigB], [fzB])
        dve(lambda e: e.tensor_scalar(out=fz[:, 64:96], in0=fz[:, 32:64], scalar1=128.0, scalar2=None,
                                      op0=ALU.mult), [fzB], [fzB])
        cur = 64
        for i, sh in enumerate((1, 2, 4, 8, 16)):
            nxt = 96 if cur != 96 else 128
            dve(lambda e, cur=cur, nxt=nxt: e.tensor_copy(out=fz[:, nxt:nxt + 32], in_=fz[:, cur:cur + 32]),
                [fzB], [fzB])
            dve(lambda e, cur=cur, nxt=nxt, sh=sh: e.tensor_tensor(
                out=fz[:, nxt + sh:nxt + 32], in0=fz[:, cur + sh:cur + 32], in1=fz[:, cur:cur + 32 - sh],
                op=ALU.add), [fzB], [fzB])
            cur = nxt
        pend = fz[:, cur:cur + 32]
        dve(lambda e: e.tensor_tensor(out=pst, in0=pend, in1=fz[:, 64:96], op=ALU.subtract), [fzB], [pstB])
        dve(lambda e: e.tensor_tensor(
            out=big.rearrange("p (b e) -> p b e", e=32),
            in0=pend.unsqueeze(1).broadcast_to([128, NB, 32]),
            in1=blk.unsqueeze(2).broadcast_to([128, NB, 32]), op=ALU.is_le), [fzB, blkB], [bigB])
        dve(lambda e: e.reduce_sum(out=be_f, in_=big.rearrange("p (b e) -> p b e", e=32), axis=AX.X),
            [bigB], [be_fB])
        dve(lambda e: e.tensor_scalar(out=be_f, in0=be_f, scalar1=31.0, scalar2=128.0, op0=ALU.min, op1=ALU.mult),
            [be_fB], [be_fB])
        dve(lambda e: e.tensor_scalar(out=be_f, in0=be_f, scalar1=iotap[:, 0:1], scalar2=float(l * 32 * 128),
                                      op0=ALU.add, op1=ALU.add), [be_fB, iotapB], [be_fB])
        sameb, samebB = A.alloc("sameb", NB, F32)
        dve(lambda e: e.memset(sameb[:, 0:2], 0.0), [], [samebB])
        dve(lambda e: e.tensor_tensor(out=sameb[:, 2:NB], in0=be_f[:, 2:NB], in1=be_f[:, 0:NB - 2],
                                      op=ALU.is_equal), [be_fB], [samebB])
        dve(lambda e: e.scalar_tensor_tensor(out=be_f, in0=sameb, scalar=1.0e7, in1=be_f, op0=ALU.mult,
                                             op1=ALU.add), [samebB, be_fB], [be_fB])
        dve(lambda e: e.tensor_copy(out=bei, in_=be_f), [be_fB], [beiB])
        hx = [A.alloc(f"hx{i}", D, BF16) for i in range(4)]
        df, dfB = A.alloc("df", 64, F32)
        for t in range(NT):
            tsl = slice(t * 128, (t + 1) * 128)
            hxi, hxiB = hx[t % 4]
            load(hxi, hxiB, H2[tsl, :], reads=[H2B])
            dve(lambda e, t=t: e.tensor_tensor(out=df[:, 0:32], in0=POS[:, t * 32:(t + 1) * 32], in1=pst, op=ALU.add),
                [RB, pstB], [dfB])
            for j, M in enumerate((M1, M2)):
                dve(lambda e, t=t, M=M: e.tensor_tensor(out=df[:, 32:64], in0=df[:, 0:32],
                                                        in1=M[:, t * 32:(t + 1) * 32], op=ALU.mult),
                    [dfB, RB], [dfB])
                dve(lambda e, t=t, j=j: e.reduce_sum(out=R["DF"][:, 2 * t + j:2 * t + j + 1], in_=df[:, 32:64],
                                                      axis=AX.X), [dfB], [R["DFB"]])
            dve(lambda e, t=t: e.tensor_copy(out=DI[:, 2 * t:2 * t + 2], in_=R["DF"][:, 2 * t:2 * t + 2]),
                [R["DFB"]], [R["DIB"]])
            if stop == "fg1":
                continue
            for j in range(2):
                S.dma("pool", lambda e, t=t, j=j, hxi=hxi: e.indirect_dma_start(
                    out=BUFD, out_offset=bass.IndirectOffsetOnAxis(ap=DI[:, 2 * t + j:2 * t + j + 1], axis=0),
                    in_=hxi, in_offset=None, bounds_check=S.pool_regs["nb"], oob_is_err=False), hxiB,
                    reads=[hxiB, R["DIB"]], writes=[BUFDB])
        if stop == "fg1":
            d1 = nc.dram_tensor("DBG_DI", [128, 2 * NT], I32, kind="ExternalOutput").ap()
            d2 = nc.dram_tensor("DBG_BEI", [128, NB], I32, kind="ExternalOutput").ap()
            d3 = nc.dram_tensor("DBG_F", [128, 64], F32, kind="ExternalOutput").ap()
            d4 = nc.dram_tensor("DBG_RT", [128, NT * 32 * 3 + 2 * NT], F32, kind="ExternalOutput").ap()
            S.dma("sp", lambda e: e.dma_start(out=d1, in_=DI), R["DIB"], reads=[R["DIB"]])
            S.dma("sp", lambda e: e.dma_start(out=d2, in_=bei), beiB, reads=[beiB])
            S.dma("sp", lambda e: e.dma_start(out=d3[:, 0:32], in_=carry), cB, reads=[cB])
            S.dma("sp", lambda e: e.dma_start(out=d3[:, 32:64], in_=pst), pstB, reads=[pstB])
            S.dma("sp", lambda e: e.dma_start(out=d4, in_=R["RT"]), RB, reads=[RB])
            final_bufs.extend([R["DIB"], beiB, cB, pstB, RB])
            return
        if stop == "fg2":
            return
        A.reset(m_big)
        m_exp = A.mark()
        wf = [[A.alloc(f"wf{i}_{j}", 4096, F32) for j in range(2)] for i in range(3)]
        wb_ = [[A.alloc(f"wb{i}_{j}", 4096, BF16) for j in range(2)] for i in range(3)]
        xb = [A.alloc(f"xb{i}", D, BF16) for i in range(2)]
        xbT, xbTB = A.alloc("xbT", 8 * 128, BF16)
        sl, slB = A.alloc("sl", 512, F32)
        hh, hhB = A.alloc("hh", 512, BF16)
        hhT, hhTB = A.alloc("hhT", 4 * 128, BF16)
        yst = [A.alloc(f"yst{i}", D, F32) for i in range(2)]
        casts = ("act", "dve", "pool")
        for b in range(NB):
            bsl = slice(b * 128, (b + 1) * 128)
            wbs = []
            for i, wsrc in enumerate((w1r, w3r, w2r)):
                wfi, wfiB = wf[i][b % 2]
                wbi, wbiB = wb_[i][b % 2]
                S.dma("pool", lambda e, wfi=wfi, wsrc=wsrc, b=b: e.indirect_dma_start(
                    out=wfi, out_offset=None, in_=wsrc,
                    in_offset=bass.IndirectOffsetOnAxis(ap=bei[:, b:b + 1], axis=0),
                    bounds_check=S.pool_regs["wrows"], oob_is_err=False), wfiB,
                    reads=[beiB], writes=[wfiB])
                if i == 0:
                    S.op("act", lambda e, wbi=wbi, wfi=wfi: e.copy(out=wbi, in_=wfi), reads=[wfiB], writes=[wbiB])
                elif i == 1:
                    S.op("dve", lambda e, wbi=wbi, wfi=wfi: e.tensor_copy(out=wbi, in_=wfi),
                         reads=[wfiB], writes=[wbiB])
                else:
                    S.op("act", lambda e, wbi=wbi, wfi=wfi: e.copy(out=wbi[:, 0:2048], in_=wfi[:, 0:2048]),
                         reads=[wfiB], writes=[wbiB])
                    S.op("dve", lambda e, wbi=wbi, wfi=wfi: e.tensor_copy(out=wbi[:, 2048:4096], in_=wfi[:, 2048:4096]),
                         reads=[wfiB], writes=[wbiB])
                wbs.append((wbi, wbiB))
            xbi, xbiB = xb[b % 2]
            if b == 0:
                load(xbi, xbiB, BUFD[bsl, :], reads=[BUFDB])
            if b + 1 < NB:
                xbn, xbnB = xb[(b + 1) % 2]
                load(xbn, xbnB, BUFD[(b + 1) * 128:(b + 2) * 128, :], reads=[BUFDB])
            for k in range(8):
                S.op("pe", lambda e, k=k, xbi=xbi: e.transpose(
                    out=psbf[0][:, k * 128:(k + 1) * 128], in_=xbi[:, k * 128:(k + 1) * 128], identity=identb),
                    reads=[xbiB, identbB], writes=[psb[0]])
            S.op("act", lambda e: e.copy(out=xbT, in_=psbf[0]), reads=[psb[0]], writes=[xbTB])
            xbT3 = xbT.rearrange("p (k t) -> p k t", k=8)
            w13 = wbs[0][0].rearrange("p (k n) -> p k n", k=8)
            w33 = wbs[1][0].rearrange("p (k n) -> p k n", k=8)
            w23 = wbs[2][0].rearrange("p (k n) -> p k n", k=4)
            for (w_, wB_, bk) in ((w13, wbs[0][1], 1), (w33, wbs[1][1], 2)):
                for k in range(8):
                    S.op("pe", lambda e, k=k, w_=w_, bk=bk: e.matmul(
                        ps[bk][:, :], lhsT=xbT3[:, k, :], rhs=w_[:, k, :], start=(k == 0), stop=(k == 7)),
                        reads=[xbTB, wB_], writes=[psb[bk]])
            S.op("act", lambda e: e.activation(out=sl, in_=ps[1][:, :], func=AF.Silu), reads=[psb[1]], writes=[slB])
            S.op("dve", lambda e: e.tensor_tensor(out=hh, in0=ps[2][:, :], in1=sl, op=ALU.mult),
                 reads=[psb[2], slB], writes=[hhB])
            for k in range(4):
                S.op("pe", lambda e, k=k: e.transpose(
                    out=psbf[3][:, k * 128:(k + 1) * 128], in_=hh[:, k * 128:(k + 1) * 128], identity=identb),
                    reads=[hhB, identbB], writes=[psb[3]])
            S.op("act", lambda e: e.copy(out=hhT, in_=psbf[3][:, 0:512]), reads=[psb[3]], writes=[hhTB])
            hhT3 = hhT.rearrange("p (k t) -> p k t", k=4)
            ys, ysB = yst[b % 2]
            for hf in range(2):
                bk = 4 + hf
                for k in range(4):
                    S.op("pe", lambda e, k=k, bk=bk, hf=hf, w23=w23: e.matmul(
                        ps[bk][:, :], lhsT=hhT3[:, k, :], rhs=w23[:, k, hf * 512:(hf + 1) * 512],
                        start=(k == 0), stop=(k == 3)), reads=[hhTB, wbs[2][1]], writes=[psb[bk]])
                S.op("dve", lambda e, bk=bk, hf=hf, ys=ys: e.tensor_copy(out=ys[:, hf * 512:(hf + 1) * 512],
                                                                          in_=ps[bk][:, :]),
                     reads=[psb[bk]], writes=[ysB])
            store(YB[bsl, :], ys, ysB, YBB)
        if stop == "fg3":
            return
        A.reset(m_exp)
        g2b, g2bB = A.alloc("g2b", D, F32)
        y1 = [A.alloc(f"y1{i}", D, F32) for i in range(4)]
        y2 = [A.alloc(f"y2{i}", D, F32) for i in range(4)]
        xg = [A.alloc(f"xg{i}", D, F32) for i in range(4)]
        o1, o1B = A.alloc("o1", D, F32)
        xo = [A.alloc(f"xo{i}", D, F32) for i in range(4)]
        if last:
            gfin, gfinB = A.alloc("gfin", D, F32)
            bload(gfin, gfinB, g_fin[0:1, :])
            junk, junkB = A.alloc("junkg", D, F32)
            ss, ssB = A.alloc("ssg", 1, F32)
            rstd, rstdB = A.alloc("rstdg", 1, F32)
        for t in range(NT):
            tsl = slice(t * 128, (t + 1) * 128)
            if t % SLOT_T == 0:
                s = t // SLOT_T
                bload(g2b, g2bB, mod_row(l, s, 5), reads=[MODSB])
            ya, yaB = y1[t % 4]
            yb_, ybB = y2[t % 4]
            xi, xiB = xg[t % 4]
            for j, (yy, yyB) in enumerate(((ya, yaB), (yb_, ybB))):
                S.dma("pool", lambda e, yy=yy, t=t, j=j: e.indirect_dma_start(
                    out=yy, out_offset=None, in_=YB,
                    in_offset=bass.IndirectOffsetOnAxis(ap=DI[:, 2 * t + j:2 * t + j + 1], axis=0),
                    bounds_check=S.pool_regs["nb"], oob_is_err=False), yyB,
                    reads=[YBB, R["DIB"]], writes=[yyB])
            if t == 0:
                for tt_ in range(min(2, NT)):
                    load(xg[tt_ % 4][0], xg[tt_ % 4][1], X[tt_ * 128:(tt_ + 1) * 128, :], reads=[XB[tt_]])
            if t + 2 < NT:
                load(xg[(t + 2) % 4][0], xg[(t + 2) % 4][1], X[(t + 2) * 128:(t + 3) * 128, :], reads=[XB[t + 2]])
            dve(lambda e, ya=ya, t=t: e.tensor_scalar(out=o1, in0=ya, scalar1=WA[:, t:t + 1], scalar2=None,
                                                      op0=ALU.mult), [yaB, RB], [o1B])
            dve(lambda e, yb_=yb_, t=t: e.scalar_tensor_tensor(out=o1, in0=yb_, scalar=WB[:, t:t + 1], in1=o1,
                                                               op0=ALU.mult, op1=ALU.add), [ybB, RB, o1B], [o1B])
            S.op("pool", lambda e: e.tensor_tensor(out=o1, in0=o1, in1=g2b, op=ALU.mult),
                 reads=[o1B, g2bB], writes=[o1B])
            xoi, xoiB = xo[t % 4]
            dve(lambda e, xi=xi, xoi=xoi: e.tensor_tensor(out=xoi, in0=o1, in1=xi, op=ALU.add),
                [o1B, xiB], [xoiB])
            if not last:
                store(X[tsl, :], xoi, xoiB, XB[t])
            else:
                S.op("act", lambda e, xoi=xoi: e.activation(out=junk, in_=xoi, func=AF.Square, accum_out=ss),
                     reads=[xoiB], writes=[junkB, ssB])
                rms_rstd(ss, ssB, D, rstd, rstdB)
                dve(lambda e, xoi=xoi: e.scalar_tensor_tensor(
                    out=xoi, in0=xoi, scalar=rstd[:, 0:1], in1=gfin, op0=ALU.mult, op1=ALU.mult),
                    [xoiB, rstdB, gfinB], [xoiB])
                S.dma("sp", lambda e, xoi=xoi, tsl=tsl: e.dma_start(out=y_out[tsl, :], in_=xoi), xoiB,
                      reads=[xoiB])
                if xoiB not in final_bufs:
                    final_bufs.append(xoiB)

    ATT_MARK = PERSIST
    MOE_MARK = PERSIST
    MOE2_MARK = PERSIST
    for l in range(depth):
        phase_mod(l)
        if stop == "mod":
            break
        phase_a(l)
        if stop == "a":
            break
        phase_c1(l)
        if stop == "c1":
            break
        A.reset(PERSIST)
        QT = [A.alloc(f"QT{i}", T, BF16) for i in range(2)]
        KT = [A.alloc(f"KT{i}", T, BF16) for i in range(2)]
        for i in range(2):
            load(QT[i][0][96:100, :], QT[i][1], qseg_in)
            load(KT[i][0][96:100, :], KT[i][1], kseg_in)
        ATT_MARK = A.mark()
        phase_c2(l, QT, KT, True)
        if stop == "c2":
            break
        for i in range(2):
            load(QT[i][0][64:96, :], QT[i][1], qrow_in)
            load(KT[i][0][64:96, :], KT[i][1], krow_in)
        phase_b(l, QT, KT)
        if stop == "b":
            break
        A.reset(PERSIST)
        R = {}
        RT, R["RB"] = A.alloc("RT", NT * 32 * 3 + 2 * NT, F32)
        R["RT"] = RT
        R["M1"] = RT[:, 0:NT * 32]
        R["M2"] = RT[:, NT * 32:2 * NT * 32]
        R["POS"] = RT[:, 2 * NT * 32:3 * NT * 32]
        R["WA"] = RT[:, 3 * NT * 32:3 * NT * 32 + NT]
        R["WB"] = RT[:, 3 * NT * 32 + NT:3 * NT * 32 + 2 * NT]
        R["carry"], R["carryB"] = A.alloc("carry", 32, F32)
        R["DF"], R["DFB"] = A.alloc("DF", 2 * NT, F32)
        R["DI"], R["DIB"] = A.alloc("DI", 2 * NT, I32)
        MOE_MARK = A.mark()
        phase_de(l, R)
        if stop == "de":
            break
        MOE2_MARK = MOE_MARK
        phase_fg(l, R, l == depth - 1)
        if stop in ("fg1", "fg2", "fg3"):
            break

    S.emit(final_bufs=final_bufs)
    es.close()
    return nc, S


def _bf(a):
    return np.ascontiguousarray(a.astype(NPBF))


def core_consts(T, L):
    NT = T // 128
    tok = np.arange(T)
    seg = tok // L
    kseg = np.zeros((4, T), np.float32)
    kseg[seg, tok] = 1.0
    qseg = np.where(kseg > 0, 0.0, NEG).astype(np.float32)
    r = tok // 64
    rps = L // 64
    seg_lo = (r // rps) * rps
    rs = np.clip(r - 4, seg_lo, seg_lo + rps - 8)
    krow = np.zeros((32, T), np.float32)
    krow[r % 32, tok] = 1.0
    p = r // 2
    base = 2 * p - 8
    j = np.arange(32)[:, None]
    a_j = base[None, :] + ((j - base[None, :]) % 32)
    valid = (a_j >= rs[None, :]) & (a_j < rs[None, :] + 8) & (a_j < (2 * p + 10)[None, :])
    qrow = np.where(valid, 0.0, NEG).astype(np.float32)
    pos = (tok % L).astype(np.float32)
    inv = (1.0 / (np.float32(10000.0) ** (np.arange(0, 32, 2, dtype=np.float32) / np.float32(32)))).astype(np.float32)
    ang = (pos[:, None] * inv[None, :]).astype(np.float32)
    cos = np.cos(ang).astype(np.float32)
    sin = np.sin(ang).astype(np.float32)
    cosk = np.ascontiguousarray(cos.reshape(NT, 128, 16).transpose(1, 0, 2).reshape(128, NT * 16))
    sink = np.ascontiguousarray(sin.reshape(NT, 128, 16).transpose(1, 0, 2).reshape(128, NT * 16))
    cosq = np.ascontiguousarray(np.concatenate([cos.T, cos.T], 0))
    sinq = np.ascontiguousarray(np.concatenate([-sin.T, sin.T], 0))
    return dict(kseg=_bf(kseg), qseg=_bf(qseg), krow=_bf(krow), qrow=_bf(qrow),
                cosk=cosk, sink=sink, cosq=cosq, sinq=sinq)


def na_tables(rpb):
    depth = rpb.shape[0]
    key = np.arange(128)
    ra, ck = key // 64, key % 64
    qry = np.arange(128)
    rq, cq = qry // 64, qry % 64
    d = 2 * (np.arange(9) - 4)
    dr = d[None, :, None] + ra[:, None, None] - rq[None, None, :]
    dc = ck[:, None, None] - cq[None, None, :] + 0 * d[None, :, None]
    cs = np.clip(cq - 8, 0, 48)
    valid = (np.abs(dr) <= 7) & (ck[:, None, None] >= cs[None, None, :]) & (ck[:, None, None] < cs[None, None, :] + 16)
    dri = np.clip(dr + 7, 0, 14)
    dci = np.clip(dc + 15, 0, 30)
    g = rpb[:, :, dri, dci]
    out = np.where(valid[None, None], g, np.float32(NEG)).astype(np.float32)
    return np.ascontiguousarray(out.reshape(depth, 8, 128, 9 * 128))


def shared_inputs(T, depth, ada_w, ada_b, norm_mix_g, w_in, mla_q_norm_g, w_uq, mla_kv_norm_g, w_ukv, na_rpb,
                  w_na_o, w_mla_o, w_out, norm_ffn_g, router_wg, router_bg, router_we, router_be, expert_w1,
                  expert_w3, expert_w2, final_norm_g):
    f = lambda a: np.ascontiguousarray(np.asarray(a, dtype=np.float32))
    NB = (2 * T + 32 * 127 + 127) // 128
    w_uq = f(w_uq)[:depth]
    perm = np.arange(768)
    for h in range(8):
        b = h * 96 + 64
        perm[b:b + 16] = np.arange(b + 16, b + 32)
        perm[b + 16:b + 32] = np.arange(b, b + 16)
    w_ukv = f(w_ukv)[:depth].reshape(depth, 128, 8, 128)
    d = dict(
        ada_w=f(ada_w)[:depth], ada_b=f(ada_b)[:depth], norm_mix_g=f(norm_mix_g)[:depth],
        norm_ffn_g=f(norm_ffn_g)[:depth], final_norm_g=f(final_norm_g).reshape(1, D), w_in=f(w_in)[:depth],
        mla_q_norm_g=f(mla_q_norm_g)[:depth], mla_kv_norm_g=f(mla_kv_norm_g)[:depth],
        w_uq=w_uq, w_uq_rot=np.ascontiguousarray(w_uq[:, :, perm]),
        w_ukv_k=np.ascontiguousarray(w_ukv[:, :, :, 0:64].reshape(depth, 128, 512)),
        w_ukv_v=np.ascontiguousarray(w_ukv[:, :, :, 64:128].reshape(depth, 128, 512)),
        na_tt=na_tables(f(na_rpb)[:depth]),
        w_na_o=f(w_na_o)[:depth], w_mla_o=f(w_mla_o)[:depth], w_out=f(w_out)[:depth],
        w_router=np.ascontiguousarray(np.concatenate([f(router_wg)[:depth], f(router_we)[:depth]], -1)),
        b_router=np.ascontiguousarray(np.concatenate([f(router_bg)[:depth], f(router_be)[:depth]], -1)),
        w1r=np.ascontiguousarray(f(expert_w1)[:depth].reshape(depth, 32, 8, 128, 512).transpose(0, 1, 3, 2, 4)
                                 .reshape(depth * 32 * 128, 4096)),
        w3r=np.ascontiguousarray(f(expert_w3)[:depth].reshape(depth, 32, 8, 128, 512).transpose(0, 1, 3, 2, 4)
                                 .reshape(depth * 32 * 128, 4096)),
        w2r=np.ascontiguousarray(f(expert_w2)[:depth].reshape(depth, 32, 4, 128, 1024).transpose(0, 1, 3, 2, 4)
                                 .reshape(depth * 32 * 128, 4096)),
        identb=_bf(np.eye(128, dtype=np.float32)), identf=np.eye(128, dtype=np.float32),
        tril=_bf(np.triu(np.ones((128, 128), np.float32), 1)), onesb=_bf(np.ones((128, 128), np.float32)),
        iotap=np.arange(128, dtype=np.float32).reshape(128, 1),
        blkstart=np.ascontiguousarray(np.broadcast_to((np.arange(NB, dtype=np.float32) * 128)[None, :], (128, NB))),
    )
    return d


def core_inputs(T, L, x, cs):
    c4 = np.stack([np.asarray(c, np.float32) for c in cs], 0)
    cT = np.ascontiguousarray(c4.reshape(4, 8, 128).transpose(2, 1, 0).reshape(128, 32))
    d = dict(x=np.ascontiguousarray(np.asarray(x, np.float32)), cT=cT)
    d.update(core_consts(T, L))
    return d


_CACHE = {}


def kernel(x_prompt, x_sample, c_prompt, c_sample, **w):
    T, depth = 8192, 4
    x_prompt = np.asarray(x_prompt, np.float32)
    x_sample = np.asarray(x_sample, np.float32)
    c_prompt = np.asarray(c_prompt, np.float32)
    c_sample = np.asarray(c_sample, np.float32)
    if "nc" not in _CACHE:
        _CACHE["nc"] = build(T, depth)[0]
    nc = _CACHE["nc"]
    sh = shared_inputs(T, depth, **w)
    in_maps = []
    for i in range(4):
        m = dict(sh)
        m.update(core_inputs(T, 8192, x_prompt[i], [c_prompt[i]] * 4))
        in_maps.append(m)
    for i in range(4):
        j = i % 2
        m = dict(sh)
        m.update(core_inputs(T, 2048, x_sample[4 * j:4 * j + 4].reshape(T, D), [c_sample[4 * j + s] for s in range(4)]))
        in_maps.append(m)
    res = run_bass_kernel_spmd(nc, in_maps, core_ids=list(range(8)))
    ys = [r["y"] for r in res.results]
    y_prompt = np.stack(ys[0:4], 0).astype(np.float32)
    y_sample = np.concatenate([ys[4].reshape(4, 2048, D), ys[5].reshape(4, 2048, D)], 0).astype(np.float32)
    return (y_prompt, y_sample)
```
